# Optimizing a Trainium2 kernel written in Bass

```python
import math
import jax, jax.numpy as jnp
from jax import lax
import numpy as np

D_MODEL = 1024
BATCH = 8
SEQ = 4096
DEPTH = 2

N_GROUPS = 4
GROUP_WIDTH = D_MODEL // N_GROUPS
HEADS_PER_GROUP = 4
HEAD_DIM = GROUP_WIDTH // HEADS_PER_GROUP
DIFF_QK_DIM = HEAD_DIM // 2
D_FF = 4 * D_MODEL
CHUNK = 64
Q_BLOCK = 128
CONV_WIDTH = 4
NORM_EPS = 1e-6
IN_WIDTHS = (GROUP_WIDTH,) * 4 + (GROUP_WIDTH,) * 3 + (GROUP_WIDTH,) * 4 + (HEADS_PER_GROUP,) + (GROUP_WIDTH,) * 4 + (HEADS_PER_GROUP,) * 2
IN_COLS = 15 * GROUP_WIDTH + 3 * HEADS_PER_GROUP

kernel_name = "hybrid_parallel_groups_hgrn2_diff_fox_mlstm"


def _rms(x, gain):
    xf = x.astype(jnp.float32)
    y = xf * lax.rsqrt(jnp.mean(xf * xf, axis=-1, keepdims=True) + NORM_EPS)
    return y * gain.astype(jnp.float32)


def _modulate(x, gain, shift, scale):
    return _rms(x, gain) * (1.0 + scale[:, None, :]) + shift[:, None, :]


def _split(z, widths):
    idx = np.cumsum(np.array(widths))[:-1].tolist()
    return jnp.split(z, idx, axis=-1)


def _heads(t):
    B, S, W = t.shape
    return t.reshape(B, S, W // HEAD_DIM, HEAD_DIM).transpose(0, 2, 1, 3)


def _unheads(t):
    B, Hh, S, d = t.shape
    return t.transpose(0, 2, 1, 3).reshape(B, S, Hh * d)


def _to_chunks(t):
    B, Hh, S = t.shape[:3]
    t = t.reshape((B, Hh, S // CHUNK, CHUNK) + t.shape[3:])
    return jnp.moveaxis(t, 2, 0)


def _from_chunks(t):
    t = jnp.moveaxis(t, 0, 2)
    B, Hh, N, C, d = t.shape
    return t.reshape(B, Hh, N * C, d)


def _hgrn2(q, f_pre, i, g, lb, norm_gain):
    f = lb + (1.0 - lb) * jax.nn.sigmoid(f_pre)
    k = 1.0 - f
    logf = jnp.log(f)
    qc, kc, vc, lc = (_to_chunks(_heads(t)) for t in (q, k, i, logf))
    causal = jnp.tril(jnp.ones((CHUNK, CHUNK), dtype=bool))

    def step(state, inp):
        qt, kt, vt, lt = inp
        b = jnp.cumsum(lt, axis=-2)
        diff = b[..., :, None, :] - b[..., None, :, :]
        decay = jnp.exp(jnp.where(causal[:, :, None], diff, -jnp.inf))
        a = jnp.einsum('bhtd,bhtsd,bhsd->bhts', qt, decay, kt)
        o = jnp.einsum('bhts,bhsv->bhtv', a, vt) + jnp.einsum('bhtd,bhdv->bhtv', qt * jnp.exp(b), state)
        b_last = b[..., -1:, :]
        state = jnp.exp(b_last[..., 0, :])[..., None] * state + jnp.einsum('bhsd,bhsv->bhdv', kt * jnp.exp(b_last - b), vt)
        return state, o

    B = q.shape[0]
    s0 = jnp.zeros((B, HEADS_PER_GROUP, HEAD_DIM, HEAD_DIM), jnp.float32)
    _, o = lax.scan(step, s0, (qc, kc, vc, lc))
    o = _from_chunks(o).transpose(0, 2, 1, 3)
    o = _rms(o, norm_gain).reshape(q.shape)
    return o * jax.nn.silu(g)


def _diff_attention(q, k, v, qn_gain, kn_gain, lam_vecs, sub_gain, layer_idx):
    B, S, _ = q.shape
    H = HEADS_PER_GROUP
    q = _rms(q.reshape(B, S, H, 2, DIFF_QK_DIM), qn_gain).transpose(0, 2, 3, 1, 4) * DIFF_QK_DIM ** -0.5
    k = _rms(k.reshape(B, S, H, 2, DIFF_QK_DIM), kn_gain).transpose(0, 2, 3, 1, 4)
    v = _heads(v)
    lam_init = 0.8 - 0.6 * math.exp(-0.3 * layer_idx)
    lam_vecs = lam_vecs.astype(jnp.float32)
    lam = jnp.exp(jnp.sum(lam_vecs[0] * lam_vecs[1])) - jnp.exp(jnp.sum(lam_vecs[2] * lam_vecs[3])) + lam_init
    slopes = 2.0 ** (-8.0 * jnp.arange(1, H + 1, dtype=jnp.float32) / H)
    pos = jnp.arange(S)

    def block(bi):
        qb = lax.dynamic_slice_in_dim(q, bi * Q_BLOCK, Q_BLOCK, axis=3)
        tq = bi * Q_BLOCK + jnp.arange(Q_BLOCK)
        dist = tq[:, None] - pos[None, :]
        bias = -slopes[:, None, None] * dist.astype(jnp.float32)
        logits = jnp.einsum('bhcqd,bhcsd->bhcqs', qb, k) + bias[None, :, None]
        logits = jnp.where(dist >= 0, logits, -jnp.inf)
        p = jax.nn.softmax(logits, axis=-1)
        w = p[:, :, 0] - lam * p[:, :, 1]
        return jnp.einsum('bhqs,bhsv->bhqv', w, v)

    o = lax.map(block, jnp.arange(S // Q_BLOCK))
    o = jnp.moveaxis(o, 0, 2).reshape(B, H, S, HEAD_DIM)
    o = _rms(o, sub_gain) * (1.0 - lam_init)
    return _unheads(o)


def _forgetting_attention(q, k, v, g, f_pre, qn_gain, kn_gain, f_bias):
    B, S, _ = q.shape
    q = _rms(_heads(q), qn_gain) * HEAD_DIM ** -0.5
    k = _rms(_heads(k), kn_gain)
    v = _heads(v)
    logf = jax.nn.log_sigmoid(f_pre + f_bias.astype(jnp.float32))
    F = jnp.cumsum(logf, axis=1).transpose(0, 2, 1)
    pos = jnp.arange(S)

    def block(bi):
        qb = lax.dynamic_slice_in_dim(q, bi * Q_BLOCK, Q_BLOCK, axis=2)
        Fq = lax.dynamic_slice_in_dim(F, bi * Q_BLOCK, Q_BLOCK, axis=2)
        tq = bi * Q_BLOCK + jnp.arange(Q_BLOCK)
        logits = jnp.einsum('bhqd,bhsd->bhqs', qb, k) + Fq[..., :, None] - F[..., None, :]
        logits = jnp.where(tq[:, None] >= pos[None, :], logits, -jnp.inf)
        p = jax.nn.softmax(logits, axis=-1)
        return jnp.einsum('bhqs,bhsv->bhqv', p, v)

    o = lax.map(block, jnp.arange(S // Q_BLOCK))
    o = jnp.moveaxis(o, 0, 2).reshape(B, HEADS_PER_GROUP, S, HEAD_DIM)
    return _unheads(o) * jax.nn.sigmoid(g)


def _mlstm(q, k, v, o_pre, i_pre, f_pre, conv_w, i_bias, f_bias):
    B, S, G = q.shape
    qk = jnp.concatenate([q, k], axis=-1)
    qk = lax.conv_general_dilated(qk, conv_w.astype(qk.dtype)[:, None, :], window_strides=(1,),
                                  padding=[(CONV_WIDTH - 1, 0)], dimension_numbers=('NWC', 'WIO', 'NWC'),
                                  feature_group_count=2 * G)
    qk = jax.nn.silu(qk)
    q, k = qk[..., :G], qk[..., G:]
    log_i = (i_pre + i_bias.astype(jnp.float32)).transpose(0, 2, 1)
    log_f = jax.nn.log_sigmoid(f_pre + f_bias.astype(jnp.float32)).transpose(0, 2, 1)
    qc = _to_chunks(_heads(q) * HEAD_DIM ** -0.5)
    kc = _to_chunks(_heads(k))
    vc = _to_chunks(_heads(v))
    ic = _to_chunks(log_i)
    fc = _to_chunks(log_f)
    causal = jnp.tril(jnp.ones((CHUNK, CHUNK), dtype=bool))

    def step(carry, inp):
        cmem, nvec, m = carry
        qt, kt, vt, it, lft = inp
        b = jnp.cumsum(lft, axis=-1)
        log_d = jnp.where(causal, b[..., :, None] - b[..., None, :] + it[..., None, :], -jnp.inf)
        inter = b + m[..., None]
        m_t = jnp.maximum(inter, jnp.max(log_d, axis=-1))
        d = jnp.exp(log_d - m_t[..., None])
        w_inter = jnp.exp(inter - m_t)
        s = jnp.einsum('bhtd,bhsd->bhts', qt, kt) * d
        num = jnp.einsum('bhts,bhsv->bhtv', s, vt) + w_inter[..., None] * jnp.einsum('bhtd,bhdv->bhtv', qt, cmem)
        den = jnp.sum(s, axis=-1) + w_inter * jnp.einsum('bhtd,bhd->bht', qt, nvec)
        h = num / jnp.maximum(jnp.abs(den), jnp.exp(-m_t))[..., None]
        b_last = b[..., -1]
        log_w = b_last[..., None] - b + it
        m_new = jnp.maximum(b_last + m, jnp.max(log_w, axis=-1))
        w = jnp.exp(log_w - m_new[..., None])
        decay = jnp.exp(b_last + m - m_new)
        cmem = decay[..., None, None] * cmem + jnp.einsum('bhs,bhsd,bhsv->bhdv', w, kt, vt)
        nvec = decay[..., None] * nvec + jnp.einsum('bhs,bhsd->bhd', w, kt)
        return (cmem, nvec, m_new), h

    H = HEADS_PER_GROUP
    init = (jnp.zeros((B, H, HEAD_DIM, HEAD_DIM), jnp.float32),
            jnp.zeros((B, H, HEAD_DIM), jnp.float32),
            jnp.zeros((B, H), jnp.float32))
    _, h = lax.scan(step, init, (qc, kc, vc, ic, fc))
    h = _unheads(_from_chunks(h))
    return jax.nn.sigmoid(o_pre) * h


def setup_inputs(seed: int = 0) -> dict:
    key = jax.random.key(seed)
    ks = jax.random.split(key, 24)
    D, G, H, L = D_MODEL, GROUP_WIDTH, HEADS_PER_GROUP, DEPTH
    nrm = jax.random.normal
    f32 = jnp.float32
    return {
        "x": nrm(ks[0], (BATCH, SEQ, D), f32),
        "c": nrm(ks[1], (BATCH, D), f32),
        "w_ada": nrm(ks[2], (L, D, 6 * D), f32) * 0.5 * D ** -0.5,
        "b_ada": 0.01 * nrm(ks[3], (L, 6 * D), f32),
        "norm_mix_gain": 1.0 + 0.05 * nrm(ks[4], (L, D), f32),
        "norm_ff_gain": 1.0 + 0.05 * nrm(ks[5], (L, D), f32),
        "w_in": nrm(ks[6], (L, D, IN_COLS), f32) * D ** -0.5,
        "w_out": nrm(ks[7], (L, D, D), f32) * D ** -0.5,
        "hg_lb_logits": 0.5 * nrm(ks[8], (L, G), f32),
        "hg_norm_gain": 1.0 + 0.05 * nrm(ks[9], (L, HEAD_DIM), f32),
        "diff_qn_gain": 1.0 + 0.05 * nrm(ks[10], (L, DIFF_QK_DIM), f32),
        "diff_kn_gain": 1.0 + 0.05 * nrm(ks[11], (L, DIFF_QK_DIM), f32),
        "diff_lambda": 0.1 * nrm(ks[12], (L, 4, DIFF_QK_DIM), f32),
        "diff_sub_gain": 1.0 + 0.05 * nrm(ks[13], (L, HEAD_DIM), f32),
        "fox_qn_gain": 1.0 + 0.05 * nrm(ks[14], (L, HEAD_DIM), f32),
        "fox_kn_gain": 1.0 + 0.05 * nrm(ks[15], (L, HEAD_DIM), f32),
        "fox_f_bias": jnp.linspace(1.0, 4.0, H, dtype=f32)[None, :] + 0.01 * nrm(ks[16], (L, H), f32),
        "mlstm_conv": nrm(ks[17], (L, CONV_WIDTH, 2 * G), f32) * CONV_WIDTH ** -0.5,
        "mlstm_i_bias": 0.01 * nrm(ks[18], (L, H), f32),
        "mlstm_f_bias": jnp.linspace(3.0, 6.0, H, dtype=f32)[None, :] + 0.01 * nrm(ks[19], (L, H), f32),
        "w_ff1": nrm(ks[20], (L, D, D_FF), f32) * D ** -0.5,
        "w_ff2": nrm(ks[21], (L, D_FF, D), f32) * D_FF ** -0.5,
    }


def reference(x, c, w_ada, b_ada, norm_mix_gain, norm_ff_gain, w_in, w_out, hg_lb_logits, hg_norm_gain,
              diff_qn_gain, diff_kn_gain, diff_lambda, diff_sub_gain, fox_qn_gain, fox_kn_gain, fox_f_bias,
              mlstm_conv, mlstm_i_bias, mlstm_f_bias, w_ff1, w_ff2):
    p_lb = jax.nn.softmax(hg_lb_logits.astype(jnp.float32), axis=0)
    lower_bounds = jnp.cumsum(p_lb, axis=0) - p_lb[0:1]
    c_act = jax.nn.silu(c.astype(jnp.float32))
    for l in range(DEPTH):
        mod = c_act @ w_ada[l] + b_ada[l]
        shift1, scale1, gate1, shift2, scale2, gate2 = jnp.split(mod, 6, axis=-1)

        h = _modulate(x, norm_mix_gain[l], shift1, scale1)
        z = (h @ w_in[l]).astype(jnp.float32)
        (hq, hf, hi, hg,
         dq, dk, dv,
         fq, fk, fv, fg, ff,
         mq, mk, mv, mo, mi, mf) = _split(z, IN_WIDTHS)
        y_a = _hgrn2(hq, hf, hi, hg, lower_bounds[l], hg_norm_gain[l])
        y_b = _diff_attention(dq, dk, dv, diff_qn_gain[l], diff_kn_gain[l], diff_lambda[l], diff_sub_gain[l], l)
        y_c = _forgetting_attention(fq, fk, fv, fg, ff, fox_qn_gain[l], fox_kn_gain[l], fox_f_bias[l])
        y_d = _mlstm(mq, mk, mv, mo, mi, mf, mlstm_conv[l], mlstm_i_bias[l], mlstm_f_bias[l])
        y = jnp.concatenate([y_a, y_b, y_c, y_d], axis=-1) @ w_out[l]
        x = x + (gate1[:, None, :] * y).astype(x.dtype)

        h = _modulate(x, norm_ff_gain[l], shift2, scale2)
        y = jnp.square(jax.nn.relu(h @ w_ff1[l])) @ w_ff2[l]
        x = x + (gate2[:, None, :] * y).astype(x.dtype)
    return x
```

```python
import numpy as np
import math
from contextlib import ExitStack
import concourse.bass as bass
import concourse.mybir as mybir
from concourse.bass_utils import run_bass_kernel_spmd

F32 = mybir.dt.float32
BF16 = mybir.dt.bfloat16
AF = mybir.ActivationFunctionType
ALU = mybir.AluOpType
AX = mybir.AxisListType

ENGS = ("pe", "act", "dve", "pool", "sp")

D = 1024
SEQ = 4096
L = 2
NFM = 34
NTM = 1024
NCOL = NFM * 128 + NTM
EPS = 1e-6
NPP = 56
import os
GLA_STOP = int(os.environ.get('GLA_STOP', '0'))
GLA_SKIP = os.environ.get('GLA_SKIP', '')


class Sched:
    def __init__(self, nc, n_dma_sems=24):
        self.nc = nc
        self.streams = {e: [] for e in ENGS}
        self.esem = {e: nc.alloc_semaphore("s_" + e) for e in ENGS}
        self.cnt = {e: 0 for e in ENGS}
        self.seen = {e: {} for e in ENGS}
        self.lastw = {}
        self.readers = {}
        self.dsems = {}
        self.dcnt = {}
        self.dnext = {}
        for q in ("sp", "act", "pool"):
            self.dsems[q] = [nc.alloc_semaphore("d_%s%d" % (q, i)) for i in range(n_dma_sems)]
            self.dcnt[q] = [0] * n_dma_sems
            self.dnext[q] = 0
        self.semh = {}
        for e in ENGS:
            self.semh[("e", e)] = self.esem[e]
        for q in self.dsems:
            for i, s in enumerate(self.dsems[q]):
                self.semh[("d", q, i)] = s
        self.n_waits = 0
        self.n_ops = 0

    def _deps(self, reads, writes):
        deps = {}

        def add(tok):
            if tok is None:
                return
            k, v = tok
            if deps.get(k, 0) < v:
                deps[k] = v

        for r in reads:
            add(self.lastw.get(r))
        for r in writes:
            add(self.lastw.get(r))
            for t in self.readers.get(r, ()):
                add(t)
        return deps

    def _emit_waits(self, e, deps):
        seen = self.seen[e]
        for k, v in deps.items():
            if seen.get(k, 0) >= v:
                continue
            if e == "pe" and k == ("e", "pe"):
                continue
            seen[k] = v
            h = self.semh[k]
            self.streams[e].append(lambda eng, h=h, v=v: eng.wait_ge(h, v))
            self.n_waits += 1

    def _commit(self, tok, reads, writes):
        for r in reads:
            lst = self.readers.setdefault(r, [])
            lst.append(tok)
            if len(lst) > 64:
                m = {}
                for k, v in lst:
                    if m.get(k, 0) < v:
                        m[k] = v
                self.readers[r] = list(m.items())
        for r in writes:
            self.lastw[r] = tok
            self.readers[r] = []

    def op(self, e, fn, reads=(), writes=()):
        deps = self._deps(reads, writes)
        self._emit_waits(e, deps)
        self.cnt[e] += 1
        sem = self.esem[e]
        self.streams[e].append(lambda eng, fn=fn, sem=sem: fn(eng).then_inc(sem, 1))
        tok = (("e", e), self.cnt[e])
        self._commit(tok, reads, writes)
        self.n_ops += 1
        return tok

    def pe_group(self, fns, reads=(), writes=()):
        deps = self._deps(reads, writes)
        self._emit_waits("pe", deps)
        self.cnt["pe"] += 1
        sem = self.esem["pe"]
        for fn in fns[:-1]:
            self.streams["pe"].append(lambda eng, fn=fn: fn(eng))
        fn = fns[-1]
        self.streams["pe"].append(lambda eng, fn=fn, sem=sem: fn(eng).then_inc(sem, 1))
        tok = (("e", "pe"), self.cnt["pe"])
        self._commit(tok, reads, writes)
        self.n_ops += len(fns)
        return tok

    def dma(self, q, out, in_, reads=(), writes=(), **kw):
        i = self.dnext[q]
        self.dnext[q] = (i + 1) % len(self.dsems[q])
        k = ("d", q, i)
        deps = self._deps(reads, writes)
        if self.dcnt[q][i] > 0:
            deps[k] = max(deps.get(k, 0), self.dcnt[q][i])
        self._emit_waits(q, deps)
        self.dcnt[q][i] += 16
        h = self.semh[k]
        self.streams[q].append(
            lambda eng, out=out, in_=in_, h=h, kw=kw: eng.dma_start(out=out, in_=in_, **kw).then_inc(h, 16))
        tok = (k, self.dcnt[q][i])
        self._commit(tok, reads, writes)
        return tok

    def barrier(self):
        allk = {}
        for e in ENGS:
            if self.cnt[e] > 0:
                allk[("e", e)] = self.cnt[e]
        for q in self.dsems:
            for i, c in enumerate(self.dcnt[q]):
                if c > 0:
                    allk[("d", q, i)] = c
        for e in ENGS:
            self._emit_waits(e, dict(allk))
        self.lastw.clear()
        self.readers.clear()

    def finish(self):
        self.barrier()
        nc = self.nc
        streams = self.streams
        with nc.Block() as block:
            @block.tensor
            def _(eng):
                for f in streams["pe"]:
                    f(eng)

            @block.scalar
            def _(eng):
                for f in streams["act"]:
                    f(eng)

            @block.vector
            def _(eng):
                for f in streams["dve"]:
                    f(eng)

            @block.gpsimd
            def _(eng):
                for f in streams["pool"]:
                    f(eng)

            @block.sync
            def _(eng):
                for f in streams["sp"]:
                    f(eng)


class Ring:
    def __init__(self, name, bufs):
        self.name = name
        self.bufs = bufs
        self.i = 0

    def next(self):
        i = self.i
        self.i = (i + 1) % len(self.bufs)
        return self.bufs[i], (self.name, i)


OFF = dict(hq=0, hf=256, hi=512, hg=768, dq=1024, dk=1280, dv=1536, fq=1792, fk=2048, fv=2304,
           fg=2560, ff=2816, mq=2820, mk=3076, mv=3332, mo=3588, mi=3844, mf=3848)
T_HQ, T_HF, T_HG, T_MQ, T_MK, T_MO, T_MI, T_MF = 0, 2, 4, 6, 8, 10, 12, 14
T_FQ, T_FK, T_FG, T_DQ, T_DK = 16, 20, 24, 26, 30
V_H, V_M, V_F, V_D = 0, 256, 512, 768


def _col_index():
    cols = []

    def rng(base, n):
        return list(range(base, base + n))

    for nm in ("hq", "hf", "hg", "mq", "mk", "mo"):
        cols += rng(OFF[nm], 256)
    for nm in ("mi", "mf"):
        for h in range(4):
            cols += [OFF[nm] + h] * 64
    for h in range(4):
        cols += rng(OFF["fq"] + 64 * h, 64) + [OFF["ff"] + h] + [-1] * 63
    for h in range(4):
        cols += rng(OFF["fk"] + 64 * h, 64) + [-1] * 64
    cols += rng(OFF["fg"], 256)
    for nm in ("dq", "dk"):
        for h in range(4):
            b = OFF[nm] + 64 * h
            cols += rng(b, 32) + [-1] * 32 + rng(b + 32, 32) + [-1] * 32
    assert len(cols) == NFM * 128
    for nm in ("hi", "mv", "fv", "dv"):
        cols += rng(OFF[nm], 256)
    assert len(cols) == NCOL
    return np.array(cols)


def _host_consts():
    p = np.arange(128)
    c = {}
    c["ident"] = np.eye(128, dtype=np.float32)
    c["blk64"] = (p[:, None] // 64 == p[None, :] // 64).astype(np.float32)
    in1 = (p < 32)
    in2 = (p >= 64) & (p < 96)
    c["blk32d"] = ((in1[:, None] & in1[None, :]) | (in2[:, None] & in2[None, :])).astype(np.float32)
    c["trimask"] = np.where(p[:, None] <= p[None, :], 0.0, -30000.0).astype(np.float32)
    c["glamask"] = ((p[:, None] // 64 == p[None, :] // 64) & (p[:, None] <= p[None, :])).astype(np.float32)
    slopes = 2.0 ** (-8.0 * np.arange(1, 5, dtype=np.float64) / 4)
    pos = (np.arange(32)[None, :] * 128 + p[:, None]).astype(np.float64)
    c["alibi"] = np.stack([slopes[h] * pos for h in range(4)], axis=1).astype(np.float32)
    c["crow"] = np.stack([-slopes[h] * np.arange(SEQ, dtype=np.float64) for h in range(4)], 0).astype(np.float32)
    cmat = np.concatenate([c["ident"], c["blk64"], c["blk32d"], c["trimask"], c["glamask"]], axis=1)
    return cmat.astype(np.float32), c["alibi"].reshape(128, 128).copy(), c["crow"]


def _mask4():
    p = np.arange(128)
    s_, t_ = p[:, None], p[None, :]
    d32 = ((s_ // 32 == t_ // 32) & (s_ <= t_)).astype(np.float32)
    ba = ((s_ // 64 == t_ // 64) & (s_ % 64 < 32) & (t_ % 64 >= 32)).astype(np.float32)
    return np.ascontiguousarray(np.concatenate([d32, ba], axis=1))


def _pp_layer(inp, l):
    pp = np.zeros((128, NPP), np.float32)
    p = np.arange(128)
    pp[:, 0:8] = inp["norm_mix_gain"][l].reshape(8, 128).T
    pp[:, 8:16] = inp["norm_ff_gain"][l].reshape(8, 128).T
    pp[:, 16:18] = inp["hg_lb_logits"][0].reshape(2, 128).T
    pp[:, 18:20] = inp["hg_lb_logits"][1].reshape(2, 128).T
    pp[:, 20] = inp["hg_norm_gain"][l][p % 64]
    for col, nm in ((21, "diff_qn_gain"), (22, "diff_kn_gain")):
        g = inp[nm][l]
        pp[0:32, col] = g
        pp[64:96, col] = g
    pp[:, 23] = inp["diff_sub_gain"][l][p % 64]
    pp[:, 24] = inp["fox_qn_gain"][l][p % 64]
    pp[:, 25] = inp["fox_kn_gain"][l][p % 64]
    for h in range(4):
        pp[:, 26 + h] = inp["fox_f_bias"][l][h]
    for tp in range(2):
        pp[:, 30 + tp] = inp["mlstm_i_bias"][l][2 * tp + p // 64]
        pp[:, 32 + tp] = inp["mlstm_f_bias"][l][2 * tp + p // 64]
        for j in range(4):
            pp[:, 34 + tp * 4 + j] = inp["mlstm_conv"][l][j, tp * 128 + p]
            pp[:, 42 + tp * 4 + j] = inp["mlstm_conv"][l][j, 256 + tp * 128 + p]
    return pp


class MK:
    def __init__(self, dbg=False, phases="0ABCD", nlayers=L, mixers="hmfd"):
        self.mixers = mixers
        self.dbg = dbg
        self.phases = phases
        self.nlayers = nlayers
        nc = bass.Bass("TRN2", target_bir_lowering=False)
        self.nc = nc
        self.S = Sched(nc)
        ik = "ExternalInput"
        sk = "ExternalOutput" if dbg else "Internal"
        dt = nc.dram_tensor
        self.xT = dt("xT", [D, SEQ], F32, kind=ik)
        self.c8 = dt("c8", [128, 8], F32, kind=ik)
        self.w_ada = dt("w_ada", [L, D, 6 * D], F32, kind=ik)
        self.b_ada = dt("b_ada", [L, 6 * D], F32, kind=ik)
        self.w_in = dt("w_in", [L, D, NCOL], F32, kind=ik)
        self.w_out = dt("w_out", [L, D, D], F32, kind=ik)
        self.w_ff1 = dt("w_ff1", [L, D, 4 * D], F32, kind=ik)
        self.w_ff2 = dt("w_ff2", [L, 4 * D, D], F32, kind=ik)
        self.pp = dt("pp", [L, 128, NPP], F32, kind=ik)
        self.lam = dt("lam", [L, 128], F32, kind=ik)
        self.cmat = dt("cmat", [128, 5 * 128], F32, kind=ik)
        self.alibi = dt("alibi", [128, 128], F32, kind=ik)
        self.crow = dt("crow", [4, SEQ], F32, kind=ik)
        self.onesrow = dt("onesrow", [1, SEQ], F32, kind=ik)
        self.mask4 = dt("mask4", [128, 256], F32, kind=ik)
        self.ycat_in = dt("ycat_in", [D, SEQ], F32, kind=ik) if dbg else None
        self.outT = dt("outT", [D, SEQ], F32, kind="ExternalOutput")
        self.xres = dt("xres", [D, SEQ], F32, kind=sk)
        self.zfm = dt("zfm", [NFM * 128, SEQ], F32, kind=sk)
        self.ztm = dt("ztm", [SEQ, NTM], BF16, kind=sk)
        self.ycat = dt("ycat", [D, SEQ], BF16, kind=sk)
        self.modd = dt("modd", [128, 48], F32, kind=sk)
        self.PS = [nc.alloc_psum_tensor("ps%d" % i, [128, 512], F32) for i in range(8)]
        self.build()

    def act(self, out, in_, func, R, W, bias=0.0, scale=1.0):
        return self.S.op("act", lambda e: e.activation(out=out, in_=in_, func=func, bias=bias, scale=scale), R, W)

    def ts(self, eng, out, in0, s1, s2, op0, op1, R, W):
        if s2 is None:
            return self.S.op(eng, lambda e: e.tensor_scalar(out, in0, s1, None, op0), R, W)
        return self.S.op(eng, lambda e: e.tensor_scalar(out, in0, s1, s2, op0, op1), R, W)

    def tt(self, eng, out, in0, in1, op, R, W):
        return self.S.op(eng, lambda e: e.tensor_tensor(out, in0, in1, op), R, W)

    def stt(self, eng, out, in0, sc, in1, op0, op1, R, W):
        return self.S.op(eng, lambda e: e.scalar_tensor_tensor(out, in0, sc, in1, op0, op1), R, W)

    def cp(self, eng, out, in_, R, W):
        if eng == "act":
            return self.act(out, in_, AF.Copy, R, W)
        return self.S.op(eng, lambda e: e.tensor_copy(out, in_), R, W)

    def mm(self, items, R, W):
        fns = []
        for (out, lhsT, rhs, st, sp) in items:
            fns.append(lambda e, out=out, lhsT=lhsT, rhs=rhs, st=st, sp=sp:
                       e.matmul(out, lhsT=lhsT, rhs=rhs, start=st, stop=sp))
        return self.S.pe_group(fns, R, W)

    def dump(self, name, ap, key, shape, dtype=F32):
        if not self.dbg:
            return
        t = self.nc.dram_tensor("dbg_" + name, list(shape), dtype, kind="ExternalOutput")
        self.S.dma("sp", t.ap(), ap, reads=[key], writes=[("dbg", name)])

    def u(self, name):
        self._uid = getattr(self, '_uid', 0) + 1
        return '%s_%d' % (name, self._uid)

    def sb(self, name, shape, dtype):
        return self.nc.alloc_sbuf_tensor(name, shape, dtype)

    def build(self):
        nc, S = self.nc, self.S
        self.cbf = self.sb("cbf", [128, 5 * 128], BF16)
        self.cf = self.sb("cf", [128, 5 * 128], F32)
        S.dma("pool", self.cbf[:], self.cmat.ap(), writes=["cbf"])
        S.dma("sp", self.cf[:], self.cmat.ap(), writes=["cf"])
        self.cf2 = self.sb("cf2", [128, 256], F32)
        S.dma("sp", self.cf2[:], self.mask4.ap(), writes=["cf2"])
        self.mask4_f = self.cf2
        self.ident_bf = self.cbf[:, 0:128]
        self.blk64_bf = self.cbf[:, 128:256]
        self.blk32d_bf = self.cbf[:, 256:384]
        self.trimask_bf = self.cbf[:, 384:512]
        self.ident_f = self.cf[:, 0:128]
        self.glamask_f = self.cf[:, 512:640]
        self.ones_bf = self.sb("ones_bf", [128, 128], BF16)
        self.ones_f = self.sb("ones_f", [128, 512], F32)
        S.op("dve", lambda e: e.memset(self.ones_bf[:], 1.0), writes=["ones_bf"])
        S.op("dve", lambda e: e.memset(self.ones_f[:], 1.0), writes=["ones_f"])
        self.ppt = self.sb("ppt", [128, L, NPP], F32)
        for l in range(L):
            S.dma("sp", self.ppt[:, l, :], self.pp[l], writes=["ppt"])
        self.modv = self.sb("modv", [128, 48], F32)
        self.AB = self.sb("AB", [128, 16], F32)
        S.barrier()
        base = (nc.sbuf_base, nc.sbuf_top) if hasattr(nc, "sbuf_base") else None
        for l in range(self.nlayers):
            src = self.xT if l == 0 else self.xres
            dst = self.xres if l == 0 and self.nlayers > 1 else self.outT
            if "0" in self.phases:
                self.phase0(l)
            if "A" in self.phases:
                self.phaseA(l, src)
            if "B" in self.phases:
                self.phaseB(l)
            if "C" in self.phases:
                self.phaseCD(l, src, dst)
        S.finish()

    def phase0(self, l):
        nc, S = self.nc, self.S
        with nc.sbuf_tensor(self.u("cin"), [128, 8], F32) as cin, \
                nc.sbuf_tensor(self.u("ctmp"), [128, 8], F32) as ctmp, \
                nc.sbuf_tensor(self.u("cact"), [128, 8], F32) as cact, \
                nc.sbuf_tensor(self.u("wa0"), [128, 8, 512], F32) as wa0, \
                nc.sbuf_tensor(self.u("wa1"), [128, 8, 512], F32) as wa1, \
                nc.sbuf_tensor(self.u("modrow"), [1, 6 * D], F32) as modrow, \
                nc.sbuf_tensor(self.u("badar"), [1, 6 * D], F32) as badar:
            S.dma("sp", cin[:], self.c8.ap(), writes=["cin"])
            S.dma("sp", badar[:], self.b_ada[l:l + 1, :], writes=["badar"])
            self.act(ctmp[:], cin[:], AF.Exp, ["cin"], ["ctmp"], scale=-1.0)
            self.ts("dve", ctmp[:], ctmp[:], 1.0, None, ALU.add, None, ["ctmp"], ["ctmp"])
            S.op("dve", lambda e: e.reciprocal(ctmp[:], ctmp[:]), ["ctmp"], ["ctmp"])
            self.tt("dve", cact[:], cin[:], ctmp[:], ALU.mult, ["cin", "ctmp"], ["cact"])
            wring = Ring("wa", [wa0, wa1])
            for j in range(12):
                buf, bk = wring.next()
                S.dma("sp", buf[:], self.w_ada[l, :, j * 512:(j + 1) * 512].rearrange("(kt p) n -> p kt n", p=128),
                      writes=[bk])
                ps = self.PS[j % 2]
                pk = ("ps", j % 2)
                self.mm([(ps[0:1, :], cact[:, kt:kt + 1], buf[:, kt, :], kt == 0, kt == 7) for kt in range(8)],
                        ["cact", bk], [pk])
                self.tt("dve", modrow[0:1, j * 512:(j + 1) * 512], ps[0:1, :], badar[0:1, j * 512:(j + 1) * 512],
                        ALU.add, [pk, "badar"], ["modrow"])
            ps = self.PS[2]
            self.mm([(ps[:, j:j + 1], modrow[0:1, j * 128:(j + 1) * 128], self.ones_f[0:1, 0:1], True, True)
                     for j in range(48)], ["modrow", "ones_f"], [("ps", 2)])
            self.cp("dve", self.modv[:], ps[:, 0:48], [("ps", 2)], ["modv"])
            pp = self.ppt
            self.stt("dve", self.AB[:, 0:8], self.modv[:, 8:16], 1.0, pp[:, l, 0:8], ALU.add, ALU.mult,
                     ["modv", "ppt"], ["AB"])
            self.stt("dve", self.AB[:, 8:16], self.modv[:, 32:40], 1.0, pp[:, l, 8:16], ALU.add, ALU.mult,
                     ["modv", "ppt"], ["AB"])
            if self.dbg:
                S.dma("sp", self.modd.ap(), self.modv[:], reads=["modv"], writes=["modd"])
            S.barrier()

    def modulate(self, xc, xk, h, hk, sq, sqk, rstd, rk, tmpring, psidx, N, Acol, Bcol):
        S = self.S
        ps = self.PS[psidx]
        pk = ("ps", psidx)
        for kt in range(8):
            self.act(sq[:, kt, :N], xc[:, kt, :N], AF.Square, [xk], [(sqk, kt)])
        self.mm([(ps[:, :N], self.ones_bf[:], sq[:, kt, :N], kt == 0, kt == 7) for kt in range(8)],
                ["ones_bf"] + [(sqk, kt) for kt in range(8)], [pk])
        self.ts("dve", rstd[:, :N], ps[:, :N], 1.0 / D, EPS, ALU.mult, ALU.add, [pk], [rk])
        self.act(rstd[:, :N], rstd[:, :N], AF.Sqrt, [rk], [rk])
        self.S.op("dve", lambda e: e.reciprocal(rstd[:, :N], rstd[:, :N]), [rk], [rk])
        for kt in range(8):
            tmp, tk = tmpring.next()
            self.stt("dve", tmp[:, :N], xc[:, kt, :N], self.AB[:, Acol + kt:Acol + kt + 1], rstd[:, :N],
                     ALU.mult, ALU.mult, [xk, "AB", rk], [tk])
            self.ts("pool", h[:, kt, :N], tmp[:, :N], self.modv[:, Bcol + kt:Bcol + kt + 1], None, ALU.add, None,
                    [tk, "modv"], [(hk, kt)])

    def phaseA(self, l, src):
        nc, S = self.nc, self.S
        with nc.sbuf_tensor(self.u("win"), [128, 8, NCOL], BF16) as win, \
                nc.sbuf_tensor(self.u("xa0"), [128, 8, 512], F32) as xa0, \
                nc.sbuf_tensor(self.u("xa1"), [128, 8, 512], F32) as xa1, \
                nc.sbuf_tensor(self.u("ha"), [128, 8, 512], BF16) as ha, \
                nc.sbuf_tensor(self.u("sqa"), [128, 8, 512], BF16) as sqa, \
                nc.sbuf_tensor(self.u("rstda"), [128, 512], F32) as rstda, \
                nc.sbuf_tensor(self.u("tmpa0"), [128, 512], F32) as tmpa0, \
                nc.sbuf_tensor(self.u("tmpa1"), [128, 512], F32) as tmpa1, \
                nc.sbuf_tensor(self.u("sta0"), [128, 512], F32) as sta0, \
                nc.sbuf_tensor(self.u("sta1"), [128, 512], F32) as sta1, \
                nc.sbuf_tensor(self.u("sta2"), [128, 512], F32) as sta2, \
                nc.sbuf_tensor(self.u("sta3"), [128, 512], F32) as sta3, \
                nc.sbuf_tensor(self.u("stb0"), [128, 512], BF16) as stb0, \
                nc.sbuf_tensor(self.u("stb1"), [128, 512], BF16) as stb1:
            for kt in range(8):
                S.dma("pool", win[:, kt, :], self.w_in[l, kt * 128:(kt + 1) * 128, :], writes=[("win", kt)])
            wink = [("win", kt) for kt in range(8)]
            xring = Ring("xa", [xa0, xa1])
            tmpring = Ring("tmpa", [tmpa0, tmpa1])
            string = Ring("sta", [sta0, sta1, sta2, sta3])
            stbring = Ring("stb", [stb0, stb1])
            srcv = src.ap().rearrange("(kt p) t -> p kt t", p=128)
            psi = 0
            ev = 0
            for c in range(8):
                xc, xk = xring.next()
                S.dma("sp", xc[:], srcv[:, :, c * 512:(c + 1) * 512], writes=[xk])
                self.modulate(xc, xk, ha, "ha", sqa, "sqa", rstda, "rstda", tmpring, 7, 512, 0, 0)
                hks = [("ha", kt) for kt in range(8)]
                for ft in range(NFM):
                    ps = self.PS[psi]
                    pk = ("ps", psi)
                    psi = (psi + 1) % 6
                    self.mm([(ps[:], win[:, kt, ft * 128:(ft + 1) * 128], ha[:, kt, :], kt == 0, kt == 7)
                             for kt in range(8)], hks + wink, [pk])
                    st, sk = string.next()
                    self.cp("act" if ev % 2 == 0 else "dve", st[:], ps[:], [pk], [sk])
                    ev += 1
                    S.dma("sp", self.zfm[ft * 128:(ft + 1) * 128, c * 512:(c + 1) * 512], st[:],
                          reads=[sk], writes=[("zfm", ft, c)])
                for tt in range(4):
                    for half in range(2):
                        ps = self.PS[psi]
                        pk = ("ps", psi)
                        psi = (psi + 1) % 6
                        c0 = NFM * 128 + half * 512
                        self.mm([(ps[:], ha[:, kt, tt * 128:(tt + 1) * 128], win[:, kt, c0:c0 + 512], kt == 0, kt == 7)
                                 for kt in range(8)], hks + wink, [pk])
                        st, sk = stbring.next()
                        self.cp("act" if ev % 2 == 0 else "dve", st[:], ps[:], [pk], [sk])
                        ev += 1
                        r0 = c * 512 + tt * 128
                        S.dma("sp", self.ztm[r0:r0 + 128, half * 512:(half + 1) * 512], st[:],
                              reads=[sk], writes=[("ztm", c, tt, half)])
            S.barrier()

    def phaseB(self, l):
        S = self.S
        self.setupB(l)
        S.barrier()
        if "h" in self.mixers:
            self.gla(l, "hgrn")
            S.barrier()
        if "m" in self.mixers:
            self.gla(l, "mlstm")
            S.barrier()
        if "f" in self.mixers:
            self.attn(l, "fox")
            S.barrier()
        if "d" in self.mixers:
            self.attn(l, "diff")
            S.barrier()

    def setupB(self, l):
        nc, S = self.nc, self.S
        if not hasattr(self, "sv"):
            self.sv = self.sb("sv", [128, 16], F32)
            self.lamrow = self.sb("lamrow", [1, 256], F32)
            self.negpp = self.sb("negpp", [128, NPP], F32)
        sv, pp = self.sv, self.ppt
        self.ts("dve", self.negpp[:], pp[:, l, :], -1.0, None, ALU.mult, None, ["ppt"], ["negpp"])
        self.tt("dve", sv[:, 0:2], pp[:, l, 18:20], pp[:, l, 16:18], ALU.subtract, ["ppt"], ["sv"])
        self.act(sv[:, 0:2], sv[:, 0:2], AF.Exp, ["sv"], ["sv"], scale=-1.0)
        self.ts("dve", sv[:, 0:2], sv[:, 0:2], 1.0, None, ALU.add, None, ["sv"], ["sv"])
        S.op("dve", lambda e: e.reciprocal(sv[:, 0:2], sv[:, 0:2]), ["sv"], ["sv"])
        self.ts("dve", sv[:, 0:2], sv[:, 0:2], float(l), None, ALU.mult, None, ["sv"], ["sv"])
        self.ts("dve", sv[:, 2:4], sv[:, 0:2], -1.0, 1.0, ALU.mult, ALU.add, ["sv"], ["sv"])
        lr = self.lamrow
        S.dma("sp", lr[0:1, 0:128], self.lam[l:l + 1, :], writes=["lamrow"])
        self.tt("dve", lr[0:1, 128:160], lr[0:1, 0:32], lr[0:1, 32:64], ALU.mult, ["lamrow"], ["lamrow"])
        self.tt("dve", lr[0:1, 160:192], lr[0:1, 64:96], lr[0:1, 96:128], ALU.mult, ["lamrow"], ["lamrow"])
        S.op("dve", lambda e: e.tensor_reduce(lr[0:1, 192:193], lr[0:1, 128:160], AX.X, ALU.add), ["lamrow"], ["lamrow"])
        S.op("dve", lambda e: e.tensor_reduce(lr[0:1, 193:194], lr[0:1, 160:192], AX.X, ALU.add), ["lamrow"], ["lamrow"])
        self.act(lr[0:1, 192:194], lr[0:1, 192:194], AF.Exp, ["lamrow"], ["lamrow"])
        import math
        lam_init = 0.8 - 0.6 * math.exp(-0.3 * l)
        self.lam_init = lam_init
        self.tt("dve", lr[0:1, 194:195], lr[0:1, 193:194], lr[0:1, 192:193], ALU.subtract, ["lamrow"], ["lamrow"])
        self.ts("dve", lr[0:1, 194:195], lr[0:1, 194:195], -lam_init, None, ALU.add, None, ["lamrow"], ["lamrow"])
        ps = self.PS[0]
        self.mm([(ps[:, 0:1], self.ones_f[0:1, 0:128], lr[0:1, 194:195], True, True)], ["lamrow", "ones_f"], [("ps", 0)])
        self.cp("dve", sv[:, 4:5], ps[:, 0:1], [("ps", 0)], ["sv"])

    def sigmoid_inplace(self, t, key):
        self.act(t, t, AF.Exp, [key], [key], scale=-1.0)
        self.ts("dve", t, t, 1.0, None, ALU.add, None, [key], [key])
        self.S.op("dve", lambda e: e.reciprocal(t, t), [key], [key])

    def rstd_from_ps(self, out, ps, pk, key, mult, add):
        self.ts("dve", out, ps, mult, add, ALU.mult, ALU.add, [pk], [key])
        self.act(out, out, AF.Sqrt, [key], [key])
        self.S.op("dve", lambda e: e.reciprocal(out, out), [key], [key])

    def attn(self, l, kind):
        nc, S = self.nc, self.S
        fox = kind == "fox"
        pp, sv = self.ppt, self.sv
        with ExitStack() as es:
            T = lambda nm, sh, dt_: es.enter_context(nc.sbuf_tensor(self.u(nm), sh, dt_))
            qraw = T("qraw", [128, SEQ], F32)
            kraw = T("kraw", [128, SEQ], F32)
            qp = T("qp", [128, SEQ], BF16)
            kp = T("kp", [128, SEQ], BF16)
            Vp = T("Vp", [128, 32, 128], BF16)
            negb = T("negb", [128, 32], F32)
            ones4k = T("ones4k", [128, SEQ], F32)
            sqt = T("sqt", [128, 512], BF16)
            rst = T("rst", [128, 512], F32)
            pT0 = T("pT0", [128, 512], BF16)
            pT1 = T("pT1", [128, 512], BF16)
            pT2 = T("pT2", [128, 512], BF16)
            pT3 = T("pT3", [128, 512], BF16)
            rrow = T("rrow", [128, 2, 512], F32)
            bcs = T("bcs", [128, 2, 512], F32)
            osb = T("osb", [128, 512], F32)
            osb2 = T("osb2", [128, 512], F32)
            gch = T("gch", [128, 512], F32)
            yst = T("yst", [128, 512], BF16)
            if fox:
                S.op("pool", lambda e: e.memset(ones4k[:], 1.0), writes=["ones4k"])
            pring = Ring("pT", [pT0, pT1, pT2, pT3])
            sring = Ring("pss", [self.PS[0], self.PS[1], self.PS[2], self.PS[3]])
            for h in range(4):
                par = h % 2
                pb = 64 * par
                M = 128
                srow = 64 if par == 0 else 0
                voff = 0 if par == 0 else 64
                tq = (T_FQ if fox else T_DQ) + h
                tk = (T_FK if fox else T_DK) + h
                S.dma("sp", qraw[:], self.zfm[tq * 128:(tq + 1) * 128, :], writes=["qraw"])
                S.dma("sp", kraw[:], self.zfm[tk * 128:(tk + 1) * 128, :], writes=["kraw"])
                S.op("pool", lambda e: e.memset(Vp[:], 0.0), writes=["Vp"])
                vcol = (V_F if fox else V_D) + 64 * h
                S.dma("sp", Vp[:, :, voff:voff + 64],
                      self.ztm[:, vcol:vcol + 64].rearrange("(j p) c -> p j c", p=128), reads=["Vp"], writes=["Vp"])
                S.op("pool", lambda e, c1=srow: e.memset(Vp[:, :, c1:c1 + 64], 1.0), reads=["Vp"], writes=["Vp"])
                if fox:
                    ranges = [(0, 64)]
                    nd = 64.0
                    gq, gk = pp[:, l, 24:25], pp[:, l, 25:26]
                    lhs_n = self.ones_bf[0:64, 0:64]
                    nrows = 64
                else:
                    ranges = [(0, 32), (64, 96)]
                    nd = 32.0
                    gq, gk = pp[:, l, 21:22], pp[:, l, 22:23]
                    lhs_n = self.blk32d_bf
                    nrows = 128
                for c in range(8):
                    cs = slice(c * 512, (c + 1) * 512)
                    for (raw, rk_, dstt, dk_, g, isq) in ((qraw, "qraw", qp, "qp", gq, True), (kraw, "kraw", kp, "kp", gk, False)):
                        self.act(sqt[0:nrows, :], raw[0:nrows, cs], AF.Square, [rk_], ["sqt"])
                        ps = self.PS[6]
                        self.mm([(ps[0:nrows, :], lhs_n, sqt[0:nrows, :], True, True)], ["sqt", "cbf", "ones_bf"], [("ps", 6)])
                        if isq:
                            self.rstd_from_ps(rst[0:nrows, :], ps[0:nrows, :], ("ps", 6), "rst", 1.0, nd * EPS)
                        else:
                            self.rstd_from_ps(rst[0:nrows, :], ps[0:nrows, :], ("ps", 6), "rst", 1.0 / nd, EPS)
                        for (a, b) in ranges:
                            self.stt("dve", dstt[a:b, cs], raw[a:b, cs], g[a:b, :], rst[a:b, :], ALU.mult, ALU.mult,
                                     [rk_, "rst", "ppt"], [dk_])
                if fox:
                    fr = qraw[64:65, :]
                    self.act(fr, fr, AF.Exp, ["qraw"], ["qraw"], scale=-1.0, bias=self.negpp[64:65, 26 + h:27 + h])
                    self.act(fr, fr, AF.Ln, ["qraw"], ["qraw"], bias=1.0)
                    S.op("dve", lambda e: e.tensor_tensor_scan(fr, ones4k[64:65, :], fr, 0.0, ALU.mult, ALU.subtract),
                         ["qraw", "ones4k"], ["qraw"])
                    self.cp("dve", qp[64:65, :], fr, ["qraw"], ["qp"])
                    S.op("pool", lambda e: e.memset(kp[64:65, :], 1.0), writes=["kp"])
                    ps = self.PS[7]
                    S.pe_group([lambda e, j=j, ps=ps: e.transpose(ps[:, j:j + 1], qraw[64:65, j * 128:(j + 1) * 128],
                                                           self.ident_f[64:65, 64:65]) for j in range(32)],
                               ["qraw", "cf"], [("ps", 7)])
                    self.ts("dve", negb[:], ps[:, 0:32], -1.0, None, ALU.mult, None, [("ps", 7)], ["negb"])
                    KR = [(0, 65)]
                else:
                    for r0 in (32, 96):
                        S.dma("pool", qp[r0:r0 + 1, :], self.crow[h:h + 1, :], reads=["qp"], writes=["qp"])
                        S.dma("pool", kp[r0:r0 + 1, :], self.onesrow[0:1, :], reads=["kp"], writes=["kp"])
                    S.dma("sp", negb[:], self.alibi[:, h * 32:(h + 1) * 32], writes=["negb"])
                    KR = [(0, 33), (64, 97)]
                nh = len(KR)
                ops_ = [self.PS[4], self.PS[5]]
                opk = [("ps", 4), ("ps", 5)]
                for c in range(8):
                    nj = 4 * c + 4
                    for j in range(nj):
                        dj = j - 4 * c
                        n0 = 128 * dj if dj > 0 else 0
                        for a in range(nh):
                            k0, k1 = KR[a]
                            ps_s, sk_ = sring.next()
                            items = [(ps_s[:, n0:512], kp[k0:k1, j * 128:(j + 1) * 128],
                                      qp[k0:k1, c * 512 + n0:(c + 1) * 512], True, dj < 0)]
                            if dj >= 0:
                                items.append((ps_s[:, n0:n0 + 128], self.ident_bf, self.trimask_bf, False, True))
                            self.mm(items, ["kp", "qp", "cbf"], [sk_])
                            pT, pk_ = pring.next()
                            self.act(pT[:, n0:512], ps_s[:, n0:512], AF.Exp, [sk_, "negb"], [pk_], bias=negb[:, j:j + 1])
                            self.mm([(ops_[a][0:M, n0:512], Vp[:, j, 0:M], pT[:, n0:512], j == 0, j == nj - 1)],
                                    ["Vp", pk_], [opk[a]])
                    cs = slice(c * 512, (c + 1) * 512)
                    for a in range(nh):
                        S.op("dve", lambda e, a=a, srow=srow: e.reciprocal(rrow[srow:srow + 64, a, :], ops_[a][srow:srow + 64, :]),
                             [opk[a]], [("rrow", a)])
                        psb = self.PS[6 + a]
                        self.mm([(psb[:, :], self.ones_f[srow:srow + 1, 0:128], rrow[srow:srow + 1, a, :], True, True)],
                                [("rrow", a), "ones_f"], [("ps", 6 + a)])
                        self.cp("act", bcs[pb:pb + 64, a, :], psb[pb:pb + 64, :], [("ps", 6 + a)], [("bcs", a)])
                    rows = slice(pb, pb + 64)
                    if self.dbg and fox and h == 0 and c == 0 and False:
                        self.cp("dve", osb2[:], ops_[0][:], [opk[0]], ["osb2"])
                        self.dump("ops", osb2[:], "osb2", [128, 512])
                        self.dump("rrow", rrow[:, 0, :], ("rrow", 0), [128, 512])
                        self.dump("bcs", bcs[:, 0, :], ("bcs", 0), [128, 512])
                        self.dump("Vp", Vp[:, 0, :], "Vp", [128, 128], BF16)
                        self.dump("negb", negb[:], "negb", [128, 32])
                        self.dump("qp", qp[:, 0:512], "qp", [128, 512], BF16)
                        self.dump("kp", kp[:, 0:512], "kp", [128, 512], BF16)
                    if fox:
                        tg = T_FG + h // 2
                        S.dma("sp", gch[rows, :], self.zfm[tg * 128 + pb:tg * 128 + pb + 64, cs], writes=["gch"])
                        self.sigmoid_inplace(gch[rows, :], "gch")
                        self.tt("dve", osb[rows, :], ops_[0][rows, :], bcs[rows, 0, :], ALU.mult, [opk[0], ("bcs", 0)], ["osb"])
                        self.tt("pool", yst[rows, :], osb[rows, :], gch[rows, :], ALU.mult, ["osb", "gch"], ["yst"])
                        S.dma("sp", self.ycat[512 + 64 * h:512 + 64 * h + 64, cs], yst[rows, :], reads=["yst"],
                              writes=[("ycat", kind, h, c)])
                    else:
                        self.tt("dve", osb[rows, :], ops_[0][rows, :], bcs[rows, 0, :], ALU.mult, [opk[0], ("bcs", 0)], ["osb"])
                        self.tt("dve", osb2[rows, :], ops_[1][rows, :], bcs[rows, 1, :], ALU.mult, [opk[1], ("bcs", 1)], ["osb2"])
                        self.stt("dve", osb[rows, :], osb2[rows, :], sv[rows, 4:5], osb[rows, :], ALU.mult, ALU.add,
                                 ["osb", "osb2", "sv"], ["osb"])
                        self.act(sqt[rows, :], osb[rows, :], AF.Square, ["osb"], ["sqt"])
                        ps = self.PS[6]
                        self.mm([(ps[:, :], self.blk64_bf[rows, :], sqt[rows, :], True, True)], ["sqt", "cbf"], [("ps", 6)])
                        self.rstd_from_ps(rst[rows, :], ps[rows, :], ("ps", 6), "rst", 1.0 / 64.0, EPS)
                        self.stt("dve", osb[rows, :], osb[rows, :], pp[rows, l, 23:24], rst[rows, :], ALU.mult, ALU.mult,
                                 ["osb", "rst", "ppt"], ["osb"])
                        self.act(yst[rows, :], osb[rows, :], AF.Copy, ["osb"], ["yst"], scale=float(1.0 - self.lam_init))
                        S.dma("sp", self.ycat[256 + 64 * h:256 + 64 * h + 64, cs], yst[rows, :], reads=["yst"],
                              writes=[("ycat", kind, h, c)])

    def gla(self, l, kind):
        nc, S = self.nc, self.S
        hg = kind == "hgrn"
        pp, sv = self.ppt, self.sv
        NV = 128 if hg else 256
        CL = 40.0
        with ExitStack() as es:
            T = lambda nm, sh, dt_: es.enter_context(nc.sbuf_tensor(self.u(nm), sh, dt_))
            B3 = T("B3", [128, 64, 64], F32)
            R3 = T("R3", [128, 64, 64], F32)
            qx = T("qx", [128, SEQ], F32)
            kx = T("kx", [128, SEQ], F32)
            tmp = T("tmp", [128, SEQ], F32)
            ones4k = T("ones4k", [128, SEQ], F32)
            Qs = T("Qs", [128, SEQ], BF16)
            Qb = T("Qb", [128, SEQ], BF16)
            Kb = T("Kb", [128, SEQ], BF16)
            Q32 = T("Q32", [128, SEQ], BF16)
            K32 = T("K32", [128, SEQ], BF16)
            K2 = T("K2", [128, SEQ], BF16)
            K2tm = T("K2tm", [128, 32, 128], BF16)
            Vx = T("Vx", [128, 32, NV], BF16)
            sm = T("sm", [128, 6, 64], F32)
            sm32 = T("sm32", [128, 128], F32)
            St = T("St", [128, NV], F32)
            Sb0 = T("Sb0", [128, NV], BF16)
            Sb1 = T("Sb1", [128, NV], BF16)
            Sb2 = T("Sb2", [128, NV], BF16)
            At0 = T("At0", [128, 512], BF16)
            At1 = T("At1", [128, 512], BF16)
            osb = T("osb", [128, 512], F32)
            dsb = T("dsb", [128, 512], F32)
            sqt = T("sqt", [128, 512], BF16)
            rst = T("rst", [128, 512], F32)
            gch = T("gch", [128, 512], F32)
            gch2 = T("gch2", [128, 512], F32)
            yst = T("yst", [128, 512], BF16)
            B2 = B3[:].rearrange("p a b -> p (a b)")
            R2 = R3[:].rearrange("p a b -> p (a b)")
            B4 = B3[:].rearrange("p a (h b) -> p (a h) b", h=2)
            R4 = R3[:].rearrange("p a (h b) -> p (a h) b", h=2)
            S.op("pool", lambda e: e.memset(ones4k[:], 1.0), writes=["ones4k"])
            bprev, blast, b31, dch = sm[:, 0, :], sm[:, 1, :], sm[:, 2, :], sm[:, 3, :]
            mask4 = self.mask4_f
            sbring = Ring("Sb", [Sb0, Sb1, Sb2])
            atring = Ring("At", [At0, At1])

            def bc64(i):
                return sm[:, i, :].unsqueeze(2).to_broadcast([128, 64, 64])

            for tp in range(2):
                if hg:
                    tq, tf = T_HQ + tp, T_HF + tp
                    S.dma("sp", tmp[:], self.zfm[tf * 128:(tf + 1) * 128, :], writes=["tmp"])
                    S.dma("sp", qx[:], self.zfm[tq * 128:(tq + 1) * 128, :], writes=["qx"])
                    self.sigmoid_inplace(tmp[:], "tmp")
                    self.ts("dve", kx[:], tmp[:], sv[:, 2 + tp:3 + tp], sv[:, tp:tp + 1], ALU.mult, ALU.add,
                            ["tmp", "sv"], ["kx"])
                    self.act(B2, kx[:], AF.Ln, ["kx"], ["B"])
                    self.ts("dve", kx[:], kx[:], -1.0, 1.0, ALU.mult, ALU.add, ["kx"], ["kx"])
                    S.op("dve", lambda e: e.tensor_tensor_scan(B2, ones4k[:], B2, 0.0, ALU.mult, ALU.add),
                         ["B", "ones4k"], ["B"])
                    qscale = 1.0
                else:
                    for (tsrc, dst_, dk_, cb) in ((T_MQ + tp, qx, "qx", 34 + tp * 4), (T_MK + tp, kx, "kx", 42 + tp * 4)):
                        S.dma("sp", R2, self.zfm[tsrc * 128:(tsrc + 1) * 128, :], writes=["R"])
                        self.ts("dve", tmp[:], R2, pp[:, l, cb + 3:cb + 4], None, ALU.mult, None, ["R", "ppt"], ["tmp"])
                        for j in (2, 1, 0):
                            sh = 3 - j
                            self.stt("dve", tmp[:, sh:], R2[:, 0:SEQ - sh], pp[:, l, cb + j:cb + j + 1], tmp[:, sh:],
                                     ALU.mult, ALU.add, ["R", "tmp", "ppt"], ["tmp"])
                        self.act(B2, tmp[:], AF.Exp, ["tmp"], ["B"], scale=-1.0)
                        self.ts("dve", B2, B2, 1.0, None, ALU.add, None, ["B"], ["B"])
                        S.op("dve", lambda e: e.reciprocal(B2, B2), ["B"], ["B"])
                        self.tt("dve", dst_[:], tmp[:], B2, ALU.mult, ["tmp", "B"], [dk_])
                    tf, ti = T_MF + tp, T_MI + tp
                    S.dma("sp", B2, self.zfm[tf * 128:(tf + 1) * 128, :], reads=["B"], writes=["B"])
                    S.dma("sp", tmp[:], self.zfm[ti * 128:(ti + 1) * 128, :], reads=["tmp"], writes=["tmp"])
                    self.act(B2, B2, AF.Exp, ["B", "negpp"], ["B"], scale=-1.0, bias=self.negpp[:, 32 + tp:33 + tp])
                    self.act(B2, B2, AF.Ln, ["B"], ["B"], bias=1.0)
                    S.op("dve", lambda e: e.tensor_tensor_scan(B2, ones4k[:], B2, 0.0, ALU.mult, ALU.subtract),
                         ["B", "ones4k"], ["B"])
                    self.ts("dve", tmp[:], tmp[:], pp[:, l, 30 + tp:31 + tp], None, ALU.add, None, ["tmp", "ppt"], ["tmp"])
                    qscale = 0.125
                self.cp("dve", blast, B3[:, :, 63], ["B"], ["sm"])
                self.cp("dve", b31, B3[:, :, 31], ["B"], ["sm"])
                S.op("dve", lambda e: e.memset(sm[:, 0, 0:1], 0.0), ["sm"], ["sm"])
                self.cp("dve", bprev[:, 1:64], blast[:, 0:63], ["sm"], ["sm"])
                self.cp("dve", sm32[:], B4[:, :, 15], ["B"], ["sm32"])
                self.tt("dve", dch, blast, bprev, ALU.subtract, ["sm"], ["sm"])
                self.act(dch, dch, AF.Exp, ["sm"], ["sm"])
                bc32 = sm32[:].unsqueeze(2).to_broadcast([128, 128, 32])

                def emit(dst_, dk_, src, sk_, with_i, scale):
                    if with_i:
                        self.tt("dve", R2, R2, tmp[:], ALU.add, ["R", "tmp"], ["R"])
                    self.act(R2, R2, AF.Exp, ["R"], ["R"])
                    if scale == 1.0:
                        self.tt("dve", dst_[:], src[:], R2, ALU.mult, [sk_, "R"], [dk_])
                    else:
                        self.stt("dve", dst_[:], src[:], scale, R2, ALU.mult, ALU.mult, [sk_, "R"], [dk_])

                wi = not hg
                self.tt("dve", R3[:], B3[:], bc64(0), ALU.subtract, ["B", "sm"], ["R"])
                emit(Qs, "Qs", qx, "qx", False, qscale)
                self.tt("dve", R3[:], B3[:], bc64(1), ALU.subtract, ["B", "sm"], ["R"])
                self.ts("dve", R2, R2, -1.0, None, ALU.mult, None, ["R"], ["R"])
                emit(K2, "K2", kx, "kx", wi, 1.0)
                self.tt("dve", R3[:], B3[:], bc64(2), ALU.subtract, ["B", "sm"], ["R"])
                self.ts("dve", R2, R2, 0.0, None, ALU.min, None, ["R"], ["R"])
                emit(Qb, "Qb", qx, "qx", False, qscale)
                self.tt("dve", R3[:], B3[:], bc64(2), ALU.subtract, ["B", "sm"], ["R"])
                self.ts("dve", R2, R2, -1.0, 0.0, ALU.mult, ALU.min, ["R"], ["R"])
                emit(Kb, "Kb", kx, "kx", wi, 1.0)
                self.tt("dve", R4, B4, bc32, ALU.subtract, ["B", "sm32"], ["R"])
                self.ts("dve", R2, R2, CL, -CL, ALU.min, ALU.max, ["R"], ["R"])
                emit(Q32, "Q32", qx, "qx", False, qscale)
                self.tt("dve", R4, B4, bc32, ALU.subtract, ["B", "sm32"], ["R"])
                self.ts("dve", R2, R2, -1.0, CL, ALU.mult, ALU.min, ["R"], ["R"])
                self.ts("dve", R2, R2, -CL, None, ALU.max, None, ["R"], ["R"])
                emit(K32, "K32", kx, "kx", wi, 1.0)
                psT = self.PS[7][:, 0:64].bitcast(BF16)
                for m in range(32):
                    S.pe_group([lambda e, m=m: e.transpose(psT, K2[:, m * 128:(m + 1) * 128], self.ident_bf)],
                               ["K2", "cbf"], [("ps", 7)])
                    self.cp("act" if m % 2 == 0 else "dve", K2tm[:, m, :], psT, [("ps", 7)], [("K2tm", m)])
                voff = (V_H if hg else V_M) + tp * 128
                S.dma("sp", Vx[:, :, 0:128], self.ztm[:, voff:voff + 128].rearrange("(j p) c -> p j c", p=128),
                      writes=["Vx"])
                if not hg:
                    S.op("pool", lambda e: e.memset(Vx[:, :, 128:256], 1.0), reads=["Vx"], writes=["Vx"])
                S.op("pool", lambda e: e.memset(St[:], 0.0), writes=["St"])
                sbt, sbk = sbring.next()
                S.op("pool", lambda e, sbt=sbt: e.memset(sbt[:], 0.0), writes=[sbk])
                NE, NO, DE, DO = self.PS[0], self.PS[1], self.PS[2], self.PS[3]
                allps = [("ps", 0), ("ps", 1), ("ps", 2), ("ps", 3)]
                for m in range(32):
                    cm = (m % 4) * 128
                    ms = slice(m * 128, (m + 1) * 128)
                    psa, psa2 = self.PS[4], self.PS[6]
                    self.mm([(psa[:, 0:128], K32[0:64, ms], Q32[0:64, ms], True, True)], ["K32", "Q32"], [("ps", 4)])
                    self.mm([(psa2[:, 0:128], K32[64:128, ms], Q32[64:128, ms], True, True)], ["K32", "Q32"], [("ps", 6)])
                    self.mm([(psa[:, 128:256], Kb[0:64, ms], Qb[0:64, ms], True, True)], ["Kb", "Qb"], [("ps", 4)])
                    self.mm([(psa2[:, 128:256], Kb[64:128, ms], Qb[64:128, ms], True, True)], ["Kb", "Qb"], [("ps", 6)])
                    At, ak = atring.next()
                    self.tt("dve", At[:, 0:256], psa[:, 0:256], mask4[:, 0:256], ALU.mult, [("ps", 4), "cf2"], [ak])
                    self.tt("dve", At[:, 256:512], psa2[:, 0:256], mask4[:, 0:256], ALU.mult, [("ps", 6), "cf2", ak], [ak])
                    items = [(NE[:, cm:cm + 128], Vx[:, m, 0:128], At[:, 0:128], True, False),
                             (NE[:, cm:cm + 128], Vx[:, m, 0:128], At[:, 128:256], False, False),
                             (NO[:, cm:cm + 128], Vx[:, m, 0:128], At[:, 256:384], True, False),
                             (NO[:, cm:cm + 128], Vx[:, m, 0:128], At[:, 384:512], False, False)]
                    if not hg:
                        items += [(DE[:, cm:cm + 128], Vx[:, m, 128:256], At[:, 0:128], True, False),
                                  (DE[:, cm:cm + 128], Vx[:, m, 128:256], At[:, 128:256], False, False),
                                  (DO[:, cm:cm + 128], Vx[:, m, 128:256], At[:, 256:384], True, False),
                                  (DO[:, cm:cm + 128], Vx[:, m, 128:256], At[:, 384:512], False, False)]
                    self.mm(items, ["Vx", ak], allps)
                    for r in range(2):
                        n = 2 * m + r
                        ns = slice(n * 64, (n + 1) * 64)
                        cn = cm + 64 * r
                        items = [(NE[:, cn:cn + 64], sbt[0:64, 0:128], Qs[0:64, ns], False, True),
                                 (NO[:, cn:cn + 64], sbt[64:128, 0:128], Qs[64:128, ns], False, True)]
                        if not hg:
                            items += [(DE[:, cn:cn + 64], sbt[0:64, 128:256], Qs[0:64, ns], False, True),
                                      (DO[:, cn:cn + 64], sbt[64:128, 128:256], Qs[64:128, ns], False, True)]
                        self.mm(items, [sbk, "Qs"], allps)
                        psd = self.PS[5]
                        self.mm([(psd[:, 0:NV], K2tm[64 * r:64 * r + 64, m, :], Vx[64 * r:64 * r + 64, m, 0:NV], True, True)],
                                [("K2tm", m), "Vx"], [("ps", 5)])
                        self.ts("dve", St[:], St[:], dch[:, n:n + 1], None, ALU.mult, None, ["St", "sm"], ["St"])
                        self.tt("dve", St[:], psd[:, 0:NV], St[:], ALU.add, ["St", ("ps", 5)], ["St"])
                        if n < 63:
                            sbt, sbk = sbring.next()
                            self.cp("pool", sbt[:], St[:], ["St"], [sbk])
                    if m % 4 == 3:
                        c = m // 4
                        cs = slice(c * 512, (c + 1) * 512)
                        tg = (T_HG if hg else T_MO) + tp
                        S.dma("sp", gch[:], self.zfm[tg * 128:(tg + 1) * 128, cs], writes=["gch"])
                        if hg:
                            self.cp("act", osb[0:64, :], NE[0:64, :], [("ps", 0)], ["osb"])
                            self.cp("act", osb[64:128, :], NO[64:128, :], [("ps", 1), "osb"], ["osb"])
                            self.act(sqt[:], osb[:], AF.Square, ["osb"], ["sqt"])
                            ps = self.PS[7]
                            self.mm([(ps[:], self.blk64_bf, sqt[:], True, True)], ["sqt", "cbf"], [("ps", 7)])
                            self.rstd_from_ps(rst[:], ps[:], ("ps", 7), "rst", 1.0 / 64.0, EPS)
                            self.stt("dve", osb[:], osb[:], pp[:, l, 20:21], rst[:], ALU.mult, ALU.mult,
                                     ["osb", "rst", "ppt"], ["osb"])
                            self.cp("pool", gch2[:], gch[:], ["gch"], ["gch2"])
                            self.sigmoid_inplace(gch2[:], "gch2")
                            self.tt("pool", gch[:], gch[:], gch2[:], ALU.mult, ["gch", "gch2"], ["gch"])
                            self.tt("dve", yst[:], osb[:], gch[:], ALU.mult, ["osb", "gch"], ["yst"])
                            S.dma("sp", self.ycat[tp * 128:(tp + 1) * 128, cs], yst[:], reads=["yst"],
                                  writes=[("ycat", kind, tp, c)])
                        else:
                            self.cp("dve", dsb[0:64, :], DE[0:64, :], [("ps", 2)], ["dsb"])
                            self.cp("dve", dsb[64:128, :], DO[64:128, :], [("ps", 3), "dsb"], ["dsb"])
                            self.stt("dve", dsb[:], dsb[:], -1.0, dsb[:], ALU.mult, ALU.max, ["dsb"], ["dsb"])
                            self.ts("dve", dsb[:], dsb[:], 1.0, None, ALU.max, None, ["dsb"], ["dsb"])
                            S.op("dve", lambda e: e.reciprocal(dsb[:], dsb[:]), ["dsb"], ["dsb"])
                            self.tt("dve", osb[0:64, :], NE[0:64, :], dsb[0:64, :], ALU.mult, [("ps", 0), "dsb"], ["osb"])
                            self.tt("dve", osb[64:128, :], NO[64:128, :], dsb[64:128, :], ALU.mult, [("ps", 1), "dsb", "osb"], ["osb"])
                            self.sigmoid_inplace(gch[:], "gch")
                            self.tt("pool", yst[:], osb[:], gch[:], ALU.mult, ["osb", "gch"], ["yst"])
                            S.dma("sp", self.ycat[768 + tp * 128:768 + (tp + 1) * 128, cs], yst[:], reads=["yst"],
                                  writes=[("ycat", kind, tp, c)])

    def phaseCD(self, l, src, dst):
        nc, S = self.nc, self.S
        N = 256
        with nc.sbuf_tensor(self.u("wout"), [128, 8, D], BF16) as wout, \
                nc.sbuf_tensor(self.u("wff1"), [128, 8, 4 * D], BF16) as wff1, \
                nc.sbuf_tensor(self.u("wff2"), [128, 32, D], BF16) as wff2, \
                nc.sbuf_tensor(self.u("xc0"), [128, 8, N], F32) as xc0, \
                nc.sbuf_tensor(self.u("xc1"), [128, 8, N], F32) as xc1, \
                nc.sbuf_tensor(self.u("yc0"), [128, 8, N], BF16) as yc0, \
                nc.sbuf_tensor(self.u("yc1"), [128, 8, N], BF16) as yc1, \
                nc.sbuf_tensor(self.u("hc"), [128, 8, N], BF16) as hc, \
                nc.sbuf_tensor(self.u("sqc"), [128, 8, N], BF16) as sqc, \
                nc.sbuf_tensor(self.u("rstdc"), [128, N], F32) as rstdc, \
                nc.sbuf_tensor(self.u("tmpc0"), [128, N], F32) as tmpc0, \
                nc.sbuf_tensor(self.u("tmpc1"), [128, N], F32) as tmpc1, \
                nc.sbuf_tensor(self.u("rl0"), [128, N], F32) as rl0, \
                nc.sbuf_tensor(self.u("rl1"), [128, N], F32) as rl1, \
                nc.sbuf_tensor(self.u("rl2"), [128, N], F32) as rl2, \
                nc.sbuf_tensor(self.u("hid"), [128, 32, N], BF16) as hid:
            for kt in range(8):
                S.dma("pool", wout[:, kt, :], self.w_out[l, kt * 128:(kt + 1) * 128, :], writes=[("wout", kt)])
            for kt in range(8):
                S.dma("pool", wff1[:, kt, :], self.w_ff1[l, kt * 128:(kt + 1) * 128, :], writes=[("wff1", kt)])
            for ft in range(32):
                S.dma("pool", wff2[:, ft, :], self.w_ff2[l, ft * 128:(ft + 1) * 128, :], writes=[("wff2", ft)])
            woutk = [("wout", kt) for kt in range(8)]
            wff1k = [("wff1", kt) for kt in range(8)]
            wff2k = [("wff2", ft) for ft in range(32)]
            xring = Ring("xc", [xc0, xc1])
            yring = Ring("yc", [yc0, yc1])
            tmpring = Ring("tmpc", [tmpc0, tmpc1])
            rlring = Ring("rl", [rl0, rl1, rl2])
            srcv = src.ap().rearrange("(kt p) t -> p kt t", p=128)
            dstv = dst.ap().rearrange("(kt p) t -> p kt t", p=128)
            if self.dbg and "B" not in self.phases:
                ysrc = self.ycat_in.ap().rearrange("(kt p) t -> p kt t", p=128)
            else:
                ysrc = self.ycat.ap().rearrange("(kt p) t -> p kt t", p=128)
            psi = 0
            ev = 0
            for c in range(SEQ // N):
                xc, xk = xring.next()
                yc, yk = yring.next()
                S.dma("sp", xc[:], srcv[:, :, c * N:(c + 1) * N], writes=[xk])
                S.dma("pool", yc[:], ysrc[:, :, c * N:(c + 1) * N], writes=[yk])
                for ot in range(8):
                    ps = self.PS[psi]
                    pk = ("ps", psi)
                    psi = (psi + 1) % 6
                    self.mm([(ps[:, :N], wout[:, kt, ot * 128:(ot + 1) * 128], yc[:, kt, :], kt == 0, kt == 7)
                             for kt in range(8)], [yk] + woutk, [pk])
                    self.stt("dve", xc[:, ot, :], ps[:, :N], self.modv[:, 16 + ot:17 + ot], xc[:, ot, :],
                             ALU.mult, ALU.add, [pk, xk, "modv"], [xk])
                self.modulate(xc, xk, hc, "hc", sqc, "sqc", rstdc, "rstdc", tmpring, 7, N, 8, 24)
                hks = [("hc", kt) for kt in range(8)]
                for ft in range(32):
                    ps = self.PS[psi]
                    pk = ("ps", psi)
                    psi = (psi + 1) % 6
                    self.mm([(ps[:, :N], wff1[:, kt, ft * 128:(ft + 1) * 128], hc[:, kt, :], kt == 0, kt == 7)
                             for kt in range(8)], hks + wff1k, [pk])
                    rl, rk = rlring.next()
                    self.act(rl[:], ps[:, :N], AF.Relu, [pk], [rk])
                    self.tt("pool" if ev % 2 == 0 else "dve", hid[:, ft, :], rl[:], rl[:], ALU.mult, [rk], [("hid", ft)])
                    ev += 1
                hidk = [("hid", ft) for ft in range(32)]
                for ot in range(8):
                    ps = self.PS[psi]
                    pk = ("ps", psi)
                    psi = (psi + 1) % 6
                    self.mm([(ps[:, :N], wff2[:, ft, ot * 128:(ot + 1) * 128], hid[:, ft, :], ft == 0, ft == 31)
                             for ft in range(32)], hidk + wff2k, [pk])
                    self.stt("dve", xc[:, ot, :], ps[:, :N], self.modv[:, 40 + ot:41 + ot], xc[:, ot, :],
                             ALU.mult, ALU.add, [pk, xk, "modv"], [xk])
                S.dma("sp", dstv[:, :, c * N:(c + 1) * N], xc[:], reads=[xk], writes=[("dst", c)])
            S.barrier()


def _prep_inputs(inp):
    cols = _col_index()
    w_in = np.asarray(inp["w_in"], np.float32)
    w_in_p = np.zeros((L, D, NCOL), np.float32)
    valid = cols >= 0
    w_in_p[:, :, valid] = w_in[:, :, cols[valid]]
    cmat, alibi, crow = _host_consts()
    pp = np.stack([_pp_layer(inp, l) for l in range(L)], 0)
    lam = np.asarray(inp["diff_lambda"], np.float32).reshape(L, 128)
    shared = dict(
        w_ada=np.ascontiguousarray(inp["w_ada"], np.float32), b_ada=np.ascontiguousarray(inp["b_ada"], np.float32),
        w_in=w_in_p, w_out=np.ascontiguousarray(inp["w_out"], np.float32),
        w_ff1=np.ascontiguousarray(inp["w_ff1"], np.float32), w_ff2=np.ascontiguousarray(inp["w_ff2"], np.float32),
        pp=pp, lam=lam, cmat=cmat, alibi=alibi, crow=crow,
        onesrow=np.ones((1, SEQ), np.float32), mask4=_mask4())
    maps = []
    x = np.asarray(inp["x"], np.float32)
    c = np.asarray(inp["c"], np.float32)
    for b in range(x.shape[0]):
        m = dict(shared)
        m["xT"] = np.ascontiguousarray(x[b].T)
        m["c8"] = np.ascontiguousarray(c[b].reshape(8, 128).T)
        maps.append(m)
    return maps


def kernel(**inputs):
    inp = {k: np.asarray(v) for k, v in inputs.items()}
    maps = _prep_inputs(inp)
    mk = MK()
    res = run_bass_kernel_spmd(mk.nc, maps, core_ids=list(range(len(maps))))
    out = np.stack([np.ascontiguousarray(r["outT"].T) for r in res.results], 0)
    return out.astype(np.float32)
```

```python
import numpy as np
import math
from contextlib import ExitStack
import concourse.bass as bass
import concourse.mybir as mybir
from concourse.bass_utils import run_bass_kernel_spmd

F32 = mybir.dt.float32
BF16 = mybir.dt.bfloat16
AF = mybir.ActivationFunctionType
ALU = mybir.AluOpType
AX = mybir.AxisListType

ENGS = ("pe", "act", "dve", "pool", "sp")

D = 1024
SEQ = 4096
L = 2
NFM = 34
NTM = 1024
NCOL = NFM * 128 + NTM
EPS = 1e-6
NPP = 56
import os
GLA_STOP = int(os.environ.get('GLA_STOP', '0'))
GLA_SKIP = os.environ.get('GLA_SKIP', '')
WARM = int(os.environ.get('WARM', '0'))


class Sched:
    def __init__(self, nc, n_dma_sems=24):
        self.nc = nc
        self.streams = {e: [] for e in ENGS}
        self.esem = {e: nc.alloc_semaphore("s_" + e) for e in ENGS}
        self.cnt = {e: 0 for e in ENGS}
        self.seen = {e: {} for e in ENGS}
        self.lastw = {}
        self.readers = {}
        self.dsems = {}
        self.dcnt = {}
        self.dnext = {}
        for q in ("sp", "act", "pool"):
            self.dsems[q] = [nc.alloc_semaphore("d_%s%d" % (q, i)) for i in range(n_dma_sems)]
            self.dcnt[q] = [0] * n_dma_sems
            self.dnext[q] = 0
        self.semh = {}
        for e in ENGS:
            self.semh[("e", e)] = self.esem[e]
        for q in self.dsems:
            for i, s in enumerate(self.dsems[q]):
                self.semh[("d", q, i)] = s
        self.n_waits = 0
        self.n_ops = 0

    def _deps(self, reads, writes):
        deps = {}

        def add(tok):
            if tok is None:
                return
            k, v = tok
            if deps.get(k, 0) < v:
                deps[k] = v

        for r in reads:
            add(self.lastw.get(r))
        for r in writes:
            add(self.lastw.get(r))
            for t in self.readers.get(r, ()):
                add(t)
        return deps

    def _emit_waits(self, e, deps):
        seen = self.seen[e]
        for k, v in deps.items():
            if seen.get(k, 0) >= v:
                continue
            if e == "pe" and k == ("e", "pe"):
                continue
            seen[k] = v
            h = self.semh[k]
            self.streams[e].append(lambda eng, h=h, v=v: eng.wait_ge(h, v))
            self.n_waits += 1

    def _commit(self, tok, reads, writes):
        for r in reads:
            lst = self.readers.setdefault(r, [])
            lst.append(tok)
            if len(lst) > 64:
                m = {}
                for k, v in lst:
                    if m.get(k, 0) < v:
                        m[k] = v
                self.readers[r] = list(m.items())
        for r in writes:
            self.lastw[r] = tok
            self.readers[r] = []

    def op(self, e, fn, reads=(), writes=()):
        deps = self._deps(reads, writes)
        self._emit_waits(e, deps)
        self.cnt[e] += 1
        sem = self.esem[e]
        self.streams[e].append(lambda eng, fn=fn, sem=sem: fn(eng).then_inc(sem, 1))
        tok = (("e", e), self.cnt[e])
        self._commit(tok, reads, writes)
        self.n_ops += 1
        return tok

    def pe_group(self, fns, reads=(), writes=()):
        deps = self._deps(reads, writes)
        self._emit_waits("pe", deps)
        self.cnt["pe"] += 1
        sem = self.esem["pe"]
        for fn in fns[:-1]:
            self.streams["pe"].append(lambda eng, fn=fn: fn(eng))
        fn = fns[-1]
        self.streams["pe"].append(lambda eng, fn=fn, sem=sem: fn(eng).then_inc(sem, 1))
        tok = (("e", "pe"), self.cnt["pe"])
        self._commit(tok, reads, writes)
        self.n_ops += len(fns)
        return tok

    def dma(self, q, out, in_, reads=(), writes=(), **kw):
        i = self.dnext[q]
        self.dnext[q] = (i + 1) % len(self.dsems[q])
        k = ("d", q, i)
        deps = self._deps(reads, writes)
        if self.dcnt[q][i] > 0:
            deps[k] = max(deps.get(k, 0), self.dcnt[q][i])
        self._emit_waits(q, deps)
        self.dcnt[q][i] += 16
        h = self.semh[k]
        self.streams[q].append(
            lambda eng, out=out, in_=in_, h=h, kw=kw: eng.dma_start(out=out, in_=in_, **kw).then_inc(h, 16))
        tok = (k, self.dcnt[q][i])
        self._commit(tok, reads, writes)
        return tok

    def barrier(self):
        allk = {}
        for e in ENGS:
            if self.cnt[e] > 0:
                allk[("e", e)] = self.cnt[e]
        for q in self.dsems:
            for i, c in enumerate(self.dcnt[q]):
                if c > 0:
                    allk[("d", q, i)] = c
        for e in ENGS:
            self._emit_waits(e, dict(allk))
        self.lastw.clear()
        self.readers.clear()

    def finish(self):
        self.barrier()
        nc = self.nc
        streams = self.streams
        with nc.Block() as block:
            @block.tensor
            def _(eng):
                for f in streams["pe"]:
                    f(eng)

            @block.scalar
            def _(eng):
                for f in streams["act"]:
                    f(eng)

            @block.vector
            def _(eng):
                for f in streams["dve"]:
                    f(eng)

            @block.gpsimd
            def _(eng):
                for f in streams["pool"]:
                    f(eng)

            @block.sync
            def _(eng):
                for f in streams["sp"]:
                    f(eng)


class Ring:
    def __init__(self, name, bufs):
        self.name = name
        self.bufs = bufs
        self.i = 0

    def next(self):
        i = self.i
        self.i = (i + 1) % len(self.bufs)
        return self.bufs[i], (self.name, i)


OFF = dict(hq=0, hf=256, hi=512, hg=768, dq=1024, dk=1280, dv=1536, fq=1792, fk=2048, fv=2304,
           fg=2560, ff=2816, mq=2820, mk=3076, mv=3332, mo=3588, mi=3844, mf=3848)
T_HQ, T_HF, T_HG, T_MQ, T_MK, T_MO, T_MI, T_MF = 0, 2, 4, 6, 8, 10, 12, 14
T_FQ, T_FK, T_FG, T_DQ, T_DK = 16, 20, 24, 26, 30
V_H, V_M, V_F, V_D = 0, 256, 512, 768


def _col_index():
    cols = []

    def rng(base, n):
        return list(range(base, base + n))

    for nm in ("hq", "hf", "hg", "mq", "mk", "mo"):
        cols += rng(OFF[nm], 256)
    for nm in ("mi", "mf"):
        for h in range(4):
            cols += [OFF[nm] + h] * 64
    for h in range(4):
        cols += rng(OFF["fq"] + 64 * h, 64) + [OFF["ff"] + h] + [-1] * 63
    for h in range(4):
        cols += rng(OFF["fk"] + 64 * h, 64) + [-1] * 64
    cols += rng(OFF["fg"], 256)
    for nm in ("dq", "dk"):
        for h in range(4):
            b = OFF[nm] + 64 * h
            cols += rng(b, 32) + [-1] * 32 + rng(b + 32, 32) + [-1] * 32
    assert len(cols) == NFM * 128
    for nm in ("hi", "mv", "fv", "dv"):
        cols += rng(OFF[nm], 256)
    assert len(cols) == NCOL
    return np.array(cols)


def _host_consts():
    p = np.arange(128)
    c = {}
    c["ident"] = np.eye(128, dtype=np.float32)
    c["blk64"] = (p[:, None] // 64 == p[None, :] // 64).astype(np.float32)
    in1 = (p < 32)
    in2 = (p >= 64) & (p < 96)
    c["blk32d"] = ((in1[:, None] & in1[None, :]) | (in2[:, None] & in2[None, :])).astype(np.float32)
    c["trimask"] = np.where(p[:, None] <= p[None, :], 0.0, -30000.0).astype(np.float32)
    c["glamask"] = ((p[:, None] // 64 == p[None, :] // 64) & (p[:, None] <= p[None, :])).astype(np.float32)
    slopes = 2.0 ** (-8.0 * np.arange(1, 5, dtype=np.float64) / 4)
    pos = (np.arange(32)[None, :] * 128 + p[:, None]).astype(np.float64)
    c["alibi"] = np.stack([slopes[h] * pos for h in range(4)], axis=1).astype(np.float32)
    c["crow"] = np.stack([-slopes[h] * np.arange(SEQ, dtype=np.float64) for h in range(4)], 0).astype(np.float32)
    cmat = np.concatenate([c["ident"], c["blk64"], c["blk32d"], c["trimask"], c["glamask"]], axis=1)
    return cmat.astype(np.float32), c["alibi"].reshape(128, 128).copy(), c["crow"]


def _mask4():
    p = np.arange(128)
    s_, t_ = p[:, None], p[None, :]
    d32 = ((s_ // 32 == t_ // 32) & (s_ <= t_)).astype(np.float32)
    ba = ((s_ // 64 == t_ // 64) & (s_ % 64 < 32) & (t_ % 64 >= 32)).astype(np.float32)
    return np.ascontiguousarray(np.concatenate([d32, ba], axis=1))


def _pp_layer(inp, l):
    pp = np.zeros((128, NPP), np.float32)
    p = np.arange(128)
    pp[:, 0:8] = inp["norm_mix_gain"][l].reshape(8, 128).T
    pp[:, 8:16] = inp["norm_ff_gain"][l].reshape(8, 128).T
    pp[:, 16:18] = inp["hg_lb_logits"][0].reshape(2, 128).T
    pp[:, 18:20] = inp["hg_lb_logits"][1].reshape(2, 128).T
    pp[:, 20] = inp["hg_norm_gain"][l][p % 64]
    for col, nm in ((21, "diff_qn_gain"), (22, "diff_kn_gain")):
        g = inp[nm][l]
        pp[0:32, col] = g
        pp[64:96, col] = g
    pp[:, 23] = inp["diff_sub_gain"][l][p % 64]
    pp[:, 24] = inp["fox_qn_gain"][l][p % 64]
    pp[:, 25] = inp["fox_kn_gain"][l][p % 64]
    for h in range(4):
        pp[:, 26 + h] = inp["fox_f_bias"][l][h]
    for tp in range(2):
        pp[:, 30 + tp] = inp["mlstm_i_bias"][l][2 * tp + p // 64]
        pp[:, 32 + tp] = inp["mlstm_f_bias"][l][2 * tp + p // 64]
        for j in range(4):
            pp[:, 34 + tp * 4 + j] = inp["mlstm_conv"][l][j, tp * 128 + p]
            pp[:, 42 + tp * 4 + j] = inp["mlstm_conv"][l][j, 256 + tp * 128 + p]
    return pp


class MK:
    def __init__(self, dbg=False, phases="0ABCD", nlayers=L, mixers="hmfd"):
        self.mixers = mixers
        self.dbg = dbg
        self.phases = phases
        self.nlayers = nlayers
        nc = bass.Bass("TRN2", target_bir_lowering=False)
        self.nc = nc
        self.S = Sched(nc)
        ik = "ExternalInput"
        sk = "ExternalOutput" if dbg else "Internal"
        dt = nc.dram_tensor
        self.xT = dt("xT", [D, SEQ], F32, kind=ik)
        self.c8 = dt("c8", [128, 8], F32, kind=ik)
        self.w_ada = dt("w_ada", [L, D, 6 * D], F32, kind=ik)
        self.b_ada = dt("b_ada", [L, 6 * D], F32, kind=ik)
        self.w_in = dt("w_in", [L, D, NCOL], F32, kind=ik)
        self.w_out = dt("w_out", [L, D, D], F32, kind=ik)
        self.w_ff1 = dt("w_ff1", [L, D, 4 * D], F32, kind=ik)
        self.w_ff2 = dt("w_ff2", [L, 4 * D, D], F32, kind=ik)
        self.pp = dt("pp", [L, 128, NPP], F32, kind=ik)
        self.lam = dt("lam", [L, 128], F32, kind=ik)
        self.cmat = dt("cmat", [128, 5 * 128], F32, kind=ik)
        self.alibi = dt("alibi", [128, 128], F32, kind=ik)
        self.crow = dt("crow", [4, SEQ], F32, kind=ik)
        self.onesrow = dt("onesrow", [1, SEQ], F32, kind=ik)
        self.mask4 = dt("mask4", [128, 256], F32, kind=ik)
        self.ycat_in = dt("ycat_in", [D, SEQ], F32, kind=ik) if dbg else None
        self.outT = dt("outT", [D, SEQ], F32, kind="ExternalOutput")
        self.xres = dt("xres", [D, SEQ], F32, kind=sk)
        self.zfm = dt("zfm", [NFM * 128, SEQ], F32, kind=sk)
        self.ztm = dt("ztm", [SEQ, NTM], BF16, kind=sk)
        self.ycat = dt("ycat", [D, SEQ], BF16, kind=sk)
        self.modd = dt("modd", [128, 48], F32, kind=sk)
        self.PS = [nc.alloc_psum_tensor("ps%d" % i, [128, 512], F32) for i in range(8)]
        self.build()

    def act(self, out, in_, func, R, W, bias=0.0, scale=1.0):
        return self.S.op("act", lambda e: e.activation(out=out, in_=in_, func=func, bias=bias, scale=scale), R, W)

    def ts(self, eng, out, in0, s1, s2, op0, op1, R, W):
        if s2 is None:
            return self.S.op(eng, lambda e: e.tensor_scalar(out, in0, s1, None, op0), R, W)
        return self.S.op(eng, lambda e: e.tensor_scalar(out, in0, s1, s2, op0, op1), R, W)

    def tt(self, eng, out, in0, in1, op, R, W):
        return self.S.op(eng, lambda e: e.tensor_tensor(out, in0, in1, op), R, W)

    def stt(self, eng, out, in0, sc, in1, op0, op1, R, W):
        return self.S.op(eng, lambda e: e.scalar_tensor_tensor(out, in0, sc, in1, op0, op1), R, W)

    def cp(self, eng, out, in_, R, W):
        if eng == "act":
            return self.act(out, in_, AF.Copy, R, W)
        return self.S.op(eng, lambda e: e.tensor_copy(out, in_), R, W)

    def mm(self, items, R, W):
        fns = []
        for (out, lhsT, rhs, st, sp) in items:
            fns.append(lambda e, out=out, lhsT=lhsT, rhs=rhs, st=st, sp=sp:
                       e.matmul(out, lhsT=lhsT, rhs=rhs, start=st, stop=sp))
        return self.S.pe_group(fns, R, W)

    def dump(self, name, ap, key, shape, dtype=F32):
        if not self.dbg:
            return
        t = self.nc.dram_tensor("dbg_" + name, list(shape), dtype, kind="ExternalOutput")
        self.S.dma("sp", t.ap(), ap, reads=[key], writes=[("dbg", name)])

    def u(self, name):
        self._uid = getattr(self, '_uid', 0) + 1
        return '%s_%d' % (name, self._uid)

    def sb(self, name, shape, dtype):
        return self.nc.alloc_sbuf_tensor(name, shape, dtype)

    def build(self):
        nc, S = self.nc, self.S
        self.cbf = self.sb("cbf", [128, 5 * 128], BF16)
        self.cf = self.sb("cf", [128, 128], F32)
        S.dma("pool", self.cbf[:], self.cmat.ap(), writes=["cbf"])
        S.dma("sp", self.cf[:], self.cmat[:, 0:128], writes=["cf"])
        self.cf2 = self.sb("cf2", [128, 256], F32)
        S.dma("sp", self.cf2[:], self.mask4.ap(), writes=["cf2"])
        self.mask4_f = self.cf2
        self.ident_bf = self.cbf[:, 0:128]
        self.blk64_bf = self.cbf[:, 128:256]
        self.blk32d_bf = self.cbf[:, 256:384]
        self.trimask_bf = self.cbf[:, 384:512]
        self.ident_f = self.cf[:, 0:128]
        self.ones_bf = self.sb("ones_bf", [128, 128], BF16)
        self.ones_f = self.sb("ones_f", [128, 128], F32)
        S.op("dve", lambda e: e.memset(self.ones_bf[:], 1.0), writes=["ones_bf"])
        S.op("dve", lambda e: e.memset(self.ones_f[:], 1.0), writes=["ones_f"])
        self.ppt = self.sb("ppt", [128, L, NPP], F32)
        for l in range(L):
            S.dma("sp", self.ppt[:, l, :], self.pp[l], writes=["ppt"])
        self.modv = self.sb("modv", [128, 48], F32)
        self.AB = self.sb("AB", [128, 16], F32)
        S.barrier()
        base = (nc.sbuf_base, nc.sbuf_top) if hasattr(nc, "sbuf_base") else None
        for l in range(self.nlayers):
            src = self.xT if l == 0 else self.xres
            dst = self.xres if l == 0 and self.nlayers > 1 else self.outT
            if "0" in self.phases:
                self.phase0(l)
            if "A" in self.phases:
                self.phaseA(l, src)
            if "B" in self.phases:
                self.phaseB(l)
            if "C" in self.phases:
                self.phaseCD(l, src, dst)
        S.finish()

    def phase0(self, l):
        nc, S = self.nc, self.S
        with nc.sbuf_tensor(self.u("cin"), [128, 8], F32) as cin, \
                nc.sbuf_tensor(self.u("ctmp"), [128, 8], F32) as ctmp, \
                nc.sbuf_tensor(self.u("cact"), [128, 8], F32) as cact, \
                nc.sbuf_tensor(self.u("wa0"), [128, 8, 512], F32) as wa0, \
                nc.sbuf_tensor(self.u("wa1"), [128, 8, 512], F32) as wa1, \
                nc.sbuf_tensor(self.u("modrow"), [1, 6 * D], F32) as modrow, \
                nc.sbuf_tensor(self.u("badar"), [1, 6 * D], F32) as badar:
            S.dma("sp", cin[:], self.c8.ap(), writes=["cin"])
            S.dma("sp", badar[:], self.b_ada[l:l + 1, :], writes=["badar"])
            self.act(ctmp[:], cin[:], AF.Exp, ["cin"], ["ctmp"], scale=-1.0)
            self.ts("dve", ctmp[:], ctmp[:], 1.0, None, ALU.add, None, ["ctmp"], ["ctmp"])
            S.op("dve", lambda e: e.reciprocal(ctmp[:], ctmp[:]), ["ctmp"], ["ctmp"])
            self.tt("dve", cact[:], cin[:], ctmp[:], ALU.mult, ["cin", "ctmp"], ["cact"])
            wring = Ring("wa", [wa0, wa1])
            for j in range(12):
                buf, bk = wring.next()
                S.dma("sp", buf[:], self.w_ada[l, :, j * 512:(j + 1) * 512].rearrange("(kt p) n -> p kt n", p=128),
                      writes=[bk])
                ps = self.PS[j % 2]
                pk = ("ps", j % 2)
                self.mm([(ps[0:1, :], cact[:, kt:kt + 1], buf[:, kt, :], kt == 0, kt == 7) for kt in range(8)],
                        ["cact", bk], [pk])
                self.tt("dve", modrow[0:1, j * 512:(j + 1) * 512], ps[0:1, :], badar[0:1, j * 512:(j + 1) * 512],
                        ALU.add, [pk, "badar"], ["modrow"])
            ps = self.PS[2]
            self.mm([(ps[:, j:j + 1], modrow[0:1, j * 128:(j + 1) * 128], self.ones_f[0:1, 0:1], True, True)
                     for j in range(48)], ["modrow", "ones_f"], [("ps", 2)])
            self.cp("dve", self.modv[:], ps[:, 0:48], [("ps", 2)], ["modv"])
            pp = self.ppt
            self.stt("dve", self.AB[:, 0:8], self.modv[:, 8:16], 1.0, pp[:, l, 0:8], ALU.add, ALU.mult,
                     ["modv", "ppt"], ["AB"])
            self.stt("dve", self.AB[:, 8:16], self.modv[:, 32:40], 1.0, pp[:, l, 8:16], ALU.add, ALU.mult,
                     ["modv", "ppt"], ["AB"])
            if self.dbg:
                S.dma("sp", self.modd.ap(), self.modv[:], reads=["modv"], writes=["modd"])
            S.barrier()

    def bg_run(self, n):
        for _ in range(n):
            if getattr(self, "bg", None) is None:
                return
            try:
                next(self.bg)
            except StopIteration:
                self.bg = None

    def bg_drain(self):
        while getattr(self, "bg", None) is not None:
            self.bg_run(64)

    def modulate_g(self, xc, xk, h, hk, sq, sqk, rstd, rk, tmpring, psidx, N, Acol, Bcol):
        ps = self.PS[psidx]
        pk = ("ps", psidx)
        for kt in range(8):
            self.tt("pool", sq[:, kt, :N], xc[:, kt, :N], xc[:, kt, :N], ALU.mult, [xk], [(sqk, kt)])
            yield
        self.mm([(ps[:, :N], self.ones_bf[:], sq[:, kt, :N], kt == 0, kt == 7) for kt in range(8)],
                ["ones_bf"] + [(sqk, kt) for kt in range(8)], [pk])
        yield
        self.ts("dve", rstd[:, :N], ps[:, :N], 1.0 / D, EPS, ALU.mult, ALU.add, [pk], [rk])
        yield
        self.act(rstd[:, :N], rstd[:, :N], AF.Sqrt, [rk], [rk])
        yield
        self.S.op("dve", lambda e: e.reciprocal(rstd[:, :N], rstd[:, :N]), [rk], [rk])
        yield
        for kt in range(8):
            tmp, tk = tmpring.next()
            self.stt("dve", tmp[:, :N], xc[:, kt, :N], self.AB[:, Acol + kt:Acol + kt + 1], rstd[:, :N],
                     ALU.mult, ALU.mult, [xk, "AB", rk], [tk])
            yield
            self.ts("pool", h[:, kt, :N], tmp[:, :N], self.modv[:, Bcol + kt:Bcol + kt + 1], None, ALU.add, None,
                    [tk, "modv"], [(hk, kt)])
            yield

    def phaseA(self, l, src):
        nc, S = self.nc, self.S
        with ExitStack() as es:
            T = lambda nm, sh, dt_: es.enter_context(nc.sbuf_tensor(self.u(nm), sh, dt_))
            win = T("win", [128, 8, NCOL], BF16)
            xa = [T("xa%d" % i, [128, 8, 512], F32) for i in range(2)]
            ha = [T("ha%d" % i, [128, 8, 512], BF16) for i in range(2)]
            sqa = T("sqa", [128, 8, 512], BF16)
            rstda = T("rstda", [128, 512], F32)
            tmpring = Ring("tmpa", [T("tmpa%d" % i, [128, 512], F32) for i in range(2)])
            string = Ring("sta", [T("sta%d" % i, [128, 512], F32) for i in range(4)])
            stbring = Ring("stb", [T("stb%d" % i, [128, 512], BF16) for i in range(2)])
            for kt in range(8):
                S.dma("pool", win[:, kt, :], self.w_in[l, kt * 128:(kt + 1) * 128, :], writes=[("win", kt)])
            wink = [("win", kt) for kt in range(8)]
            srcv = src.ap().rearrange("(kt p) t -> p kt t", p=128)

            def stage1(c):
                xc, xk = xa[c % 2], ("xa", c % 2)
                S.dma("sp", xc[:], srcv[:, :, c * 512:(c + 1) * 512], writes=[xk])
                yield
                yield from self.modulate_g(xc, xk, ha[c % 2], ("ha", c % 2), sqa, "sqa", rstda, "rstda",
                                           tmpring, 7, 512, 0, 0)

            for _ in stage1(0):
                pass
            psi = 0
            ev = 0
            for c in range(8):
                self.bg = stage1(c + 1) if c + 1 < 8 else None
                h = ha[c % 2]
                hks = [(("ha", c % 2), kt) for kt in range(8)]
                for ft in range(NFM):
                    ps = self.PS[psi]
                    pk = ("ps", psi)
                    psi = (psi + 1) % 6
                    self.mm([(ps[:], win[:, kt, ft * 128:(ft + 1) * 128], h[:, kt, :], kt == 0, kt == 7)
                             for kt in range(8)], hks + wink, [pk])
                    st, sk = string.next()
                    self.cp("act" if ev % 2 == 0 else "dve", st[:], ps[:], [pk], [sk])
                    ev += 1
                    S.dma("sp", self.zfm[ft * 128:(ft + 1) * 128, c * 512:(c + 1) * 512], st[:],
                          reads=[sk], writes=[("zfm", ft, c)])
                    self.bg_run(1)
                for tt in range(4):
                    for half in range(2):
                        ps = self.PS[psi]
                        pk = ("ps", psi)
                        psi = (psi + 1) % 6
                        c0 = NFM * 128 + half * 512
                        self.mm([(ps[:], h[:, kt, tt * 128:(tt + 1) * 128], win[:, kt, c0:c0 + 512], kt == 0, kt == 7)
                                 for kt in range(8)], hks + wink, [pk])
                        st, sk = stbring.next()
                        self.cp("act" if ev % 2 == 0 else "dve", st[:], ps[:], [pk], [sk])
                        ev += 1
                        r0 = c * 512 + tt * 128
                        S.dma("sp", self.ztm[r0:r0 + 128, half * 512:(half + 1) * 512], st[:],
                              reads=[sk], writes=[("ztm", c, tt, half)])
                        self.bg_run(1)
                self.bg_drain()
            S.barrier()

    def phaseB(self, l):
        S = self.S
        self.setupB(l)
        S.barrier()
        if "h" in self.mixers:
            self.gla(l, "hgrn")
            S.barrier()
        if "m" in self.mixers:
            self.gla(l, "mlstm")
            S.barrier()
        if "f" in self.mixers:
            self.attn(l, "fox")
            S.barrier()
        if "d" in self.mixers:
            self.attn(l, "diff")
            S.barrier()

    def setupB(self, l):
        nc, S = self.nc, self.S
        if not hasattr(self, "sv"):
            self.sv = self.sb("sv", [128, 16], F32)
            self.lamrow = self.sb("lamrow", [1, 256], F32)
            self.negpp = self.sb("negpp", [128, NPP], F32)
        sv, pp = self.sv, self.ppt
        self.ts("dve", self.negpp[:], pp[:, l, :], -1.0, None, ALU.mult, None, ["ppt"], ["negpp"])
        self.tt("dve", sv[:, 0:2], pp[:, l, 18:20], pp[:, l, 16:18], ALU.subtract, ["ppt"], ["sv"])
        self.act(sv[:, 0:2], sv[:, 0:2], AF.Exp, ["sv"], ["sv"], scale=-1.0)
        self.ts("dve", sv[:, 0:2], sv[:, 0:2], 1.0, None, ALU.add, None, ["sv"], ["sv"])
        S.op("dve", lambda e: e.reciprocal(sv[:, 0:2], sv[:, 0:2]), ["sv"], ["sv"])
        self.ts("dve", sv[:, 0:2], sv[:, 0:2], float(l), None, ALU.mult, None, ["sv"], ["sv"])
        self.ts("dve", sv[:, 2:4], sv[:, 0:2], -1.0, 1.0, ALU.mult, ALU.add, ["sv"], ["sv"])
        lr = self.lamrow
        S.dma("sp", lr[0:1, 0:128], self.lam[l:l + 1, :], writes=["lamrow"])
        self.tt("dve", lr[0:1, 128:160], lr[0:1, 0:32], lr[0:1, 32:64], ALU.mult, ["lamrow"], ["lamrow"])
        self.tt("dve", lr[0:1, 160:192], lr[0:1, 64:96], lr[0:1, 96:128], ALU.mult, ["lamrow"], ["lamrow"])
        S.op("dve", lambda e: e.tensor_reduce(lr[0:1, 192:193], lr[0:1, 128:160], AX.X, ALU.add), ["lamrow"], ["lamrow"])
        S.op("dve", lambda e: e.tensor_reduce(lr[0:1, 193:194], lr[0:1, 160:192], AX.X, ALU.add), ["lamrow"], ["lamrow"])
        self.act(lr[0:1, 192:194], lr[0:1, 192:194], AF.Exp, ["lamrow"], ["lamrow"])
        import math
        lam_init = 0.8 - 0.6 * math.exp(-0.3 * l)
        self.lam_init = lam_init
        self.tt("dve", lr[0:1, 194:195], lr[0:1, 193:194], lr[0:1, 192:193], ALU.subtract, ["lamrow"], ["lamrow"])
        self.ts("dve", lr[0:1, 194:195], lr[0:1, 194:195], -lam_init, None, ALU.add, None, ["lamrow"], ["lamrow"])
        ps = self.PS[0]
        self.mm([(ps[:, 0:1], self.ones_f[0:1, 0:128], lr[0:1, 194:195], True, True)], ["lamrow", "ones_f"], [("ps", 0)])
        self.cp("dve", sv[:, 4:5], ps[:, 0:1], [("ps", 0)], ["sv"])

    def sigmoid_inplace(self, t, key):
        self.act(t, t, AF.Exp, [key], [key], scale=-1.0)
        self.ts("dve", t, t, 1.0, None, ALU.add, None, [key], [key])
        self.S.op("dve", lambda e: e.reciprocal(t, t), [key], [key])

    def rstd_from_ps(self, out, ps, pk, key, mult, add):
        self.act(out, ps, AF.Ln, [pk], [key], scale=mult, bias=add)
        self.act(out, out, AF.Exp, [key], [key], scale=-0.5)

    def sigmoid_act(self, t, key):
        self.act(t, t, AF.Sigmoid, [key], [key])

    def attn(self, l, kind):
        nc, S = self.nc, self.S
        fox = kind == "fox"
        pp, sv = self.ppt, self.sv
        with ExitStack() as es:
            T = lambda nm, sh, dt_: es.enter_context(nc.sbuf_tensor(self.u(nm), sh, dt_))
            qraws = [T("qraw%d" % i, [128, SEQ], F32) for i in range(2)]
            kraws = [T("kraw%d" % i, [128, SEQ], F32) for i in range(2)]
            qps = [T("qp%d" % i, [128, SEQ], BF16) for i in range(2)]
            kps = [T("kp%d" % i, [128, SEQ], BF16) for i in range(2)]
            Vps = [T("Vp%d" % i, [128, 32, 128], BF16) for i in range(2)]
            negbs = [T("negb%d" % i, [128, 32], F32) for i in range(2)]
            ones4k = T("ones4k", [128, SEQ], F32) if fox else None
            sqring = Ring("sqt", [T("sqt%d" % i, [128, 512], BF16) for i in range(2)])
            rsring = Ring("rst", [T("rst%d" % i, [128, 512], F32) for i in range(2)])
            pring = Ring("pT", [T("pT%d" % i, [128, 512], BF16) for i in range(4)])
            rrow = T("rrow", [128, 2, 512], F32)
            ocp = T("ocp", [128, 2, 512], F32)
            bcs = T("bcs", [128, 2, 512], F32)
            osb = T("osb", [128, 512], F32)
            osb2 = T("osb2", [128, 512], F32)
            esq = T("esq", [128, 512], BF16)
            erst = T("erst", [128, 512], F32)
            gch = T("gch", [128, 512], F32)
            yst = T("yst", [128, 512], BF16)
            if fox:
                S.op("pool", lambda e: e.memset(ones4k[:], 1.0), writes=["ones4k"])
            sring = Ring("pss", [self.PS[0], self.PS[1], self.PS[2], self.PS[3]])
            if fox:
                ranges = [(0, 64)]
                nd = 64.0
                gq, gk = pp[:, l, 24:25], pp[:, l, 25:26]
                lhs_n = self.ones_bf[0:64, 0:64]
                nrows = 64
                KR = [(0, 65)]
            else:
                ranges = [(0, 32), (64, 96)]
                nd = 32.0
                gq, gk = pp[:, l, 21:22], pp[:, l, 22:23]
                lhs_n = self.blk32d_bf
                nrows = 128
                KR = [(0, 33), (64, 97)]
            nh = len(KR)
            M = 128
            ops_ = [self.PS[4], self.PS[5]]
            opk = [("ps", 4), ("ps", 5)]

            def prep(h):
                b = h % 2
                qraw, kraw, qp, kp, Vp, negb = qraws[b], kraws[b], qps[b], kps[b], Vps[b], negbs[b]
                kq, kk, kqp, kkp, kv, kn = ("qraw", b), ("kraw", b), ("qp", b), ("kp", b), ("Vp", b), ("negb", b)
                par = h % 2
                srow = 64 if par == 0 else 0
                voff = 0 if par == 0 else 64
                tq = (T_FQ if fox else T_DQ) + h
                tk = (T_FK if fox else T_DK) + h
                S.dma("sp", qraw[:], self.zfm[tq * 128:(tq + 1) * 128, :], writes=[kq])
                S.dma("sp", kraw[:], self.zfm[tk * 128:(tk + 1) * 128, :], writes=[kk])
                yield
                S.op("pool", lambda e: e.memset(Vp[:], 0.0), writes=[kv])
                vcol = (V_F if fox else V_D) + 64 * h
                S.dma("sp", Vp[:, :, voff:voff + 64],
                      self.ztm[:, vcol:vcol + 64].rearrange("(j p) c -> p j c", p=128), reads=[kv], writes=[kv])
                S.op("pool", lambda e, c1=srow: e.memset(Vp[:, :, c1:c1 + 64], 1.0), reads=[kv], writes=[kv])
                yield
                for c in range(8):
                    cs = slice(c * 512, (c + 1) * 512)
                    for (raw, rk_, dstt, dk_, g, isq) in ((qraw, kq, qp, kqp, gq, True), (kraw, kk, kp, kkp, gk, False)):
                        sqt, sqk = sqring.next()
                        rst, rsk = rsring.next()
                        self.tt("pool", sqt[0:nrows, :], raw[0:nrows, cs], raw[0:nrows, cs], ALU.mult, [rk_], [sqk])
                        yield
                        ps = self.PS[7]
                        self.mm([(ps[0:nrows, :], lhs_n, sqt[0:nrows, :], True, True)], [sqk, "cbf", "ones_bf"], [("ps", 7)])
                        yield
                        if isq:
                            self.act(rst[0:nrows, :], ps[0:nrows, :], AF.Ln, [("ps", 7)], [rsk], scale=1.0, bias=nd * EPS)
                        else:
                            self.act(rst[0:nrows, :], ps[0:nrows, :], AF.Ln, [("ps", 7)], [rsk], scale=1.0 / nd, bias=EPS)
                        yield
                        self.act(rst[0:nrows, :], rst[0:nrows, :], AF.Exp, [rsk], [rsk], scale=-0.5)
                        yield
                        for (a, b_) in ranges:
                            self.stt("dve", dstt[a:b_, cs], raw[a:b_, cs], g[a:b_, :], rst[a:b_, :], ALU.mult, ALU.mult,
                                     [rk_, rsk, "ppt"], [dk_])
                        yield
                if fox:
                    fr = qraw[64:65, :]
                    self.act(fr, fr, AF.Exp, [kq], [kq], scale=-1.0, bias=self.negpp[64:65, 26 + h:27 + h])
                    yield
                    self.act(fr, fr, AF.Ln, [kq], [kq], bias=1.0)
                    yield
                    S.op("dve", lambda e: e.tensor_tensor_scan(fr, ones4k[64:65, :], fr, 0.0, ALU.mult, ALU.subtract),
                         [kq, "ones4k"], [kq])
                    yield
                    self.cp("dve", qp[64:65, :], fr, [kq], [kqp])
                    S.op("pool", lambda e: e.memset(kp[64:65, :], 1.0), reads=[kkp], writes=[kkp])
                    yield
                    ps = self.PS[7]
                    S.pe_group([lambda e, j=j, ps=ps: e.transpose(ps[:, j:j + 1], qraw[64:65, j * 128:(j + 1) * 128],
                                                                  self.ident_f[64:65, 64:65]) for j in range(32)],
                               [kq, "cf"], [("ps", 7)])
                    yield
                    self.ts("dve", negb[:], ps[:, 0:32], -1.0, None, ALU.mult, None, [("ps", 7)], [kn])
                    yield
                else:
                    for r0 in (32, 96):
                        S.dma("pool", qp[r0:r0 + 1, :], self.crow[h:h + 1, :], reads=[kqp], writes=[kqp])
                        S.dma("pool", kp[r0:r0 + 1, :], self.onesrow[0:1, :], reads=[kkp], writes=[kkp])
                    S.dma("sp", negb[:], self.alibi[:, h * 32:(h + 1) * 32], writes=[kn])
                    yield

            def main(h):
                b = h % 2
                qp, kp, Vp, negb = qps[b], kps[b], Vps[b], negbs[b]
                kqp, kkp, kv, kn = ("qp", b), ("kp", b), ("Vp", b), ("negb", b)
                par = h % 2
                pb = 64 * par
                srow = 64 if par == 0 else 0

                def emit_qk(c, j, a):
                    dj = j - 4 * c
                    n0 = 128 * dj if dj > 0 else 0
                    k0, k1 = KR[a]
                    ps_s, sk_ = sring.next()
                    items = [(ps_s[:, n0:512], kp[k0:k1, j * 128:(j + 1) * 128],
                              qp[k0:k1, c * 512 + n0:(c + 1) * 512], True, dj < 0)]
                    if nh == 1 and WARM:
                        items = [items[0], items[0]]
                    if dj >= 0:
                        items.append((ps_s[:, n0:n0 + 128], self.ident_bf, self.trimask_bf, False, True))
                    self.mm(items, [kkp, kqp, "cbf"], [sk_])
                    pT, pk_ = pring.next()
                    self.act(pT[:, n0:512], ps_s[:, n0:512], AF.Exp, [sk_, kn], [pk_], bias=negb[:, j:j + 1])
                    return pT, pk_, n0

                def emit_pv(c, j, a, pT, pk_, n0):
                    nj = 4 * c + 4
                    self.mm([(ops_[a][0:M, n0:512], Vp[:, j, 0:M], pT[:, n0:512], j == 0, j == nj - 1)],
                            [kv, pk_], [opk[a]])
                    if j == nj - 1 and a == nh - 1:
                        epilogue(c)

                def epilogue(c):
                    cs = slice(c * 512, (c + 1) * 512)
                    for a in range(nh):
                        self.cp("dve", ocp[:, a, :], ops_[a][:, :], [opk[a]], [("ocp", a)])
                    for a in range(nh):
                        S.op("dve", lambda e, a=a, srow=srow: e.reciprocal(rrow[srow:srow + 64, a, :], ocp[srow:srow + 64, a, :]),
                             [("ocp", a)], [("rrow", a)])
                        psb = self.PS[6]
                        self.mm([(psb[:, :], self.ones_f[srow:srow + 1, 0:128], rrow[srow:srow + 1, a, :], True, True)],
                                [("rrow", a), "ones_f"], [("ps", 6)])
                        self.cp("act", bcs[pb:pb + 64, a, :], psb[pb:pb + 64, :], [("ps", 6)], [("bcs", a)])
                    rows = slice(pb, pb + 64)
                    if fox:
                        tg = T_FG + h // 2
                        S.dma("sp", gch[rows, :], self.zfm[tg * 128 + pb:tg * 128 + pb + 64, cs], writes=["gch"])
                        self.sigmoid_inplace(gch[rows, :], "gch")
                        self.tt("dve", osb[rows, :], ocp[rows, 0, :], bcs[rows, 0, :], ALU.mult, [("ocp", 0), ("bcs", 0)], ["osb"])
                        self.tt("pool", yst[rows, :], osb[rows, :], gch[rows, :], ALU.mult, ["osb", "gch"], ["yst"])
                        S.dma("sp", self.ycat[512 + 64 * h:512 + 64 * h + 64, cs], yst[rows, :], reads=["yst"],
                              writes=[("ycat", kind, h, c)])
                    else:
                        self.tt("dve", osb[rows, :], ocp[rows, 0, :], bcs[rows, 0, :], ALU.mult, [("ocp", 0), ("bcs", 0)], ["osb"])
                        self.tt("dve", osb2[rows, :], ocp[rows, 1, :], bcs[rows, 1, :], ALU.mult, [("ocp", 1), ("bcs", 1)], ["osb2"])
                        self.stt("dve", osb[rows, :], osb2[rows, :], sv[rows, 4:5], osb[rows, :], ALU.mult, ALU.add,
                                 ["osb", "osb2", "sv"], ["osb"])
                        self.tt("pool", esq[rows, :], osb[rows, :], osb[rows, :], ALU.mult, ["osb"], ["esq"])
                        ps = self.PS[6]
                        self.mm([(ps[:, :], self.blk64_bf[rows, :], esq[rows, :], True, True)], ["esq", "cbf"], [("ps", 6)])
                        self.act(erst[rows, :], ps[rows, :], AF.Ln, [("ps", 6)], ["erst"], scale=1.0 / 64.0, bias=EPS)
                        self.act(erst[rows, :], erst[rows, :], AF.Exp, ["erst"], ["erst"], scale=-0.5)
                        self.stt("dve", osb[rows, :], osb[rows, :], pp[rows, l, 23:24], erst[rows, :], ALU.mult, ALU.mult,
                                 ["osb", "erst", "ppt"], ["osb"])
                        self.act(yst[rows, :], osb[rows, :], AF.Copy, ["osb"], ["yst"], scale=float(1.0 - self.lam_init))
                        S.dma("sp", self.ycat[256 + 64 * h:256 + 64 * h + 64, cs], yst[rows, :], reads=["yst"],
                              writes=[("ycat", kind, h, c)])

                steps = [(c, j, a) for c in range(8) for j in range(4 * c + 4) for a in range(nh)]
                LAG = 3
                pend = []
                for st in steps:
                    pend.append((st, emit_qk(*st)))
                    if len(pend) > LAG:
                        st0, info = pend.pop(0)
                        emit_pv(*st0, *info)
                    self.bg_run(1)
                while pend:
                    st0, info = pend.pop(0)
                    emit_pv(*st0, *info)

            for _ in prep(0):
                pass
            for h in range(4):
                self.bg = prep(h + 1) if h + 1 < 4 else None
                main(h)
                self.bg_drain()

    def gla(self, l, kind):
        nc, S = self.nc, self.S
        hg = kind == "hgrn"
        pp, sv = self.ppt, self.sv
        NV = 128 if hg else 256
        CL = 40.0
        with ExitStack() as es:
            T = lambda nm, sh, dt_: es.enter_context(nc.sbuf_tensor(self.u(nm), sh, dt_))
            BR = T("BR", [128, 2, 64, 64], F32)
            B3 = BR[:, 0]
            R3 = BR[:, 1]
            qx = T("qx", [128, SEQ], F32)
            kx = T("kx", [128, SEQ], F32)
            tmp = T("tmp", [128, SEQ], F32)
            ones4k = T("ones4k", [128, SEQ], F32)
            Qs = T("Qs", [128, SEQ], BF16)
            Qb = T("Qb", [128, SEQ], BF16)
            Kb = T("Kb", [128, SEQ], BF16)
            Q32 = T("Q32", [128, SEQ], BF16)
            K32 = T("K32", [128, SEQ], BF16)
            K2 = T("K2", [128, SEQ], BF16)
            K2tm = T("K2tm", [128, 32, 128], BF16)
            Vx = T("Vx", [128, 32, 128], BF16)
            sm = T("sm", [128, 6, 64], F32)
            sm32 = T("sm32", [128, 128], F32)
            St = T("St", [128, NV], F32)
            Sall = BR[:].rearrange("p a b c -> p (a b c)").bitcast(BF16)[:, 0:64 * NV].rearrange("p (n v) -> p n v", v=NV)
            At0 = T("At0", [128, 512], BF16)
            At1 = T("At1", [128, 512], BF16)
            osb = T("osb", [128, 512], F32)
            dsb = T("dsb", [128, 512], F32) if not hg else None
            sqt = T("sqt", [128, 512], BF16) if hg else None
            rst = T("rst", [128, 512], F32) if hg else None
            gch = T("gch", [128, 512], F32)
            gch2 = T("gch2", [128, 512], F32) if hg else None
            yst = T("yst", [128, 512], BF16)
            B2 = B3[:].rearrange("p a b -> p (a b)")
            R2 = R3[:].rearrange("p a b -> p (a b)")
            B4 = B3[:].rearrange("p a (h b) -> p (a h) b", h=2)
            R4 = R3[:].rearrange("p a (h b) -> p (a h) b", h=2)
            S.op("pool", lambda e: e.memset(ones4k[:], 1.0), writes=["ones4k"])
            bprev, blast, b31, dch = sm[:, 0, :], sm[:, 1, :], sm[:, 2, :], sm[:, 3, :]
            mask4 = self.mask4_f
            atring = Ring("At", [At0, At1])

            def bc64(i):
                return sm[:, i, :].unsqueeze(2).to_broadcast([128, 64, 64])

            for tp in range(2):
                if hg:
                    tq, tf = T_HQ + tp, T_HF + tp
                    S.dma("sp", tmp[:], self.zfm[tf * 128:(tf + 1) * 128, :], writes=["tmp"])
                    S.dma("sp", qx[:], self.zfm[tq * 128:(tq + 1) * 128, :], writes=["qx"])
                    self.sigmoid_act(tmp[:], "tmp")
                    self.ts("dve", kx[:], tmp[:], sv[:, 2 + tp:3 + tp], sv[:, tp:tp + 1], ALU.mult, ALU.add,
                            ["tmp", "sv"], ["kx"])
                    self.act(B2, kx[:], AF.Ln, ["kx"], ["B"])
                    self.ts("dve", kx[:], kx[:], -1.0, 1.0, ALU.mult, ALU.add, ["kx"], ["kx"])
                    S.op("dve", lambda e: e.tensor_tensor_scan(B2, ones4k[:], B2, 0.0, ALU.mult, ALU.add),
                         ["B", "ones4k"], ["B"])
                    qscale = 1.0
                else:
                    for (tsrc, dst_, dk_, cb) in ((T_MQ + tp, qx, "qx", 34 + tp * 4), (T_MK + tp, kx, "kx", 42 + tp * 4)):
                        S.dma("sp", R2, self.zfm[tsrc * 128:(tsrc + 1) * 128, :], writes=["R"])
                        self.ts("dve", tmp[:], R2, pp[:, l, cb + 3:cb + 4], None, ALU.mult, None, ["R", "ppt"], ["tmp"])
                        for j in (2, 1, 0):
                            sh = 3 - j
                            self.stt("dve", tmp[:, sh:], R2[:, 0:SEQ - sh], pp[:, l, cb + j:cb + j + 1], tmp[:, sh:],
                                     ALU.mult, ALU.add, ["R", "tmp", "ppt"], ["tmp"])
                        self.act(dst_[:], tmp[:], AF.Silu, ["tmp"], [dk_])
                    tf, ti = T_MF + tp, T_MI + tp
                    S.dma("sp", B2, self.zfm[tf * 128:(tf + 1) * 128, :], reads=["B"], writes=["B"])
                    S.dma("sp", tmp[:], self.zfm[ti * 128:(ti + 1) * 128, :], reads=["tmp"], writes=["tmp"])
                    self.act(B2, B2, AF.Exp, ["B", "negpp"], ["B"], scale=-1.0, bias=self.negpp[:, 32 + tp:33 + tp])
                    self.act(B2, B2, AF.Ln, ["B"], ["B"], bias=1.0)
                    S.op("dve", lambda e: e.tensor_tensor_scan(B2, ones4k[:], B2, 0.0, ALU.mult, ALU.subtract),
                         ["B", "ones4k"], ["B"])
                    self.ts("dve", tmp[:], tmp[:], pp[:, l, 30 + tp:31 + tp], None, ALU.add, None, ["tmp", "ppt"], ["tmp"])
                    qscale = 0.125
                self.cp("dve", blast, B3[:, :, 63], ["B"], ["sm"])
                self.cp("dve", b31, B3[:, :, 31], ["B"], ["sm"])
                S.op("dve", lambda e: e.memset(sm[:, 0, 0:1], 0.0), ["sm"], ["sm"])
                self.cp("dve", bprev[:, 1:64], blast[:, 0:63], ["sm"], ["sm"])
                self.cp("dve", sm32[:], B4[:, :, 15], ["B"], ["sm32"])
                self.tt("dve", dch, blast, bprev, ALU.subtract, ["sm"], ["sm"])
                self.act(dch, dch, AF.Exp, ["sm"], ["sm"])
                bc32 = sm32[:].unsqueeze(2).to_broadcast([128, 128, 32])

                def emit(dst_, dk_, src, sk_, with_i, scale):
                    if with_i:
                        self.tt("dve", R2, R2, tmp[:], ALU.add, ["R", "tmp"], ["R"])
                    self.act(R2, R2, AF.Exp, ["R"], ["R"])
                    if scale == 1.0:
                        self.tt("dve", dst_[:], src[:], R2, ALU.mult, [sk_, "R"], [dk_])
                    else:
                        self.stt("dve", dst_[:], src[:], scale, R2, ALU.mult, ALU.mult, [sk_, "R"], [dk_])

                wi = not hg
                self.tt("dve", R3[:], B3[:], bc64(0), ALU.subtract, ["B", "sm"], ["R"])
                emit(Qs, "Qs", qx, "qx", False, qscale)
                self.tt("dve", R3[:], B3[:], bc64(1), ALU.subtract, ["B", "sm"], ["R"])
                self.ts("dve", R2, R2, -1.0, None, ALU.mult, None, ["R"], ["R"])
                emit(K2, "K2", kx, "kx", wi, 1.0)
                self.tt("dve", R3[:], B3[:], bc64(2), ALU.subtract, ["B", "sm"], ["R"])
                self.ts("dve", R2, R2, 0.0, None, ALU.min, None, ["R"], ["R"])
                emit(Qb, "Qb", qx, "qx", False, qscale)
                self.tt("dve", R3[:], B3[:], bc64(2), ALU.subtract, ["B", "sm"], ["R"])
                self.ts("dve", R2, R2, -1.0, 0.0, ALU.mult, ALU.min, ["R"], ["R"])
                emit(Kb, "Kb", kx, "kx", wi, 1.0)
                self.tt("dve", R4, B4, bc32, ALU.subtract, ["B", "sm32"], ["R"])
                self.ts("dve", R2, R2, CL, -CL, ALU.min, ALU.max, ["R"], ["R"])
                emit(Q32, "Q32", qx, "qx", False, qscale)
                self.tt("dve", R4, B4, bc32, ALU.subtract, ["B", "sm32"], ["R"])
                self.ts("dve", R2, R2, -1.0, CL, ALU.mult, ALU.min, ["R"], ["R"])
                self.ts("dve", R2, R2, -CL, None, ALU.max, None, ["R"], ["R"])
                emit(K32, "K32", kx, "kx", wi, 1.0)
                psT = self.PS[7][:, 0:64].bitcast(BF16)
                for m in range(32):
                    S.pe_group([lambda e, m=m: e.transpose(psT, K2[:, m * 128:(m + 1) * 128], self.ident_bf)],
                               ["K2", "cbf"], [("ps", 7)])
                    self.cp("act" if m % 2 == 0 else "dve", K2tm[:, m, :], psT, [("ps", 7)], [("K2tm", m)])
                voff = (V_H if hg else V_M) + tp * 128
                S.dma("sp", Vx[:, :, 0:128], self.ztm[:, voff:voff + 128].rearrange("(j p) c -> p j c", p=128),
                      writes=["Vx"])
                S.barrier()
                S.op("pool", lambda e: e.memset(St[:], 0.0), writes=["St"])
                S.op("pool", lambda e: e.memset(Sall[:, 0, :], 0.0), writes=[("Sall", 0)])
                pdr = [(self.PS[5], ("ps", 5)), (self.PS[7], ("ps", 7))]
                for n in range(64):
                    m, r = n // 2, n % 2
                    psd, pdk = pdr[n % 2]
                    items = [(psd[:, 0:128], K2tm[64 * r:64 * r + 64, m, :], Vx[64 * r:64 * r + 64, m, :], True, True)]
                    if not hg:
                        items.append((psd[:, 128:256], K2tm[64 * r:64 * r + 64, m, :], self.ones_bf[64 * r:64 * r + 64, :], True, True))
                    self.mm(items, [("K2tm", m), "Vx", "ones_bf"], [pdk])
                    self.stt("dve", St[:], St[:], dch[:, n:n + 1], psd[:, 0:NV], ALU.mult, ALU.add,
                             ["St", "sm", pdk], ["St"])
                    if n < 63:
                        self.cp("pool" if n % 2 == 0 else "act", Sall[:, n + 1, :], St[:], ["St"], [("Sall", n + 1)])
                NE, NO, DE, DO = self.PS[0], self.PS[1], self.PS[2], self.PS[3]
                allps = [("ps", 0), ("ps", 1), ("ps", 2), ("ps", 3)]
                for m in range(32):
                    cm = (m % 4) * 128
                    ms = slice(m * 128, (m + 1) * 128)
                    psa, psa2 = self.PS[4], self.PS[6]
                    self.mm([(psa[:, 0:128], K32[0:64, ms], Q32[0:64, ms], True, True)], ["K32", "Q32"], [("ps", 4)])
                    self.mm([(psa2[:, 0:128], K32[64:128, ms], Q32[64:128, ms], True, True)], ["K32", "Q32"], [("ps", 6)])
                    self.mm([(psa[:, 128:256], Kb[0:64, ms], Qb[0:64, ms], True, True)], ["Kb", "Qb"], [("ps", 4)])
                    self.mm([(psa2[:, 128:256], Kb[64:128, ms], Qb[64:128, ms], True, True)], ["Kb", "Qb"], [("ps", 6)])
                    At, ak = atring.next()
                    self.tt("dve", At[:, 0:256], psa[:, 0:256], mask4[:, 0:256], ALU.mult, [("ps", 4), "cf2"], [ak])
                    self.tt("dve", At[:, 256:512], psa2[:, 0:256], mask4[:, 0:256], ALU.mult, [("ps", 6), "cf2", ak], [ak])
                    items = [(NE[:, cm:cm + 128], Vx[:, m, 0:128], At[:, 0:128], True, False),
                             (NE[:, cm:cm + 128], Vx[:, m, 0:128], At[:, 128:256], False, False),
                             (NO[:, cm:cm + 128], Vx[:, m, 0:128], At[:, 256:384], True, False),
                             (NO[:, cm:cm + 128], Vx[:, m, 0:128], At[:, 384:512], False, False)]
                    if not hg:
                        items += [(DE[:, cm:cm + 128], self.ones_bf[:, :], At[:, 0:128], True, False),
                                  (DE[:, cm:cm + 128], self.ones_bf[:, :], At[:, 128:256], False, False),
                                  (DO[:, cm:cm + 128], self.ones_bf[:, :], At[:, 256:384], True, False),
                                  (DO[:, cm:cm + 128], self.ones_bf[:, :], At[:, 384:512], False, False)]
                    self.mm(items, ["Vx", ak], allps)
                    for r in range(2):
                        n = 2 * m + r
                        ns = slice(n * 64, (n + 1) * 64)
                        cn = cm + 64 * r
                        sbt = Sall[:, n, :]
                        items = [(NE[:, cn:cn + 64], sbt[0:64, 0:128], Qs[0:64, ns], False, True),
                                 (NO[:, cn:cn + 64], sbt[64:128, 0:128], Qs[64:128, ns], False, True)]
                        if not hg:
                            items += [(DE[:, cn:cn + 64], sbt[0:64, 128:256], Qs[0:64, ns], False, True),
                                      (DO[:, cn:cn + 64], sbt[64:128, 128:256], Qs[64:128, ns], False, True)]
                        self.mm(items, [("Sall", n), "Qs"], allps)
                    if m % 4 == 3:
                        c = m // 4
                        cs = slice(c * 512, (c + 1) * 512)
                        tg = (T_HG if hg else T_MO) + tp
                        S.dma("sp", gch[:], self.zfm[tg * 128:(tg + 1) * 128, cs], writes=["gch"])
                        if hg:
                            self.cp("act", osb[0:64, :], NE[0:64, :], [("ps", 0)], ["osb"])
                            self.cp("act", osb[64:128, :], NO[64:128, :], [("ps", 1), "osb"], ["osb"])
                            self.tt("pool", sqt[:], osb[:], osb[:], ALU.mult, ["osb"], ["sqt"])
                            ps = self.PS[7]
                            self.mm([(ps[:], self.blk64_bf, sqt[:], True, True)], ["sqt", "cbf"], [("ps", 7)])
                            self.rstd_from_ps(rst[:], ps[:], ("ps", 7), "rst", 1.0 / 64.0, EPS)
                            self.stt("dve", osb[:], osb[:], pp[:, l, 20:21], rst[:], ALU.mult, ALU.mult,
                                     ["osb", "rst", "ppt"], ["osb"])
                            self.act(gch[:], gch[:], AF.Silu, ["gch"], ["gch"])
                            self.tt("dve", yst[:], osb[:], gch[:], ALU.mult, ["osb", "gch"], ["yst"])
                            S.dma("sp", self.ycat[tp * 128:(tp + 1) * 128, cs], yst[:], reads=["yst"],
                                  writes=[("ycat", kind, tp, c)])
                        else:
                            self.cp("dve", dsb[0:64, :], DE[0:64, :], [("ps", 2)], ["dsb"])
                            self.cp("dve", dsb[64:128, :], DO[64:128, :], [("ps", 3), "dsb"], ["dsb"])
                            self.stt("dve", dsb[:], dsb[:], -1.0, dsb[:], ALU.mult, ALU.max, ["dsb"], ["dsb"])
                            self.ts("dve", dsb[:], dsb[:], 1.0, None, ALU.max, None, ["dsb"], ["dsb"])
                            S.op("dve", lambda e: e.reciprocal(dsb[:], dsb[:]), ["dsb"], ["dsb"])
                            self.tt("dve", osb[0:64, :], NE[0:64, :], dsb[0:64, :], ALU.mult, [("ps", 0), "dsb"], ["osb"])
                            self.tt("dve", osb[64:128, :], NO[64:128, :], dsb[64:128, :], ALU.mult, [("ps", 1), "dsb", "osb"], ["osb"])
                            self.sigmoid_act(gch[:], "gch")
                            self.tt("pool", yst[:], osb[:], gch[:], ALU.mult, ["osb", "gch"], ["yst"])
                            S.dma("sp", self.ycat[768 + tp * 128:768 + (tp + 1) * 128, cs], yst[:], reads=["yst"],
                                  writes=[("ycat", kind, tp, c)])
                S.barrier()

    def phaseCD(self, l, src, dst):
        nc, S = self.nc, self.S
        N = 256
        NCH = SEQ // N
        with ExitStack() as es:
            T = lambda nm, sh, dt_: es.enter_context(nc.sbuf_tensor(self.u(nm), sh, dt_))
            wout = T("wout", [128, 8, D], BF16)
            wff1 = T("wff1", [128, 8, 4 * D], BF16)
            wff2 = T("wff2", [128, 32, D], BF16)
            xcs = [T("xc%d" % i, [128, 8, N], F32) for i in range(2)]
            ycs = [T("yc%d" % i, [128, 8, N], BF16) for i in range(2)]
            hcs = [T("hc%d" % i, [128, 8, N], BF16) for i in range(2)]
            sqc = T("sqc", [128, 8, N], BF16)
            rstdc = T("rstdc", [128, N], F32)
            tmpring = Ring("tmpc", [T("tmpc%d" % i, [128, N], F32) for i in range(2)])
            rlring = Ring("rl", [T("rl%d" % i, [128, N], F32) for i in range(2)])
            hid = T("hid", [128, 32, N], BF16)
            for kt in range(8):
                S.dma("pool", wout[:, kt, :], self.w_out[l, kt * 128:(kt + 1) * 128, :], writes=[("wout", kt)])
            for kt in range(8):
                S.dma("pool", wff1[:, kt, :], self.w_ff1[l, kt * 128:(kt + 1) * 128, :], writes=[("wff1", kt)])
            for ft in range(32):
                S.dma("pool", wff2[:, ft, :], self.w_ff2[l, ft * 128:(ft + 1) * 128, :], writes=[("wff2", ft)])
            woutk = [("wout", kt) for kt in range(8)]
            wff1k = [("wff1", kt) for kt in range(8)]
            wff2k = [("wff2", ft) for ft in range(32)]
            srcv = src.ap().rearrange("(kt p) t -> p kt t", p=128)
            dstv = dst.ap().rearrange("(kt p) t -> p kt t", p=128)
            if self.dbg and "B" not in self.phases:
                ysrc = self.ycat_in.ap().rearrange("(kt p) t -> p kt t", p=128)
            else:
                ysrc = self.ycat.ap().rearrange("(kt p) t -> p kt t", p=128)
            self._psi = 0

            def nextps():
                i = self._psi
                self._psi = (i + 1) % 6
                return self.PS[i], ("ps", i)

            def stage1(c):
                xc, xk = xcs[c % 2], ("xc", c % 2)
                yc, yk = ycs[c % 2], ("yc", c % 2)
                S.dma("sp", xc[:], srcv[:, :, c * N:(c + 1) * N], writes=[xk])
                S.dma("pool", yc[:], ysrc[:, :, c * N:(c + 1) * N], writes=[yk])
                yield
                for ot in range(8):
                    ps, pk = nextps()
                    self.mm([(ps[:, :N], wout[:, kt, ot * 128:(ot + 1) * 128], yc[:, kt, :], kt == 0, kt == 7)
                             for kt in range(8)], [yk] + woutk, [pk])
                    yield
                    self.stt("dve", xc[:, ot, :], ps[:, :N], self.modv[:, 16 + ot:17 + ot], xc[:, ot, :],
                             ALU.mult, ALU.add, [pk, xk, "modv"], [xk])
                    yield
                yield from self.modulate_g(xc, xk, hcs[c % 2], ("hc", c % 2), sqc, "sqc", rstdc, "rstdc",
                                           tmpring, 7, N, 8, 24)

            for _ in stage1(0):
                pass
            ev = 0
            for c in range(NCH):
                self.bg = stage1(c + 1) if c + 1 < NCH else None
                xc, xk = xcs[c % 2], ("xc", c % 2)
                hc = hcs[c % 2]
                hks = [(("hc", c % 2), kt) for kt in range(8)]
                for ft in range(32):
                    ps, pk = nextps()
                    self.mm([(ps[:, :N], wff1[:, kt, ft * 128:(ft + 1) * 128], hc[:, kt, :], kt == 0, kt == 7)
                             for kt in range(8)], hks + wff1k, [pk])
                    rl, rk = rlring.next()
                    self.act(rl[:], ps[:, :N], AF.Relu, [pk], [rk])
                    self.tt("pool" if ev % 2 == 0 else "dve", hid[:, ft, :], rl[:], rl[:], ALU.mult, [rk], [("hid", ft)])
                    ev += 1
                    self.bg_run(2)
                hidk = [("hid", ft) for ft in range(32)]
                for ot in range(8):
                    ps, pk = nextps()
                    self.mm([(ps[:, :N], wff2[:, ft, ot * 128:(ot + 1) * 128], hid[:, ft, :], ft == 0, ft == 31)
                             for ft in range(32)], hidk + wff2k, [pk])
                    self.stt("dve", xc[:, ot, :], ps[:, :N], self.modv[:, 40 + ot:41 + ot], xc[:, ot, :],
                             ALU.mult, ALU.add, [pk, xk, "modv"], [xk])
                    self.bg_run(2)
                S.dma("sp", dstv[:, :, c * N:(c + 1) * N], xc[:], reads=[xk], writes=[("dst", c)])
                self.bg_drain()
            S.barrier()


def _prep_inputs(inp):
    cols = _col_index()
    w_in = np.asarray(inp["w_in"], np.float32)
    w_in_p = np.zeros((L, D, NCOL), np.float32)
    valid = cols >= 0
    w_in_p[:, :, valid] = w_in[:, :, cols[valid]]
    cmat, alibi, crow = _host_consts()
    pp = np.stack([_pp_layer(inp, l) for l in range(L)], 0)
    lam = np.asarray(inp["diff_lambda"], np.float32).reshape(L, 128)
    shared = dict(
        w_ada=np.ascontiguousarray(inp["w_ada"], np.float32), b_ada=np.ascontiguousarray(inp["b_ada"], np.float32),
        w_in=w_in_p, w_out=np.ascontiguousarray(inp["w_out"], np.float32),
        w_ff1=np.ascontiguousarray(inp["w_ff1"], np.float32), w_ff2=np.ascontiguousarray(inp["w_ff2"], np.float32),
        pp=pp, lam=lam, cmat=cmat, alibi=alibi, crow=crow,
        onesrow=np.ones((1, SEQ), np.float32), mask4=_mask4())
    maps = []
    x = np.asarray(inp["x"], np.float32)
    c = np.asarray(inp["c"], np.float32)
    for b in range(x.shape[0]):
        m = dict(shared)
        m["xT"] = np.ascontiguousarray(x[b].T)
        m["c8"] = np.ascontiguousarray(c[b].reshape(8, 128).T)
        maps.append(m)
    return maps


def kernel(**inputs):
    inp = {k: np.asarray(v) for k, v in inputs.items()}
    maps = _prep_inputs(inp)
    mk = MK()
    res = run_bass_kernel_spmd(mk.nc, maps, core_ids=list(range(len(maps))))
    out = np.stack([np.ascontiguousarray(r["outT"].T) for r in res.results], 0)
    return out.astype(np.float32)
```

```python
import numpy as np
import math
from contextlib import ExitStack
import concourse.bass as bass
import concourse.mybir as mybir
from concourse.bass_utils import run_bass_kernel_spmd

F32 = mybir.dt.float32
BF16 = mybir.dt.bfloat16
AF = mybir.ActivationFunctionType
ALU = mybir.AluOpType
AX = mybir.AxisListType

ENGS = ("pe", "act", "dve", "pool", "sp")

D = 1024
SEQ = 4096
L = 2
NFM = 34
NTM = 1024
NCOL = NFM * 128 + NTM
EPS = 1e-6
NPP = 56
import os
GLA_STOP = int(os.environ.get('GLA_STOP', '0'))
GLA_SKIP = os.environ.get('GLA_SKIP', '')
WARM = int(os.environ.get('WARM', '0'))


class Sched:
    def __init__(self, nc, n_dma_sems=24):
        self.nc = nc
        self.streams = {e: [] for e in ENGS}
        self.esem = {e: nc.alloc_semaphore("s_" + e) for e in ENGS}
        self.cnt = {e: 0 for e in ENGS}
        self.seen = {e: {} for e in ENGS}
        self.lastw = {}
        self.readers = {}
        self.dsems = {}
        self.dcnt = {}
        self.dnext = {}
        for q in ("sp", "act", "pool"):
            self.dsems[q] = [nc.alloc_semaphore("d_%s%d" % (q, i)) for i in range(n_dma_sems)]
            self.dcnt[q] = [0] * n_dma_sems
            self.dnext[q] = 0
        self.semh = {}
        for e in ENGS:
            self.semh[("e", e)] = self.esem[e]
        for q in self.dsems:
            for i, s in enumerate(self.dsems[q]):
                self.semh[("d", q, i)] = s
        self.n_waits = 0
        self.n_ops = 0

    def _deps(self, reads, writes):
        deps = {}

        def add(tok):
            if tok is None:
                return
            k, v = tok
            if deps.get(k, 0) < v:
                deps[k] = v

        for r in reads:
            add(self.lastw.get(r))
        for r in writes:
            add(self.lastw.get(r))
            for t in self.readers.get(r, ()):
                add(t)
        return deps

    def _emit_waits(self, e, deps):
        seen = self.seen[e]
        for k, v in deps.items():
            if seen.get(k, 0) >= v:
                continue
            if e == "pe" and k == ("e", "pe"):
                continue
            seen[k] = v
            h = self.semh[k]
            self.streams[e].append(lambda eng, h=h, v=v: eng.wait_ge(h, v))
            self.n_waits += 1

    def _commit(self, tok, reads, writes):
        for r in reads:
            lst = self.readers.setdefault(r, [])
            lst.append(tok)
            if len(lst) > 64:
                m = {}
                for k, v in lst:
                    if m.get(k, 0) < v:
                        m[k] = v
                self.readers[r] = list(m.items())
        for r in writes:
            self.lastw[r] = tok
            self.readers[r] = []

    def op(self, e, fn, reads=(), writes=()):
        deps = self._deps(reads, writes)
        self._emit_waits(e, deps)
        self.cnt[e] += 1
        sem = self.esem[e]
        self.streams[e].append(lambda eng, fn=fn, sem=sem: fn(eng).then_inc(sem, 1))
        tok = (("e", e), self.cnt[e])
        self._commit(tok, reads, writes)
        self.n_ops += 1
        return tok

    def pe_group(self, fns, reads=(), writes=()):
        deps = self._deps(reads, writes)
        self._emit_waits("pe", deps)
        self.cnt["pe"] += 1
        sem = self.esem["pe"]
        for fn in fns[:-1]:
            self.streams["pe"].append(lambda eng, fn=fn: fn(eng))
        fn = fns[-1]
        self.streams["pe"].append(lambda eng, fn=fn, sem=sem: fn(eng).then_inc(sem, 1))
        tok = (("e", "pe"), self.cnt["pe"])
        self._commit(tok, reads, writes)
        self.n_ops += len(fns)
        return tok

    def dma(self, q, out, in_, reads=(), writes=(), **kw):
        i = self.dnext[q]
        self.dnext[q] = (i + 1) % len(self.dsems[q])
        k = ("d", q, i)
        deps = self._deps(reads, writes)
        if self.dcnt[q][i] > 0:
            deps[k] = max(deps.get(k, 0), self.dcnt[q][i])
        self._emit_waits(q, deps)
        self.dcnt[q][i] += 16
        h = self.semh[k]
        self.streams[q].append(
            lambda eng, out=out, in_=in_, h=h, kw=kw: eng.dma_start(out=out, in_=in_, **kw).then_inc(h, 16))
        tok = (k, self.dcnt[q][i])
        self._commit(tok, reads, writes)
        return tok

    def barrier(self):
        allk = {}
        for e in ENGS:
            if self.cnt[e] > 0:
                allk[("e", e)] = self.cnt[e]
        for q in self.dsems:
            for i, c in enumerate(self.dcnt[q]):
                if c > 0:
                    allk[("d", q, i)] = c
        for e in ENGS:
            self._emit_waits(e, dict(allk))
        self.lastw.clear()
        self.readers.clear()

    def finish(self):
        self.barrier()
        nc = self.nc
        streams = self.streams
        with nc.Block() as block:
            @block.tensor
            def _(eng):
                for f in streams["pe"]:
                    f(eng)

            @block.scalar
            def _(eng):
                for f in streams["act"]:
                    f(eng)

            @block.vector
            def _(eng):
                for f in streams["dve"]:
                    f(eng)

            @block.gpsimd
            def _(eng):
                for f in streams["pool"]:
                    f(eng)

            @block.sync
            def _(eng):
                for f in streams["sp"]:
                    f(eng)


class Ring:
    def __init__(self, name, bufs):
        self.name = name
        self.bufs = bufs
        self.i = 0

    def next(self):
        i = self.i
        self.i = (i + 1) % len(self.bufs)
        return self.bufs[i], (self.name, i)


OFF = dict(hq=0, hf=256, hi=512, hg=768, dq=1024, dk=1280, dv=1536, fq=1792, fk=2048, fv=2304,
           fg=2560, ff=2816, mq=2820, mk=3076, mv=3332, mo=3588, mi=3844, mf=3848)
T_HQ, T_HF, T_HG, T_MQ, T_MK, T_MO, T_MI, T_MF = 0, 2, 4, 6, 8, 10, 12, 14
T_FQ, T_FK, T_FG, T_DQ, T_DK = 16, 20, 24, 26, 30
V_H, V_M, V_F, V_D = 0, 256, 512, 768


def _col_index():
    cols = []

    def rng(base, n):
        return list(range(base, base + n))

    for nm in ("hq", "hf", "hg", "mq", "mk", "mo"):
        cols += rng(OFF[nm], 256)
    for nm in ("mi", "mf"):
        for h in range(4):
            cols += [OFF[nm] + h] * 64
    for h in range(4):
        cols += rng(OFF["fq"] + 64 * h, 64) + [OFF["ff"] + h] + [-1] * 63
    for h in range(4):
        cols += rng(OFF["fk"] + 64 * h, 64) + [-1] * 64
    cols += rng(OFF["fg"], 256)
    for nm in ("dq", "dk"):
        for h in range(4):
            b = OFF[nm] + 64 * h
            cols += rng(b, 32) + [-1] * 32 + rng(b + 32, 32) + [-1] * 32
    assert len(cols) == NFM * 128
    for nm in ("hi", "mv", "fv", "dv"):
        cols += rng(OFF[nm], 256)
    assert len(cols) == NCOL
    return np.array(cols)


def _host_consts():
    p = np.arange(128)
    c = {}
    c["ident"] = np.eye(128, dtype=np.float32)
    c["blk64"] = (p[:, None] // 64 == p[None, :] // 64).astype(np.float32)
    in1 = (p < 32)
    in2 = (p >= 64) & (p < 96)
    c["blk32d"] = ((in1[:, None] & in1[None, :]) | (in2[:, None] & in2[None, :])).astype(np.float32)
    c["trimask"] = np.where(p[:, None] <= p[None, :], 0.0, -30000.0).astype(np.float32)
    c["glamask"] = ((p[:, None] // 64 == p[None, :] // 64) & (p[:, None] <= p[None, :])).astype(np.float32)
    slopes = 2.0 ** (-8.0 * np.arange(1, 5, dtype=np.float64) / 4)
    pos = (np.arange(32)[None, :] * 128 + p[:, None]).astype(np.float64)
    c["alibi"] = np.stack([slopes[h] * pos for h in range(4)], axis=1).astype(np.float32)
    c["crow"] = np.stack([-slopes[h] * np.arange(SEQ, dtype=np.float64) for h in range(4)], 0).astype(np.float32)
    cmat = np.concatenate([c["ident"], c["blk64"], c["blk32d"], c["trimask"], c["glamask"]], axis=1)
    return cmat.astype(np.float32), c["alibi"].reshape(128, 128).copy(), c["crow"]


def _mask4():
    p = np.arange(128)
    s_, t_ = p[:, None], p[None, :]
    d32 = ((s_ // 32 == t_ // 32) & (s_ <= t_)).astype(np.float32)
    ba = ((s_ // 64 == t_ // 64) & (s_ % 64 < 32) & (t_ % 64 >= 32)).astype(np.float32)
    return np.ascontiguousarray(np.concatenate([d32, ba], axis=1))


def _pp_layer(inp, l):
    pp = np.zeros((128, NPP), np.float32)
    p = np.arange(128)
    pp[:, 0:8] = inp["norm_mix_gain"][l].reshape(8, 128).T
    pp[:, 8:16] = inp["norm_ff_gain"][l].reshape(8, 128).T
    pp[:, 16:18] = inp["hg_lb_logits"][0].reshape(2, 128).T
    pp[:, 18:20] = inp["hg_lb_logits"][1].reshape(2, 128).T
    pp[:, 20] = inp["hg_norm_gain"][l][p % 64]
    for col, nm in ((21, "diff_qn_gain"), (22, "diff_kn_gain")):
        g = inp[nm][l]
        pp[0:32, col] = g
        pp[64:96, col] = g
    pp[:, 23] = inp["diff_sub_gain"][l][p % 64]
    pp[:, 24] = inp["fox_qn_gain"][l][p % 64]
    pp[:, 25] = inp["fox_kn_gain"][l][p % 64]
    for h in range(4):
        pp[:, 26 + h] = inp["fox_f_bias"][l][h]
    for tp in range(2):
        pp[:, 30 + tp] = inp["mlstm_i_bias"][l][2 * tp + p // 64]
        pp[:, 32 + tp] = inp["mlstm_f_bias"][l][2 * tp + p // 64]
        for j in range(4):
            pp[:, 34 + tp * 4 + j] = inp["mlstm_conv"][l][j, tp * 128 + p]
            pp[:, 42 + tp * 4 + j] = inp["mlstm_conv"][l][j, 256 + tp * 128 + p]
    return pp


class MK:
    def __init__(self, dbg=False, phases="0ABCD", nlayers=L, mixers="hmfd"):
        self.mixers = mixers
        self.dbg = dbg
        self.phases = phases
        self.nlayers = nlayers
        nc = bass.Bass("TRN2", target_bir_lowering=False)
        self.nc = nc
        self.S = Sched(nc)
        ik = "ExternalInput"
        sk = "ExternalOutput" if dbg else "Internal"
        dt = nc.dram_tensor
        self.xT = dt("xT", [D, SEQ], F32, kind=ik)
        self.c8 = dt("c8", [128, 8], F32, kind=ik)
        self.w_ada = dt("w_ada", [L, D, 6 * D], F32, kind=ik)
        self.b_ada = dt("b_ada", [L, 6 * D], F32, kind=ik)
        self.w_in = dt("w_in", [L, D, NCOL], F32, kind=ik)
        self.w_out = dt("w_out", [L, D, D], F32, kind=ik)
        self.w_ff1 = dt("w_ff1", [L, D, 4 * D], F32, kind=ik)
        self.w_ff2 = dt("w_ff2", [L, 4 * D, D], F32, kind=ik)
        self.pp = dt("pp", [L, 128, NPP], F32, kind=ik)
        self.lam = dt("lam", [L, 128], F32, kind=ik)
        self.cmat = dt("cmat", [128, 5 * 128], F32, kind=ik)
        self.alibi = dt("alibi", [128, 128], F32, kind=ik)
        self.crow = dt("crow", [4, SEQ], F32, kind=ik)
        self.onesrow = dt("onesrow", [1, SEQ], F32, kind=ik)
        self.mask4 = dt("mask4", [128, 256], F32, kind=ik)
        self.ycat_in = dt("ycat_in", [D, SEQ], F32, kind=ik) if dbg else None
        self.outT = dt("outT", [D, SEQ], F32, kind="ExternalOutput")
        self.xres = dt("xres", [D, SEQ], F32, kind=sk)
        self.zfm = dt("zfm", [NFM * 128, SEQ], F32, kind=sk)
        self.ztm = dt("ztm", [SEQ, NTM], BF16, kind=sk)
        self.ycat = dt("ycat", [D, SEQ], BF16, kind=sk)
        self.modd = dt("modd", [128, 48], F32, kind=sk)
        self.h2d = dt("h2d", [D, SEQ], BF16, kind="Internal")
        self.PS = [nc.alloc_psum_tensor("ps%d" % i, [128, 512], F32) for i in range(8)]
        self.build()

    def act(self, out, in_, func, R, W, bias=0.0, scale=1.0):
        return self.S.op("act", lambda e: e.activation(out=out, in_=in_, func=func, bias=bias, scale=scale), R, W)

    def ts(self, eng, out, in0, s1, s2, op0, op1, R, W):
        if s2 is None:
            return self.S.op(eng, lambda e: e.tensor_scalar(out, in0, s1, None, op0), R, W)
        return self.S.op(eng, lambda e: e.tensor_scalar(out, in0, s1, s2, op0, op1), R, W)

    def tt(self, eng, out, in0, in1, op, R, W):
        return self.S.op(eng, lambda e: e.tensor_tensor(out, in0, in1, op), R, W)

    def stt(self, eng, out, in0, sc, in1, op0, op1, R, W):
        return self.S.op(eng, lambda e: e.scalar_tensor_tensor(out, in0, sc, in1, op0, op1), R, W)

    def cp(self, eng, out, in_, R, W):
        if eng == "act":
            return self.act(out, in_, AF.Copy, R, W)
        return self.S.op(eng, lambda e: e.tensor_copy(out, in_), R, W)

    def mm(self, items, R, W):
        fns = []
        for (out, lhsT, rhs, st, sp) in items:
            fns.append(lambda e, out=out, lhsT=lhsT, rhs=rhs, st=st, sp=sp:
                       e.matmul(out, lhsT=lhsT, rhs=rhs, start=st, stop=sp))
        return self.S.pe_group(fns, R, W)

    def dump(self, name, ap, key, shape, dtype=F32):
        if not self.dbg:
            return
        t = self.nc.dram_tensor("dbg_" + name, list(shape), dtype, kind="ExternalOutput")
        self.S.dma("sp", t.ap(), ap, reads=[key], writes=[("dbg", name)])

    def u(self, name):
        self._uid = getattr(self, '_uid', 0) + 1
        return '%s_%d' % (name, self._uid)

    def sb(self, name, shape, dtype):
        return self.nc.alloc_sbuf_tensor(name, shape, dtype)

    def build(self):
        nc, S = self.nc, self.S
        self.cbf = self.sb("cbf", [128, 5 * 128], BF16)
        self.cf = self.sb("cf", [128, 128], F32)
        S.dma("pool", self.cbf[:], self.cmat.ap(), writes=["cbf"])
        S.dma("sp", self.cf[:], self.cmat[:, 0:128], writes=["cf"])
        self.cf2 = self.sb("cf2", [128, 256], F32)
        S.dma("sp", self.cf2[:], self.mask4.ap(), writes=["cf2"])
        self.mask4_f = self.cf2
        self.ident_bf = self.cbf[:, 0:128]
        self.blk64_bf = self.cbf[:, 128:256]
        self.blk32d_bf = self.cbf[:, 256:384]
        self.trimask_bf = self.cbf[:, 384:512]
        self.ident_f = self.cf[:, 0:128]
        self.ones_bf = self.sb("ones_bf", [128, 128], BF16)
        self.ones_f = self.sb("ones_f", [128, 128], F32)
        S.op("dve", lambda e: e.memset(self.ones_bf[:], 1.0), writes=["ones_bf"])
        S.op("dve", lambda e: e.memset(self.ones_f[:], 1.0), writes=["ones_f"])
        self.ppt = self.sb("ppt", [128, L, NPP], F32)
        for l in range(L):
            S.dma("sp", self.ppt[:, l, :], self.pp[l], writes=["ppt"])
        self.modv = self.sb("modv", [128, 48], F32)
        self.AB = self.sb("AB", [128, 16], F32)
        S.barrier()
        base = (nc.sbuf_base, nc.sbuf_top) if hasattr(nc, "sbuf_base") else None
        for l in range(self.nlayers):
            src = self.xT if l == 0 else self.xres
            dst = self.xres if l == 0 and self.nlayers > 1 else self.outT
            if "0" in self.phases:
                self.phase0(l)
            if "A" in self.phases:
                self.phaseA(l, src)
            if "B" in self.phases:
                self.phaseB(l)
            if "C" in self.phases:
                self.phaseCD(l, src, dst)
        S.finish()

    def phase0(self, l):
        nc, S = self.nc, self.S
        with nc.sbuf_tensor(self.u("cin"), [128, 8], F32) as cin, \
                nc.sbuf_tensor(self.u("ctmp"), [128, 8], F32) as ctmp, \
                nc.sbuf_tensor(self.u("cact"), [128, 8], F32) as cact, \
                nc.sbuf_tensor(self.u("wa0"), [128, 8, 512], F32) as wa0, \
                nc.sbuf_tensor(self.u("wa1"), [128, 8, 512], F32) as wa1, \
                nc.sbuf_tensor(self.u("modrow"), [1, 6 * D], F32) as modrow, \
                nc.sbuf_tensor(self.u("badar"), [1, 6 * D], F32) as badar:
            S.dma("sp", cin[:], self.c8.ap(), writes=["cin"])
            S.dma("sp", badar[:], self.b_ada[l:l + 1, :], writes=["badar"])
            self.act(ctmp[:], cin[:], AF.Exp, ["cin"], ["ctmp"], scale=-1.0)
            self.ts("dve", ctmp[:], ctmp[:], 1.0, None, ALU.add, None, ["ctmp"], ["ctmp"])
            S.op("dve", lambda e: e.reciprocal(ctmp[:], ctmp[:]), ["ctmp"], ["ctmp"])
            self.tt("dve", cact[:], cin[:], ctmp[:], ALU.mult, ["cin", "ctmp"], ["cact"])
            wring = Ring("wa", [wa0, wa1])
            for j in range(12):
                buf, bk = wring.next()
                S.dma("sp", buf[:], self.w_ada[l, :, j * 512:(j + 1) * 512].rearrange("(kt p) n -> p kt n", p=128),
                      writes=[bk])
                ps = self.PS[j % 2]
                pk = ("ps", j % 2)
                self.mm([(ps[0:1, :], cact[:, kt:kt + 1], buf[:, kt, :], kt == 0, kt == 7) for kt in range(8)],
                        ["cact", bk], [pk])
                self.tt("dve", modrow[0:1, j * 512:(j + 1) * 512], ps[0:1, :], badar[0:1, j * 512:(j + 1) * 512],
                        ALU.add, [pk, "badar"], ["modrow"])
            ps = self.PS[2]
            self.mm([(ps[:, j:j + 1], modrow[0:1, j * 128:(j + 1) * 128], self.ones_f[0:1, 0:1], True, True)
                     for j in range(48)], ["modrow", "ones_f"], [("ps", 2)])
            self.cp("dve", self.modv[:], ps[:, 0:48], [("ps", 2)], ["modv"])
            pp = self.ppt
            self.stt("dve", self.AB[:, 0:8], self.modv[:, 8:16], 1.0, pp[:, l, 0:8], ALU.add, ALU.mult,
                     ["modv", "ppt"], ["AB"])
            self.stt("dve", self.AB[:, 8:16], self.modv[:, 32:40], 1.0, pp[:, l, 8:16], ALU.add, ALU.mult,
                     ["modv", "ppt"], ["AB"])
            if self.dbg:
                S.dma("sp", self.modd.ap(), self.modv[:], reads=["modv"], writes=["modd"])
            S.barrier()

    def bg_run(self, n):
        for _ in range(n):
            if getattr(self, "bg", None) is None:
                return
            try:
                next(self.bg)
            except StopIteration:
                self.bg = None

    def bg_drain(self):
        while getattr(self, "bg", None) is not None:
            self.bg_run(64)

    def modulate_g(self, xc, xk, h, hk, sq, sqk, rstd, rk, tmpring, psidx, N, Acol, Bcol, on_act=False):
        ps = self.PS[psidx]
        pk = ("ps", psidx)
        for kt in range(8):
            if on_act:
                self.act(sq[:, kt, :N], xc[:, kt, :N], AF.Square, [xk], [(sqk, kt)])
            else:
                self.tt("pool", sq[:, kt, :N], xc[:, kt, :N], xc[:, kt, :N], ALU.mult, [xk], [(sqk, kt)])
            yield
        self.mm([(ps[:, :N], self.ones_bf[:], sq[:, kt, :N], kt == 0, kt == 7) for kt in range(8)],
                ["ones_bf"] + [(sqk, kt) for kt in range(8)], [pk])
        yield
        self.ts("dve", rstd[:, :N], ps[:, :N], 1.0 / D, EPS, ALU.mult, ALU.add, [pk], [rk])
        yield
        self.act(rstd[:, :N], rstd[:, :N], AF.Sqrt, [rk], [rk])
        yield
        self.S.op("dve", lambda e: e.reciprocal(rstd[:, :N], rstd[:, :N]), [rk], [rk])
        yield
        for kt in range(8):
            tmp, tk = tmpring.next()
            self.stt("dve", tmp[:, :N], xc[:, kt, :N], self.AB[:, Acol + kt:Acol + kt + 1], rstd[:, :N],
                     ALU.mult, ALU.mult, [xk, "AB", rk], [tk])
            yield
            if on_act:
                self.act(h[:, kt, :N], tmp[:, :N], AF.Identity, [tk, "modv"], [(hk, kt)],
                         bias=self.modv[:, Bcol + kt:Bcol + kt + 1])
            else:
                self.ts("pool", h[:, kt, :N], tmp[:, :N], self.modv[:, Bcol + kt:Bcol + kt + 1], None, ALU.add, None,
                        [tk, "modv"], [(hk, kt)])
            yield

    def phaseA(self, l, src):
        nc, S = self.nc, self.S
        with ExitStack() as es:
            T = lambda nm, sh, dt_: es.enter_context(nc.sbuf_tensor(self.u(nm), sh, dt_))
            win = T("win", [128, 8, NCOL], BF16)
            xa = [T("xa%d" % i, [128, 8, 512], F32) for i in range(2)]
            ha = [T("ha%d" % i, [128, 8, 512], BF16) for i in range(2)]
            sqa = T("sqa", [128, 8, 512], BF16)
            rstda = T("rstda", [128, 512], F32)
            tmpring = Ring("tmpa", [T("tmpa%d" % i, [128, 512], F32) for i in range(2)])
            string = Ring("sta", [T("sta%d" % i, [128, 512], F32) for i in range(4)])
            stbring = Ring("stb", [T("stb%d" % i, [128, 512], BF16) for i in range(2)])
            for kt in range(8):
                S.dma("pool", win[:, kt, :], self.w_in[l, kt * 128:(kt + 1) * 128, :], writes=[("win", kt)])
            wink = [("win", kt) for kt in range(8)]
            srcv = src.ap().rearrange("(kt p) t -> p kt t", p=128)

            def stage1(c):
                xc, xk = xa[c % 2], ("xa", c % 2)
                S.dma("sp", xc[:], srcv[:, :, c * 512:(c + 1) * 512], writes=[xk])
                yield
                yield from self.modulate_g(xc, xk, ha[c % 2], ("ha", c % 2), sqa, "sqa", rstda, "rstda",
                                           tmpring, 7, 512, 0, 0)

            for _ in stage1(0):
                pass
            psi = 0
            ev = 0
            for c in range(8):
                self.bg = stage1(c + 1) if c + 1 < 8 else None
                h = ha[c % 2]
                hks = [(("ha", c % 2), kt) for kt in range(8)]
                for ft in range(NFM):
                    ps = self.PS[psi]
                    pk = ("ps", psi)
                    psi = (psi + 1) % 6
                    self.mm([(ps[:], win[:, kt, ft * 128:(ft + 1) * 128], h[:, kt, :], kt == 0, kt == 7)
                             for kt in range(8)], hks + wink, [pk])
                    st, sk = string.next()
                    self.cp("act" if ev % 2 == 0 else "dve", st[:], ps[:], [pk], [sk])
                    ev += 1
                    S.dma("sp", self.zfm[ft * 128:(ft + 1) * 128, c * 512:(c + 1) * 512], st[:],
                          reads=[sk], writes=[("zfm", ft, c)])
                    self.bg_run(1)
                for tt in range(4):
                    for half in range(2):
                        ps = self.PS[psi]
                        pk = ("ps", psi)
                        psi = (psi + 1) % 6
                        c0 = NFM * 128 + half * 512
                        self.mm([(ps[:], h[:, kt, tt * 128:(tt + 1) * 128], win[:, kt, c0:c0 + 512], kt == 0, kt == 7)
                                 for kt in range(8)], hks + wink, [pk])
                        st, sk = stbring.next()
                        self.cp("act" if ev % 2 == 0 else "dve", st[:], ps[:], [pk], [sk])
                        ev += 1
                        r0 = c * 512 + tt * 128
                        S.dma("sp", self.ztm[r0:r0 + 128, half * 512:(half + 1) * 512], st[:],
                              reads=[sk], writes=[("ztm", c, tt, half)])
                        self.bg_run(1)
                self.bg_drain()
            S.barrier()

    def phaseB(self, l):
        S = self.S
        self.setupB(l)
        S.barrier()
        if "h" in self.mixers:
            self.gla(l, "hgrn")
            S.barrier()
        if "m" in self.mixers:
            self.gla(l, "mlstm")
            S.barrier()
        if "f" in self.mixers:
            self.attn(l, "fox")
            S.barrier()
        if "d" in self.mixers:
            self.attn(l, "diff")
            S.barrier()

    def setupB(self, l):
        nc, S = self.nc, self.S
        if not hasattr(self, "sv"):
            self.sv = self.sb("sv", [128, 16], F32)
            self.lamrow = self.sb("lamrow", [1, 256], F32)
            self.negpp = self.sb("negpp", [128, NPP], F32)
        sv, pp = self.sv, self.ppt
        self.ts("dve", self.negpp[:], pp[:, l, :], -1.0, None, ALU.mult, None, ["ppt"], ["negpp"])
        self.tt("dve", sv[:, 0:2], pp[:, l, 18:20], pp[:, l, 16:18], ALU.subtract, ["ppt"], ["sv"])
        self.act(sv[:, 0:2], sv[:, 0:2], AF.Exp, ["sv"], ["sv"], scale=-1.0)
        self.ts("dve", sv[:, 0:2], sv[:, 0:2], 1.0, None, ALU.add, None, ["sv"], ["sv"])
        S.op("dve", lambda e: e.reciprocal(sv[:, 0:2], sv[:, 0:2]), ["sv"], ["sv"])
        self.ts("dve", sv[:, 0:2], sv[:, 0:2], float(l), None, ALU.mult, None, ["sv"], ["sv"])
        self.ts("dve", sv[:, 2:4], sv[:, 0:2], -1.0, 1.0, ALU.mult, ALU.add, ["sv"], ["sv"])
        lr = self.lamrow
        S.dma("sp", lr[0:1, 0:128], self.lam[l:l + 1, :], writes=["lamrow"])
        self.tt("dve", lr[0:1, 128:160], lr[0:1, 0:32], lr[0:1, 32:64], ALU.mult, ["lamrow"], ["lamrow"])
        self.tt("dve", lr[0:1, 160:192], lr[0:1, 64:96], lr[0:1, 96:128], ALU.mult, ["lamrow"], ["lamrow"])
        S.op("dve", lambda e: e.tensor_reduce(lr[0:1, 192:193], lr[0:1, 128:160], AX.X, ALU.add), ["lamrow"], ["lamrow"])
        S.op("dve", lambda e: e.tensor_reduce(lr[0:1, 193:194], lr[0:1, 160:192], AX.X, ALU.add), ["lamrow"], ["lamrow"])
        self.act(lr[0:1, 192:194], lr[0:1, 192:194], AF.Exp, ["lamrow"], ["lamrow"])
        import math
        lam_init = 0.8 - 0.6 * math.exp(-0.3 * l)
        self.lam_init = lam_init
        self.tt("dve", lr[0:1, 194:195], lr[0:1, 193:194], lr[0:1, 192:193], ALU.subtract, ["lamrow"], ["lamrow"])
        self.ts("dve", lr[0:1, 194:195], lr[0:1, 194:195], -lam_init, None, ALU.add, None, ["lamrow"], ["lamrow"])
        ps = self.PS[0]
        self.mm([(ps[:, 0:1], self.ones_f[0:1, 0:128], lr[0:1, 194:195], True, True)], ["lamrow", "ones_f"], [("ps", 0)])
        self.cp("dve", sv[:, 4:5], ps[:, 0:1], [("ps", 0)], ["sv"])

    def sigmoid_inplace(self, t, key):
        self.act(t, t, AF.Exp, [key], [key], scale=-1.0)
        self.ts("dve", t, t, 1.0, None, ALU.add, None, [key], [key])
        self.S.op("dve", lambda e: e.reciprocal(t, t), [key], [key])

    def rstd_from_ps(self, out, ps, pk, key, mult, add):
        self.act(out, ps, AF.Ln, [pk], [key], scale=mult, bias=add)
        self.act(out, out, AF.Exp, [key], [key], scale=-0.5)

    def sigmoid_act(self, t, key):
        self.act(t, t, AF.Sigmoid, [key], [key])

    def attn(self, l, kind):
        nc, S = self.nc, self.S
        fox = kind == "fox"
        pp, sv = self.ppt, self.sv
        with ExitStack() as es:
            T = lambda nm, sh, dt_: es.enter_context(nc.sbuf_tensor(self.u(nm), sh, dt_))
            qraws = [T("qraw%d" % i, [128, SEQ], F32) for i in range(2)]
            kraws = [T("kraw%d" % i, [128, SEQ], F32) for i in range(2)]
            qps = [T("qp%d" % i, [128, SEQ], BF16) for i in range(2)]
            kps = [T("kp%d" % i, [128, SEQ], BF16) for i in range(2)]
            Vps = [T("Vp%d" % i, [128, 32, 128], BF16) for i in range(2)]
            negbs = [T("negb%d" % i, [128, 32], F32) for i in range(2)]
            ones4k = T("ones4k", [128, SEQ], F32) if fox else None
            sqring = Ring("sqt", [T("sqt%d" % i, [128, 512], BF16) for i in range(2)])
            rsring = Ring("rst", [T("rst%d" % i, [128, 512], F32) for i in range(2)])
            pring = Ring("pT", [T("pT%d" % i, [128, 512], BF16) for i in range(4)])
            rrow = T("rrow", [128, 2, 512], F32)
            ocp = T("ocp", [128, 2, 512], F32)
            bcs = T("bcs", [128, 2, 512], F32)
            osb = T("osb", [128, 512], F32)
            osb2 = T("osb2", [128, 512], F32)
            esq = T("esq", [128, 512], BF16)
            erst = T("erst", [128, 512], F32)
            gch = T("gch", [128, 512], F32)
            yst = T("yst", [128, 512], BF16)
            if fox:
                S.op("pool", lambda e: e.memset(ones4k[:], 1.0), writes=["ones4k"])
            sring = Ring("pss", [self.PS[0], self.PS[1], self.PS[2], self.PS[3]])
            if fox:
                ranges = [(0, 64)]
                nd = 64.0
                gq, gk = pp[:, l, 24:25], pp[:, l, 25:26]
                lhs_n = self.ones_bf[0:64, 0:64]
                nrows = 64
                KR = [(0, 65)]
            else:
                ranges = [(0, 32), (64, 96)]
                nd = 32.0
                gq, gk = pp[:, l, 21:22], pp[:, l, 22:23]
                lhs_n = self.blk32d_bf
                nrows = 128
                KR = [(0, 33), (64, 97)]
            nh = len(KR)
            M = 128
            ops_ = [self.PS[4], self.PS[5]]
            opk = [("ps", 4), ("ps", 5)]

            def prep(h):
                b = h % 2
                qraw, kraw, qp, kp, Vp, negb = qraws[b], kraws[b], qps[b], kps[b], Vps[b], negbs[b]
                kq, kk, kqp, kkp, kv, kn = ("qraw", b), ("kraw", b), ("qp", b), ("kp", b), ("Vp", b), ("negb", b)
                par = h % 2
                srow = 64 if par == 0 else 0
                voff = 0 if par == 0 else 64
                tq = (T_FQ if fox else T_DQ) + h
                tk = (T_FK if fox else T_DK) + h
                S.dma("sp", qraw[:], self.zfm[tq * 128:(tq + 1) * 128, :], writes=[kq])
                S.dma("sp", kraw[:], self.zfm[tk * 128:(tk + 1) * 128, :], writes=[kk])
                yield
                S.op("pool", lambda e: e.memset(Vp[:], 0.0), writes=[kv])
                vcol = (V_F if fox else V_D) + 64 * h
                S.dma("sp", Vp[:, :, voff:voff + 64],
                      self.ztm[:, vcol:vcol + 64].rearrange("(j p) c -> p j c", p=128), reads=[kv], writes=[kv])
                S.op("pool", lambda e, c1=srow: e.memset(Vp[:, :, c1:c1 + 64], 1.0), reads=[kv], writes=[kv])
                yield
                for c in range(8):
                    cs = slice(c * 512, (c + 1) * 512)
                    for (raw, rk_, dstt, dk_, g, isq) in ((qraw, kq, qp, kqp, gq, True), (kraw, kk, kp, kkp, gk, False)):
                        sqt, sqk = sqring.next()
                        rst, rsk = rsring.next()
                        self.tt("pool", sqt[0:nrows, :], raw[0:nrows, cs], raw[0:nrows, cs], ALU.mult, [rk_], [sqk])
                        yield
                        ps = self.PS[7]
                        self.mm([(ps[0:nrows, :], lhs_n, sqt[0:nrows, :], True, True)], [sqk, "cbf", "ones_bf"], [("ps", 7)])
                        yield
                        if isq:
                            self.act(rst[0:nrows, :], ps[0:nrows, :], AF.Ln, [("ps", 7)], [rsk], scale=1.0, bias=nd * EPS)
                        else:
                            self.act(rst[0:nrows, :], ps[0:nrows, :], AF.Ln, [("ps", 7)], [rsk], scale=1.0 / nd, bias=EPS)
                        yield
                        self.act(rst[0:nrows, :], rst[0:nrows, :], AF.Exp, [rsk], [rsk], scale=-0.5)
                        yield
                        for (a, b_) in ranges:
                            self.stt("dve", dstt[a:b_, cs], raw[a:b_, cs], g[a:b_, :], rst[a:b_, :], ALU.mult, ALU.mult,
                                     [rk_, rsk, "ppt"], [dk_])
                        yield
                if fox:
                    fr = qraw[64:65, :]
                    self.act(fr, fr, AF.Exp, [kq], [kq], scale=-1.0, bias=self.negpp[64:65, 26 + h:27 + h])
                    yield
                    self.act(fr, fr, AF.Ln, [kq], [kq], bias=1.0)
                    yield
                    S.op("dve", lambda e: e.tensor_tensor_scan(fr, ones4k[64:65, :], fr, 0.0, ALU.mult, ALU.subtract),
                         [kq, "ones4k"], [kq])
                    yield
                    self.cp("dve", qp[64:65, :], fr, [kq], [kqp])
                    S.op("pool", lambda e: e.memset(kp[64:65, :], 1.0), reads=[kkp], writes=[kkp])
                    yield
                    ps = self.PS[7]
                    S.pe_group([lambda e, j=j, ps=ps: e.transpose(ps[:, j:j + 1], qraw[64:65, j * 128:(j + 1) * 128],
                                                                  self.ident_f[64:65, 64:65]) for j in range(32)],
                               [kq, "cf"], [("ps", 7)])
                    yield
                    self.ts("dve", negb[:], ps[:, 0:32], -1.0, None, ALU.mult, None, [("ps", 7)], [kn])
                    yield
                else:
                    for r0 in (32, 96):
                        S.dma("pool", qp[r0:r0 + 1, :], self.crow[h:h + 1, :], reads=[kqp], writes=[kqp])
                        S.dma("pool", kp[r0:r0 + 1, :], self.onesrow[0:1, :], reads=[kkp], writes=[kkp])
                    S.dma("sp", negb[:], self.alibi[:, h * 32:(h + 1) * 32], writes=[kn])
                    yield

            def main(h):
                b = h % 2
                qp, kp, Vp, negb = qps[b], kps[b], Vps[b], negbs[b]
                kqp, kkp, kv, kn = ("qp", b), ("kp", b), ("Vp", b), ("negb", b)
                par = h % 2
                pb = 64 * par
                srow = 64 if par == 0 else 0

                def emit_qk(c, j, a):
                    dj = j - 4 * c
                    n0 = 128 * dj if dj > 0 else 0
                    k0, k1 = KR[a]
                    ps_s, sk_ = sring.next()
                    items = [(ps_s[:, n0:512], kp[k0:k1, j * 128:(j + 1) * 128],
                              qp[k0:k1, c * 512 + n0:(c + 1) * 512], True, dj < 0)]
                    if nh == 1 and WARM:
                        items = [items[0], items[0]]
                    if dj >= 0:
                        items.append((ps_s[:, n0:n0 + 128], self.ident_bf, self.trimask_bf, False, True))
                    self.mm(items, [kkp, kqp, "cbf"], [sk_])
                    pT, pk_ = pring.next()
                    self.act(pT[:, n0:512], ps_s[:, n0:512], AF.Exp, [sk_, kn], [pk_], bias=negb[:, j:j + 1])
                    return pT, pk_, n0

                def emit_pv(c, j, a, pT, pk_, n0):
                    nj = 4 * c + 4
                    self.mm([(ops_[a][0:M, n0:512], Vp[:, j, 0:M], pT[:, n0:512], j == 0, j == nj - 1)],
                            [kv, pk_], [opk[a]])
                    if j == nj - 1 and a == nh - 1:
                        epilogue(c)

                def epilogue(c):
                    cs = slice(c * 512, (c + 1) * 512)
                    for a in range(nh):
                        self.cp("dve", ocp[:, a, :], ops_[a][:, :], [opk[a]], [("ocp", a)])
                    for a in range(nh):
                        S.op("dve", lambda e, a=a, srow=srow: e.reciprocal(rrow[srow:srow + 64, a, :], ocp[srow:srow + 64, a, :]),
                             [("ocp", a)], [("rrow", a)])
                        psb = self.PS[6]
                        self.mm([(psb[:, :], self.ones_f[srow:srow + 1, 0:128], rrow[srow:srow + 1, a, :], True, True)],
                                [("rrow", a), "ones_f"], [("ps", 6)])
                        self.cp("act", bcs[pb:pb + 64, a, :], psb[pb:pb + 64, :], [("ps", 6)], [("bcs", a)])
                    rows = slice(pb, pb + 64)
                    if fox:
                        tg = T_FG + h // 2
                        S.dma("sp", gch[rows, :], self.zfm[tg * 128 + pb:tg * 128 + pb + 64, cs], writes=["gch"])
                        self.sigmoid_inplace(gch[rows, :], "gch")
                        self.tt("dve", osb[rows, :], ocp[rows, 0, :], bcs[rows, 0, :], ALU.mult, [("ocp", 0), ("bcs", 0)], ["osb"])
                        self.tt("pool", yst[rows, :], osb[rows, :], gch[rows, :], ALU.mult, ["osb", "gch"], ["yst"])
                        S.dma("sp", self.ycat[512 + 64 * h:512 + 64 * h + 64, cs], yst[rows, :], reads=["yst"],
                              writes=[("ycat", kind, h, c)])
                    else:
                        self.tt("dve", osb[rows, :], ocp[rows, 0, :], bcs[rows, 0, :], ALU.mult, [("ocp", 0), ("bcs", 0)], ["osb"])
                        self.tt("dve", osb2[rows, :], ocp[rows, 1, :], bcs[rows, 1, :], ALU.mult, [("ocp", 1), ("bcs", 1)], ["osb2"])
                        self.stt("dve", osb[rows, :], osb2[rows, :], sv[rows, 4:5], osb[rows, :], ALU.mult, ALU.add,
                                 ["osb", "osb2", "sv"], ["osb"])
                        self.tt("pool", esq[rows, :], osb[rows, :], osb[rows, :], ALU.mult, ["osb"], ["esq"])
                        ps = self.PS[6]
                        self.mm([(ps[:, :], self.blk64_bf[rows, :], esq[rows, :], True, True)], ["esq", "cbf"], [("ps", 6)])
                        self.act(erst[rows, :], ps[rows, :], AF.Ln, [("ps", 6)], ["erst"], scale=1.0 / 64.0, bias=EPS)
                        self.act(erst[rows, :], erst[rows, :], AF.Exp, ["erst"], ["erst"], scale=-0.5)
                        self.stt("dve", osb[rows, :], osb[rows, :], pp[rows, l, 23:24], erst[rows, :], ALU.mult, ALU.mult,
                                 ["osb", "erst", "ppt"], ["osb"])
                        self.act(yst[rows, :], osb[rows, :], AF.Copy, ["osb"], ["yst"], scale=float(1.0 - self.lam_init))
                        S.dma("sp", self.ycat[256 + 64 * h:256 + 64 * h + 64, cs], yst[rows, :], reads=["yst"],
                              writes=[("ycat", kind, h, c)])

                steps = [(c, j, a) for c in range(8) for j in range(4 * c + 4) for a in range(nh)]
                LAG = 3
                pend = []
                for st in steps:
                    pend.append((st, emit_qk(*st)))
                    if len(pend) > LAG:
                        st0, info = pend.pop(0)
                        emit_pv(*st0, *info)
                    self.bg_run(1)
                while pend:
                    st0, info = pend.pop(0)
                    emit_pv(*st0, *info)

            for _ in prep(0):
                pass
            for h in range(4):
                self.bg = prep(h + 1) if h + 1 < 4 else None
                main(h)
                self.bg_drain()

    def gla(self, l, kind):
        nc, S = self.nc, self.S
        hg = kind == "hgrn"
        pp, sv = self.ppt, self.sv
        NV = 128 if hg else 256
        CL = 40.0
        with ExitStack() as es:
            T = lambda nm, sh, dt_: es.enter_context(nc.sbuf_tensor(self.u(nm), sh, dt_))
            BR = T("BR", [128, 2, 64, 64], F32)
            B3 = BR[:, 0]
            R3 = BR[:, 1]
            qx = T("qx", [128, SEQ], F32)
            kx = T("kx", [128, SEQ], F32)
            tmp = T("tmp", [128, SEQ], F32)
            ones4k = T("ones4k", [128, SEQ], F32)
            Qs = T("Qs", [128, SEQ], BF16)
            Qb = T("Qb", [128, SEQ], BF16)
            Kb = T("Kb", [128, SEQ], BF16)
            Q32 = T("Q32", [128, SEQ], BF16)
            K32 = T("K32", [128, SEQ], BF16)
            K2 = T("K2", [128, SEQ], BF16)
            K2tm = T("K2tm", [128, 32, 128], BF16)
            Vx = T("Vx", [128, 32, 128], BF16)
            sm = T("sm", [128, 6, 64], F32)
            sm32 = T("sm32", [128, 128], F32)
            St = T("St", [128, NV], F32)
            Sall = BR[:].rearrange("p a b c -> p (a b c)").bitcast(BF16)[:, 0:64 * NV].rearrange("p (n v) -> p n v", v=NV)
            At0 = T("At0", [128, 512], BF16)
            At1 = T("At1", [128, 512], BF16)
            osb = T("osb", [128, 512], F32)
            dsb = T("dsb", [128, 512], F32) if not hg else None
            sqt = T("sqt", [128, 512], BF16) if hg else None
            rst = T("rst", [128, 512], F32) if hg else None
            gch = T("gch", [128, 512], F32)
            gch2 = T("gch2", [128, 512], F32) if hg else None
            yst = T("yst", [128, 512], BF16)
            B2 = B3[:].rearrange("p a b -> p (a b)")
            R2 = R3[:].rearrange("p a b -> p (a b)")
            B4 = B3[:].rearrange("p a (h b) -> p (a h) b", h=2)
            R4 = R3[:].rearrange("p a (h b) -> p (a h) b", h=2)
            S.op("pool", lambda e: e.memset(ones4k[:], 1.0), writes=["ones4k"])
            bprev, blast, b31, dch = sm[:, 0, :], sm[:, 1, :], sm[:, 2, :], sm[:, 3, :]
            mask4 = self.mask4_f
            atring = Ring("At", [At0, At1])

            def bc64(i):
                return sm[:, i, :].unsqueeze(2).to_broadcast([128, 64, 64])

            for tp in range(2):
                if hg:
                    tq, tf = T_HQ + tp, T_HF + tp
                    S.dma("sp", tmp[:], self.zfm[tf * 128:(tf + 1) * 128, :], writes=["tmp"])
                    S.dma("sp", qx[:], self.zfm[tq * 128:(tq + 1) * 128, :], writes=["qx"])
                    self.sigmoid_act(tmp[:], "tmp")
                    self.ts("dve", kx[:], tmp[:], sv[:, 2 + tp:3 + tp], sv[:, tp:tp + 1], ALU.mult, ALU.add,
                            ["tmp", "sv"], ["kx"])
                    self.act(B2, kx[:], AF.Ln, ["kx"], ["B"])
                    self.ts("dve", kx[:], kx[:], -1.0, 1.0, ALU.mult, ALU.add, ["kx"], ["kx"])
                    S.op("dve", lambda e: e.tensor_tensor_scan(B2, ones4k[:], B2, 0.0, ALU.mult, ALU.add),
                         ["B", "ones4k"], ["B"])
                    qscale = 1.0
                else:
                    for (tsrc, dst_, dk_, cb) in ((T_MQ + tp, qx, "qx", 34 + tp * 4), (T_MK + tp, kx, "kx", 42 + tp * 4)):
                        S.dma("sp", R2, self.zfm[tsrc * 128:(tsrc + 1) * 128, :], writes=["R"])
                        self.ts("dve", tmp[:], R2, pp[:, l, cb + 3:cb + 4], None, ALU.mult, None, ["R", "ppt"], ["tmp"])
                        for j in (2, 1, 0):
                            sh = 3 - j
                            self.stt("dve", tmp[:, sh:], R2[:, 0:SEQ - sh], pp[:, l, cb + j:cb + j + 1], tmp[:, sh:],
                                     ALU.mult, ALU.add, ["R", "tmp", "ppt"], ["tmp"])
                        self.act(dst_[:], tmp[:], AF.Silu, ["tmp"], [dk_])
                    tf, ti = T_MF + tp, T_MI + tp
                    S.dma("sp", B2, self.zfm[tf * 128:(tf + 1) * 128, :], reads=["B"], writes=["B"])
                    S.dma("sp", tmp[:], self.zfm[ti * 128:(ti + 1) * 128, :], reads=["tmp"], writes=["tmp"])
                    self.act(B2, B2, AF.Exp, ["B", "negpp"], ["B"], scale=-1.0, bias=self.negpp[:, 32 + tp:33 + tp])
                    self.act(B2, B2, AF.Ln, ["B"], ["B"], bias=1.0)
                    S.op("dve", lambda e: e.tensor_tensor_scan(B2, ones4k[:], B2, 0.0, ALU.mult, ALU.subtract),
                         ["B", "ones4k"], ["B"])
                    self.ts("dve", tmp[:], tmp[:], pp[:, l, 30 + tp:31 + tp], None, ALU.add, None, ["tmp", "ppt"], ["tmp"])
                    qscale = 0.125
                self.cp("dve", blast, B3[:, :, 63], ["B"], ["sm"])
                self.cp("dve", b31, B3[:, :, 31], ["B"], ["sm"])
                S.op("dve", lambda e: e.memset(sm[:, 0, 0:1], 0.0), ["sm"], ["sm"])
                self.cp("dve", bprev[:, 1:64], blast[:, 0:63], ["sm"], ["sm"])
                self.cp("dve", sm32[:], B4[:, :, 15], ["B"], ["sm32"])
                self.tt("dve", dch, blast, bprev, ALU.subtract, ["sm"], ["sm"])
                self.act(dch, dch, AF.Exp, ["sm"], ["sm"])
                bc32 = sm32[:].unsqueeze(2).to_broadcast([128, 128, 32])

                def emit(dst_, dk_, src, sk_, with_i, scale):
                    if with_i:
                        self.tt("dve", R2, R2, tmp[:], ALU.add, ["R", "tmp"], ["R"])
                    self.act(R2, R2, AF.Exp, ["R"], ["R"])
                    if scale == 1.0:
                        self.tt("dve", dst_[:], src[:], R2, ALU.mult, [sk_, "R"], [dk_])
                    else:
                        self.stt("dve", dst_[:], src[:], scale, R2, ALU.mult, ALU.mult, [sk_, "R"], [dk_])

                wi = not hg
                self.tt("dve", R3[:], B3[:], bc64(0), ALU.subtract, ["B", "sm"], ["R"])
                emit(Qs, "Qs", qx, "qx", False, qscale)
                self.tt("dve", R3[:], B3[:], bc64(1), ALU.subtract, ["B", "sm"], ["R"])
                self.ts("dve", R2, R2, -1.0, None, ALU.mult, None, ["R"], ["R"])
                emit(K2, "K2", kx, "kx", wi, 1.0)
                self.tt("dve", R3[:], B3[:], bc64(2), ALU.subtract, ["B", "sm"], ["R"])
                self.ts("dve", R2, R2, 0.0, None, ALU.min, None, ["R"], ["R"])
                emit(Qb, "Qb", qx, "qx", False, qscale)
                self.tt("dve", R3[:], B3[:], bc64(2), ALU.subtract, ["B", "sm"], ["R"])
                self.ts("dve", R2, R2, -1.0, 0.0, ALU.mult, ALU.min, ["R"], ["R"])
                emit(Kb, "Kb", kx, "kx", wi, 1.0)
                self.tt("dve", R4, B4, bc32, ALU.subtract, ["B", "sm32"], ["R"])
                self.ts("dve", R2, R2, CL, -CL, ALU.min, ALU.max, ["R"], ["R"])
                emit(Q32, "Q32", qx, "qx", False, qscale)
                self.tt("dve", R4, B4, bc32, ALU.subtract, ["B", "sm32"], ["R"])
                self.ts("dve", R2, R2, -1.0, CL, ALU.mult, ALU.min, ["R"], ["R"])
                self.ts("dve", R2, R2, -CL, None, ALU.max, None, ["R"], ["R"])
                emit(K32, "K32", kx, "kx", wi, 1.0)
                psT = self.PS[7][:, 0:64].bitcast(BF16)
                for m in range(32):
                    S.pe_group([lambda e, m=m: e.transpose(psT, K2[:, m * 128:(m + 1) * 128], self.ident_bf)],
                               ["K2", "cbf"], [("ps", 7)])
                    self.cp("act" if m % 2 == 0 else "dve", K2tm[:, m, :], psT, [("ps", 7)], [("K2tm", m)])
                voff = (V_H if hg else V_M) + tp * 128
                S.dma("sp", Vx[:, :, 0:128], self.ztm[:, voff:voff + 128].rearrange("(j p) c -> p j c", p=128),
                      writes=["Vx"])
                S.barrier()
                S.op("pool", lambda e: e.memset(St[:], 0.0), writes=["St"])
                S.op("pool", lambda e: e.memset(Sall[:, 0, :], 0.0), writes=[("Sall", 0)])
                pdr = [(self.PS[5], ("ps", 5)), (self.PS[7], ("ps", 7))]
                for n in range(64):
                    m, r = n // 2, n % 2
                    psd, pdk = pdr[n % 2]
                    items = [(psd[:, 0:128], K2tm[64 * r:64 * r + 64, m, :], Vx[64 * r:64 * r + 64, m, :], True, True)]
                    if not hg:
                        items.append((psd[:, 128:256], K2tm[64 * r:64 * r + 64, m, :], self.ones_bf[64 * r:64 * r + 64, :], True, True))
                    self.mm(items, [("K2tm", m), "Vx", "ones_bf"], [pdk])
                    self.stt("dve", St[:], St[:], dch[:, n:n + 1], psd[:, 0:NV], ALU.mult, ALU.add,
                             ["St", "sm", pdk], ["St"])
                    if n < 63:
                        self.cp("pool" if n % 2 == 0 else "act", Sall[:, n + 1, :], St[:], ["St"], [("Sall", n + 1)])
                NE, NO, DE, DO = self.PS[0], self.PS[1], self.PS[2], self.PS[3]
                allps = [("ps", 0), ("ps", 1), ("ps", 2), ("ps", 3)]
                for m in range(32):
                    cm = (m % 4) * 128
                    ms = slice(m * 128, (m + 1) * 128)
                    psa, psa2 = self.PS[4], self.PS[6]
                    self.mm([(psa[:, 0:128], K32[0:64, ms], Q32[0:64, ms], True, True)], ["K32", "Q32"], [("ps", 4)])
                    self.mm([(psa2[:, 0:128], K32[64:128, ms], Q32[64:128, ms], True, True)], ["K32", "Q32"], [("ps", 6)])
                    self.mm([(psa[:, 128:256], Kb[0:64, ms], Qb[0:64, ms], True, True)], ["Kb", "Qb"], [("ps", 4)])
                    self.mm([(psa2[:, 128:256], Kb[64:128, ms], Qb[64:128, ms], True, True)], ["Kb", "Qb"], [("ps", 6)])
                    At, ak = atring.next()
                    self.tt("dve", At[:, 0:256], psa[:, 0:256], mask4[:, 0:256], ALU.mult, [("ps", 4), "cf2"], [ak])
                    self.tt("dve", At[:, 256:512], psa2[:, 0:256], mask4[:, 0:256], ALU.mult, [("ps", 6), "cf2", ak], [ak])
                    items = [(NE[:, cm:cm + 128], Vx[:, m, 0:128], At[:, 0:128], True, False),
                             (NE[:, cm:cm + 128], Vx[:, m, 0:128], At[:, 128:256], False, False),
                             (NO[:, cm:cm + 128], Vx[:, m, 0:128], At[:, 256:384], True, False),
                             (NO[:, cm:cm + 128], Vx[:, m, 0:128], At[:, 384:512], False, False)]
                    if not hg:
                        items += [(DE[:, cm:cm + 128], self.ones_bf[:, :], At[:, 0:128], True, False),
                                  (DE[:, cm:cm + 128], self.ones_bf[:, :], At[:, 128:256], False, False),
                                  (DO[:, cm:cm + 128], self.ones_bf[:, :], At[:, 256:384], True, False),
                                  (DO[:, cm:cm + 128], self.ones_bf[:, :], At[:, 384:512], False, False)]
                    self.mm(items, ["Vx", ak], allps)
                    for r in range(2):
                        n = 2 * m + r
                        ns = slice(n * 64, (n + 1) * 64)
                        cn = cm + 64 * r
                        sbt = Sall[:, n, :]
                        items = [(NE[:, cn:cn + 64], sbt[0:64, 0:128], Qs[0:64, ns], False, True),
                                 (NO[:, cn:cn + 64], sbt[64:128, 0:128], Qs[64:128, ns], False, True)]
                        if not hg:
                            items += [(DE[:, cn:cn + 64], sbt[0:64, 128:256], Qs[0:64, ns], False, True),
                                      (DO[:, cn:cn + 64], sbt[64:128, 128:256], Qs[64:128, ns], False, True)]
                        self.mm(items, [("Sall", n), "Qs"], allps)
                    if m % 4 == 3:
                        c = m // 4
                        cs = slice(c * 512, (c + 1) * 512)
                        tg = (T_HG if hg else T_MO) + tp
                        S.dma("sp", gch[:], self.zfm[tg * 128:(tg + 1) * 128, cs], writes=["gch"])
                        if hg:
                            self.cp("act", osb[0:64, :], NE[0:64, :], [("ps", 0)], ["osb"])
                            self.cp("act", osb[64:128, :], NO[64:128, :], [("ps", 1), "osb"], ["osb"])
                            self.tt("pool", sqt[:], osb[:], osb[:], ALU.mult, ["osb"], ["sqt"])
                            ps = self.PS[7]
                            self.mm([(ps[:], self.blk64_bf, sqt[:], True, True)], ["sqt", "cbf"], [("ps", 7)])
                            self.rstd_from_ps(rst[:], ps[:], ("ps", 7), "rst", 1.0 / 64.0, EPS)
                            self.stt("dve", osb[:], osb[:], pp[:, l, 20:21], rst[:], ALU.mult, ALU.mult,
                                     ["osb", "rst", "ppt"], ["osb"])
                            self.act(gch[:], gch[:], AF.Silu, ["gch"], ["gch"])
                            self.tt("dve", yst[:], osb[:], gch[:], ALU.mult, ["osb", "gch"], ["yst"])
                            S.dma("sp", self.ycat[tp * 128:(tp + 1) * 128, cs], yst[:], reads=["yst"],
                                  writes=[("ycat", kind, tp, c)])
                        else:
                            self.cp("dve", dsb[0:64, :], DE[0:64, :], [("ps", 2)], ["dsb"])
                            self.cp("dve", dsb[64:128, :], DO[64:128, :], [("ps", 3), "dsb"], ["dsb"])
                            self.stt("dve", dsb[:], dsb[:], -1.0, dsb[:], ALU.mult, ALU.max, ["dsb"], ["dsb"])
                            self.ts("dve", dsb[:], dsb[:], 1.0, None, ALU.max, None, ["dsb"], ["dsb"])
                            S.op("dve", lambda e: e.reciprocal(dsb[:], dsb[:]), ["dsb"], ["dsb"])
                            self.tt("dve", osb[0:64, :], NE[0:64, :], dsb[0:64, :], ALU.mult, [("ps", 0), "dsb"], ["osb"])
                            self.tt("dve", osb[64:128, :], NO[64:128, :], dsb[64:128, :], ALU.mult, [("ps", 1), "dsb", "osb"], ["osb"])
                            self.sigmoid_act(gch[:], "gch")
                            self.tt("pool", yst[:], osb[:], gch[:], ALU.mult, ["osb", "gch"], ["yst"])
                            S.dma("sp", self.ycat[768 + tp * 128:768 + (tp + 1) * 128, cs], yst[:], reads=["yst"],
                                  writes=[("ycat", kind, tp, c)])
                S.barrier()

    def phaseCD(self, l, src, dst):
        nc, S = self.nc, self.S
        N = 512
        NCH = SEQ // N
        dstv = dst.ap().rearrange("(kt p) t -> p kt t", p=128)
        h2v = self.h2d.ap().rearrange("(kt p) t -> p kt t", p=128)
        srcv = src.ap().rearrange("(kt p) t -> p kt t", p=128)
        if self.dbg and "B" not in self.phases:
            ysrc = self.ycat_in.ap().rearrange("(kt p) t -> p kt t", p=128)
        else:
            ysrc = self.ycat.ap().rearrange("(kt p) t -> p kt t", p=128)
        self._psi = 0

        def nextps():
            i = self._psi
            self._psi = (i + 1) % 6
            return self.PS[i], ("ps", i)

        with ExitStack() as es0:
            T0 = lambda nm, sh, dt_: es0.enter_context(nc.sbuf_tensor(self.u(nm), sh, dt_))
            wff1 = T0("wff1", [128, 8, 4 * D], BF16)
            wff2 = T0("wff2", [128, 32, D], BF16)
            with ExitStack() as es:
                T = lambda nm, sh, dt_: es.enter_context(nc.sbuf_tensor(self.u(nm), sh, dt_))
                wout = T("wout", [128, 8, D], BF16)
                xcs = [T("xc%d" % i, [128, 8, N], F32) for i in range(2)]
                ycs = [T("yc0", [128, 8, N], BF16)] * 2
                hcs = [T("hc0", [128, 8, N], BF16)] * 2
                rstdc = T("rstdc", [128, N], F32)
                tmpring = Ring("tmpc", [T("tmpc%d" % i, [128, N], F32) for i in range(2)])
                for kt in range(8):
                    S.dma("pool", wout[:, kt, :], self.w_out[l, kt * 128:(kt + 1) * 128, :], writes=[("wout", kt)])
                woutk = [("wout", kt) for kt in range(8)]

                def loads(c):
                    S.dma("sp", xcs[c % 2][:], srcv[:, :, c * N:(c + 1) * N], writes=[("xc", c % 2)])

                def loady(c):
                    S.dma("pool" if self.dbg and "B" not in self.phases else "sp", ycs[0][:], ysrc[:, :, c * N:(c + 1) * N], writes=[("yc", 0)])

                loads(0)
                loady(0)
                for kt in range(8):
                    S.dma("pool", wff1[:, kt, :], self.w_ff1[l, kt * 128:(kt + 1) * 128, :], writes=[("wff1", kt)])
                for ft in range(32):
                    S.dma("pool", wff2[:, ft, :], self.w_ff2[l, ft * 128:(ft + 1) * 128, :], writes=[("wff2", ft)])
                for c in range(NCH):
                    if c + 1 < NCH:
                        loads(c + 1)
                    xc, xk = xcs[c % 2], ("xc", c % 2)
                    yc, yk = ycs[0], ("yc", 0)
                    hc, hk = hcs[0], ("hc", 0)
                    for ot in range(8):
                        ps, pk = nextps()
                        self.mm([(ps[:, :N], wout[:, kt, ot * 128:(ot + 1) * 128], yc[:, kt, :], kt == 0, kt == 7)
                                 for kt in range(8)], [yk] + woutk, [pk])
                        self.stt("dve", xc[:, ot, :], ps[:, :N], self.modv[:, 16 + ot:17 + ot], xc[:, ot, :],
                                 ALU.mult, ALU.add, [pk, xk, "modv"], [xk])
                    if c + 1 < NCH:
                        loady(c + 1)
                    S.dma("sp", dstv[:, :, c * N:(c + 1) * N], xc[:], reads=[xk], writes=[("dst", c)])
                    for _ in self.modulate_g(xc, xk, hc, hk, hc, hk, rstdc, "rstdc", tmpring, 7, N, 8, 24, on_act=True):
                        pass
                    S.dma("sp", h2v[:, :, c * N:(c + 1) * N], hc[:], reads=[(hk, kt) for kt in range(8)],
                          writes=[("h2d", c)])
                S.barrier()
            with ExitStack() as es:
                T = lambda nm, sh, dt_: es.enter_context(nc.sbuf_tensor(self.u(nm), sh, dt_))
                xds = [T("xd%d" % i, [128, 8, N], F32) for i in range(2)]
                hds = [T("hd%d" % i, [128, 8, N], BF16) for i in range(2)]
                hid = T("hid", [128, 16, N], BF16)
                rlring = Ring("rl", [T("rl%d" % i, [128, N], F32) for i in range(2)])
                wff1k = [("wff1", kt) for kt in range(8)]
                wff2k = [("wff2", ft) for ft in range(32)]

                def loads2(c):
                    S.dma("sp", xds[c % 2][:], dstv[:, :, c * N:(c + 1) * N], writes=[("xd", c % 2)])
                    S.dma("sp", hds[c % 2][:], h2v[:, :, c * N:(c + 1) * N], writes=[("hd", c % 2)])

                loads2(0)
                ev = 0
                for c in range(NCH):
                    if c + 1 < NCH:
                        loads2(c + 1)
                    xd, xk = xds[c % 2], ("xd", c % 2)
                    hd, hk = hds[c % 2], ("hd", c % 2)
                    for half in range(2):
                        for f in range(16):
                            ft = half * 16 + f
                            ps, pk = nextps()
                            self.mm([(ps[:, :N], wff1[:, kt, ft * 128:(ft + 1) * 128], hd[:, kt, :], kt == 0, kt == 7)
                                     for kt in range(8)], [hk] + wff1k, [pk])
                            rl, rk = rlring.next()
                            self.act(rl[:], ps[:, :N], AF.Relu, [pk], [rk])
                            self.tt("pool" if ev % 2 == 0 else "dve", hid[:, f, :], rl[:], rl[:], ALU.mult, [rk], [("hid", f)])
                            ev += 1
                        hidk = [("hid", f) for f in range(16)]
                        for ot in range(8):
                            ps, pk = nextps()
                            self.mm([(ps[:, :N], wff2[:, half * 16 + f, ot * 128:(ot + 1) * 128], hid[:, f, :], f == 0, f == 15)
                                     for f in range(16)], hidk + wff2k, [pk])
                            self.stt("dve", xd[:, ot, :], ps[:, :N], self.modv[:, 40 + ot:41 + ot], xd[:, ot, :],
                                     ALU.mult, ALU.add, [pk, xk, "modv"], [xk])
                    S.dma("sp", dstv[:, :, c * N:(c + 1) * N], xd[:], reads=[xk], writes=[("dst2", c)])
                S.barrier()


def _prep_inputs(inp):
    cols = _col_index()
    w_in = np.asarray(inp["w_in"], np.float32)
    w_in_p = np.zeros((L, D, NCOL), np.float32)
    valid = cols >= 0
    w_in_p[:, :, valid] = w_in[:, :, cols[valid]]
    cmat, alibi, crow = _host_consts()
    pp = np.stack([_pp_layer(inp, l) for l in range(L)], 0)
    lam = np.asarray(inp["diff_lambda"], np.float32).reshape(L, 128)
    shared = dict(
        w_ada=np.ascontiguousarray(inp["w_ada"], np.float32), b_ada=np.ascontiguousarray(inp["b_ada"], np.float32),
        w_in=w_in_p, w_out=np.ascontiguousarray(inp["w_out"], np.float32),
        w_ff1=np.ascontiguousarray(inp["w_ff1"], np.float32), w_ff2=np.ascontiguousarray(inp["w_ff2"], np.float32),
        pp=pp, lam=lam, cmat=cmat, alibi=alibi, crow=crow,
        onesrow=np.ones((1, SEQ), np.float32), mask4=_mask4())
    maps = []
    x = np.asarray(inp["x"], np.float32)
    c = np.asarray(inp["c"], np.float32)
    for b in range(x.shape[0]):
        m = dict(shared)
        m["xT"] = np.ascontiguousarray(x[b].T)
        m["c8"] = np.ascontiguousarray(c[b].reshape(8, 128).T)
        maps.append(m)
    return maps


def kernel(**inputs):
    inp = {k: np.asarray(v) for k, v in inputs.items()}
    maps = _prep_inputs(inp)
    mk = MK()
    res = run_bass_kernel_spmd(mk.nc, maps, core_ids=list(range(len(maps))))
    out = np.stack([np.ascontiguousarray(r["outT"].T) for r in res.results], 0)
    return out.astype(np.float32)
```

```python
import numpy as np
import math
from contextlib import ExitStack
import concourse.bass as bass
import concourse.mybir as mybir
from concourse.bass_utils import run_bass_kernel_spmd

F32 = mybir.dt.float32
BF16 = mybir.dt.bfloat16
AF = mybir.ActivationFunctionType
ALU = mybir.AluOpType
AX = mybir.AxisListType

ENGS = ("pe", "act", "dve", "pool", "sp")

D = 1024
SEQ = 4096
L = 2
NFM = 34
NTM = 1024
NCOL = NFM * 128 + NTM
EPS = 1e-6
NPP = 56
import os
GLA_STOP = int(os.environ.get('GLA_STOP', '0'))
GLA_SKIP = os.environ.get('GLA_SKIP', '')
WARM = int(os.environ.get('WARM', '0'))
FULLK = int(os.environ.get('FULLK', '1'))


class Sched:
    def __init__(self, nc, n_dma_sems=24):
        self.nc = nc
        self.streams = {e: [] for e in ENGS}
        self.esem = {e: nc.alloc_semaphore("s_" + e) for e in ENGS}
        self.cnt = {e: 0 for e in ENGS}
        self.seen = {e: {} for e in ENGS}
        self.lastw = {}
        self.readers = {}
        self.dsems = {}
        self.dcnt = {}
        self.dnext = {}
        for q in ("sp", "act", "pool"):
            self.dsems[q] = [nc.alloc_semaphore("d_%s%d" % (q, i)) for i in range(n_dma_sems)]
            self.dcnt[q] = [0] * n_dma_sems
            self.dnext[q] = 0
        self.semh = {}
        for e in ENGS:
            self.semh[("e", e)] = self.esem[e]
        for q in self.dsems:
            for i, s in enumerate(self.dsems[q]):
                self.semh[("d", q, i)] = s
        self.n_waits = 0
        self.n_ops = 0

    def _deps(self, reads, writes):
        deps = {}

        def add(tok):
            if tok is None:
                return
            k, v = tok
            if deps.get(k, 0) < v:
                deps[k] = v

        for r in reads:
            add(self.lastw.get(r))
        for r in writes:
            add(self.lastw.get(r))
            for t in self.readers.get(r, ()):
                add(t)
        return deps

    def _emit_waits(self, e, deps):
        seen = self.seen[e]
        for k, v in deps.items():
            if seen.get(k, 0) >= v:
                continue
            if e == "pe" and k == ("e", "pe"):
                continue
            seen[k] = v
            h = self.semh[k]
            self.streams[e].append(lambda eng, h=h, v=v: eng.wait_ge(h, v))
            self.n_waits += 1

    def _commit(self, tok, reads, writes):
        for r in reads:
            lst = self.readers.setdefault(r, [])
            lst.append(tok)
            if len(lst) > 64:
                m = {}
                for k, v in lst:
                    if m.get(k, 0) < v:
                        m[k] = v
                self.readers[r] = list(m.items())
        for r in writes:
            self.lastw[r] = tok
            self.readers[r] = []

    def op(self, e, fn, reads=(), writes=()):
        deps = self._deps(reads, writes)
        self._emit_waits(e, deps)
        self.cnt[e] += 1
        sem = self.esem[e]
        self.streams[e].append(lambda eng, fn=fn, sem=sem: fn(eng).then_inc(sem, 1))
        tok = (("e", e), self.cnt[e])
        self._commit(tok, reads, writes)
        self.n_ops += 1
        return tok

    def pe_group(self, fns, reads=(), writes=()):
        deps = self._deps(reads, writes)
        self._emit_waits("pe", deps)
        self.cnt["pe"] += 1
        sem = self.esem["pe"]
        for fn in fns[:-1]:
            self.streams["pe"].append(lambda eng, fn=fn: fn(eng))
        fn = fns[-1]
        self.streams["pe"].append(lambda eng, fn=fn, sem=sem: fn(eng).then_inc(sem, 1))
        tok = (("e", "pe"), self.cnt["pe"])
        self._commit(tok, reads, writes)
        self.n_ops += len(fns)
        return tok

    def dma(self, q, out, in_, reads=(), writes=(), **kw):
        i = self.dnext[q]
        self.dnext[q] = (i + 1) % len(self.dsems[q])
        k = ("d", q, i)
        deps = self._deps(reads, writes)
        if self.dcnt[q][i] > 0:
            deps[k] = max(deps.get(k, 0), self.dcnt[q][i])
        self._emit_waits(q, deps)
        self.dcnt[q][i] += 16
        h = self.semh[k]
        self.streams[q].append(
            lambda eng, out=out, in_=in_, h=h, kw=kw: eng.dma_start(out=out, in_=in_, **kw).then_inc(h, 16))
        tok = (k, self.dcnt[q][i])
        self._commit(tok, reads, writes)
        return tok

    def barrier(self):
        allk = {}
        for e in ENGS:
            if self.cnt[e] > 0:
                allk[("e", e)] = self.cnt[e]
        for q in self.dsems:
            for i, c in enumerate(self.dcnt[q]):
                if c > 0:
                    allk[("d", q, i)] = c
        for e in ENGS:
            self._emit_waits(e, dict(allk))
        self.lastw.clear()
        self.readers.clear()

    def finish(self):
        self.barrier()
        nc = self.nc
        streams = self.streams
        with nc.Block() as block:
            @block.tensor
            def _(eng):
                for f in streams["pe"]:
                    f(eng)

            @block.scalar
            def _(eng):
                for f in streams["act"]:
                    f(eng)

            @block.vector
            def _(eng):
                for f in streams["dve"]:
                    f(eng)

            @block.gpsimd
            def _(eng):
                for f in streams["pool"]:
                    f(eng)

            @block.sync
            def _(eng):
                for f in streams["sp"]:
                    f(eng)


class Ring:
    def __init__(self, name, bufs):
        self.name = name
        self.bufs = bufs
        self.i = 0

    def next(self):
        i = self.i
        self.i = (i + 1) % len(self.bufs)
        return self.bufs[i], (self.name, i)


OFF = dict(hq=0, hf=256, hi=512, hg=768, dq=1024, dk=1280, dv=1536, fq=1792, fk=2048, fv=2304,
           fg=2560, ff=2816, mq=2820, mk=3076, mv=3332, mo=3588, mi=3844, mf=3848)
T_HQ, T_HF, T_HG, T_MQ, T_MK, T_MO, T_MI, T_MF = 0, 2, 4, 6, 8, 10, 12, 14
T_FQ, T_FK, T_FG, T_DQ, T_DK = 16, 20, 24, 26, 30
V_H, V_M, V_F, V_D = 0, 256, 512, 768


def _col_index():
    cols = []

    def rng(base, n):
        return list(range(base, base + n))

    for nm in ("hq", "hf", "hg", "mq", "mk", "mo"):
        cols += rng(OFF[nm], 256)
    for nm in ("mi", "mf"):
        for h in range(4):
            cols += [OFF[nm] + h] * 64
    for h in range(4):
        cols += rng(OFF["fq"] + 64 * h, 64) + [OFF["ff"] + h] + [-1] * 63
    for h in range(4):
        cols += rng(OFF["fk"] + 64 * h, 64) + [-1] * 64
    cols += rng(OFF["fg"], 256)
    for nm in ("dq", "dk"):
        for h in range(4):
            b = OFF[nm] + 64 * h
            cols += rng(b, 32) + [-1] * 32 + rng(b + 32, 32) + [-1] * 32
    assert len(cols) == NFM * 128
    for nm in ("hi", "mv", "fv", "dv"):
        cols += rng(OFF[nm], 256)
    assert len(cols) == NCOL
    return np.array(cols)


def _host_consts():
    p = np.arange(128)
    c = {}
    c["ident"] = np.eye(128, dtype=np.float32)
    c["blk64"] = (p[:, None] // 64 == p[None, :] // 64).astype(np.float32)
    in1 = (p < 32)
    in2 = (p >= 64) & (p < 96)
    c["blk32d"] = ((in1[:, None] & in1[None, :]) | (in2[:, None] & in2[None, :])).astype(np.float32)
    c["trimask"] = np.where(p[:, None] <= p[None, :], 0.0, -30000.0).astype(np.float32)
    c["glamask"] = ((p[:, None] // 64 == p[None, :] // 64) & (p[:, None] <= p[None, :])).astype(np.float32)
    slopes = 2.0 ** (-8.0 * np.arange(1, 5, dtype=np.float64) / 4)
    pos = (np.arange(32)[None, :] * 128 + p[:, None]).astype(np.float64)
    c["alibi"] = np.stack([slopes[h] * pos for h in range(4)], axis=1).astype(np.float32)
    c["crow"] = np.stack([-slopes[h] * np.arange(SEQ, dtype=np.float64) for h in range(4)], 0).astype(np.float32)
    cmat = np.concatenate([c["ident"], c["blk64"], c["blk32d"], c["trimask"], c["glamask"]], axis=1)
    return cmat.astype(np.float32), c["alibi"].reshape(128, 128).copy(), c["crow"]


def _mask4():
    p = np.arange(128)
    s_, t_ = p[:, None], p[None, :]
    d32 = ((s_ // 32 == t_ // 32) & (s_ <= t_)).astype(np.float32)
    ba = ((s_ // 64 == t_ // 64) & (s_ % 64 < 32) & (t_ % 64 >= 32)).astype(np.float32)
    return np.ascontiguousarray(np.concatenate([d32, ba], axis=1))


def _pp_layer(inp, l):
    pp = np.zeros((128, NPP), np.float32)
    p = np.arange(128)
    pp[:, 0:8] = inp["norm_mix_gain"][l].reshape(8, 128).T
    pp[:, 8:16] = inp["norm_ff_gain"][l].reshape(8, 128).T
    pp[:, 16:18] = inp["hg_lb_logits"][0].reshape(2, 128).T
    pp[:, 18:20] = inp["hg_lb_logits"][1].reshape(2, 128).T
    pp[:, 20] = inp["hg_norm_gain"][l][p % 64]
    for col, nm in ((21, "diff_qn_gain"), (22, "diff_kn_gain")):
        g = inp[nm][l]
        pp[0:32, col] = g
        pp[64:96, col] = g
    pp[:, 23] = inp["diff_sub_gain"][l][p % 64]
    pp[:, 24] = inp["fox_qn_gain"][l][p % 64]
    pp[:, 25] = inp["fox_kn_gain"][l][p % 64]
    for h in range(4):
        pp[:, 26 + h] = inp["fox_f_bias"][l][h]
    for tp in range(2):
        pp[:, 30 + tp] = inp["mlstm_i_bias"][l][2 * tp + p // 64]
        pp[:, 32 + tp] = inp["mlstm_f_bias"][l][2 * tp + p // 64]
        for j in range(4):
            pp[:, 34 + tp * 4 + j] = inp["mlstm_conv"][l][j, tp * 128 + p]
            pp[:, 42 + tp * 4 + j] = inp["mlstm_conv"][l][j, 256 + tp * 128 + p]
    return pp


class MK:
    def __init__(self, dbg=False, phases="0ABCD", nlayers=L, mixers="hmfd"):
        self.mixers = mixers
        self.dbg = dbg
        self.phases = phases
        self.nlayers = nlayers
        nc = bass.Bass("TRN2", target_bir_lowering=False)
        self.nc = nc
        self.S = Sched(nc)
        ik = "ExternalInput"
        sk = "ExternalOutput" if dbg else "Internal"
        dt = nc.dram_tensor
        self.xT = dt("xT", [D, SEQ], F32, kind=ik)
        self.c8 = dt("c8", [128, 8], F32, kind=ik)
        self.w_ada = dt("w_ada", [L, D, 6 * D], F32, kind=ik)
        self.b_ada = dt("b_ada", [L, 6 * D], F32, kind=ik)
        self.w_in = dt("w_in", [L, D, NCOL], F32, kind=ik)
        self.w_out = dt("w_out", [L, D, D], F32, kind=ik)
        self.w_ff1 = dt("w_ff1", [L, D, 4 * D], F32, kind=ik)
        self.w_ff2 = dt("w_ff2", [L, 4 * D, D], F32, kind=ik)
        self.pp = dt("pp", [L, 128, NPP], F32, kind=ik)
        self.lam = dt("lam", [L, 128], F32, kind=ik)
        self.cmat = dt("cmat", [128, 5 * 128], F32, kind=ik)
        self.alibi = dt("alibi", [128, 128], F32, kind=ik)
        self.crow = dt("crow", [4, SEQ], F32, kind=ik)
        self.onesrow = dt("onesrow", [1, SEQ], F32, kind=ik)
        self.mask4 = dt("mask4", [128, 256], F32, kind=ik)
        self.ycat_in = dt("ycat_in", [D, SEQ], F32, kind=ik) if dbg else None
        self.outT = dt("outT", [D, SEQ], F32, kind="ExternalOutput")
        self.xres = dt("xres", [D, SEQ], F32, kind=sk)
        self.zfm = dt("zfm", [NFM * 128, SEQ], F32, kind=sk)
        self.ztm = dt("ztm", [SEQ, NTM], BF16, kind=sk)
        self.ycat = dt("ycat", [D, SEQ], BF16, kind=sk)
        self.modd = dt("modd", [128, 48], F32, kind=sk)
        self.h2d = dt("h2d", [D, SEQ], BF16, kind="Internal")
        self.PS = [nc.alloc_psum_tensor("ps%d" % i, [128, 512], F32) for i in range(8)]
        self.build()

    def act(self, out, in_, func, R, W, bias=0.0, scale=1.0):
        return self.S.op("act", lambda e: e.activation(out=out, in_=in_, func=func, bias=bias, scale=scale), R, W)

    def ts(self, eng, out, in0, s1, s2, op0, op1, R, W):
        if s2 is None:
            return self.S.op(eng, lambda e: e.tensor_scalar(out, in0, s1, None, op0), R, W)
        return self.S.op(eng, lambda e: e.tensor_scalar(out, in0, s1, s2, op0, op1), R, W)

    def tt(self, eng, out, in0, in1, op, R, W):
        return self.S.op(eng, lambda e: e.tensor_tensor(out, in0, in1, op), R, W)

    def stt(self, eng, out, in0, sc, in1, op0, op1, R, W):
        return self.S.op(eng, lambda e: e.scalar_tensor_tensor(out, in0, sc, in1, op0, op1), R, W)

    def cp(self, eng, out, in_, R, W):
        if eng == "act":
            return self.act(out, in_, AF.Copy, R, W)
        return self.S.op(eng, lambda e: e.tensor_copy(out, in_), R, W)

    def mm(self, items, R, W):
        fns = []
        for (out, lhsT, rhs, st, sp) in items:
            fns.append(lambda e, out=out, lhsT=lhsT, rhs=rhs, st=st, sp=sp:
                       e.matmul(out, lhsT=lhsT, rhs=rhs, start=st, stop=sp))
        return self.S.pe_group(fns, R, W)

    def dump(self, name, ap, key, shape, dtype=F32):
        if not self.dbg:
            return
        t = self.nc.dram_tensor("dbg_" + name, list(shape), dtype, kind="ExternalOutput")
        self.S.dma("sp", t.ap(), ap, reads=[key], writes=[("dbg", name)])

    def u(self, name):
        self._uid = getattr(self, '_uid', 0) + 1
        return '%s_%d' % (name, self._uid)

    def sb(self, name, shape, dtype):
        return self.nc.alloc_sbuf_tensor(name, shape, dtype)

    def build(self):
        nc, S = self.nc, self.S
        self.cbf = self.sb("cbf", [128, 5 * 128], BF16)
        self.cf = self.sb("cf", [128, 128], F32)
        S.dma("pool", self.cbf[:], self.cmat.ap(), writes=["cbf"])
        S.dma("sp", self.cf[:], self.cmat[:, 0:128], writes=["cf"])
        self.cf2 = self.sb("cf2", [128, 256], F32)
        S.dma("sp", self.cf2[:], self.mask4.ap(), writes=["cf2"])
        self.mask4_f = self.cf2
        self.ident_bf = self.cbf[:, 0:128]
        self.blk64_bf = self.cbf[:, 128:256]
        self.blk32d_bf = self.cbf[:, 256:384]
        self.trimask_bf = self.cbf[:, 384:512]
        self.ident_f = self.cf[:, 0:128]
        self.ones_bf = self.sb("ones_bf", [128, 128], BF16)
        self.ones_f = self.sb("ones_f", [128, 128], F32)
        S.op("dve", lambda e: e.memset(self.ones_bf[:], 1.0), writes=["ones_bf"])
        S.op("dve", lambda e: e.memset(self.ones_f[:], 1.0), writes=["ones_f"])
        self.ppt = self.sb("ppt", [128, L, NPP], F32)
        for l in range(L):
            S.dma("sp", self.ppt[:, l, :], self.pp[l], writes=["ppt"])
        self.modv = self.sb("modv", [128, 48], F32)
        self.AB = self.sb("AB", [128, 16], F32)
        S.barrier()
        base = (nc.sbuf_base, nc.sbuf_top) if hasattr(nc, "sbuf_base") else None
        for l in range(self.nlayers):
            src = self.xT if l == 0 else self.xres
            dst = self.xres if l == 0 and self.nlayers > 1 else self.outT
            if "0" in self.phases:
                self.phase0(l)
            if "A" in self.phases:
                self.phaseA(l, src)
            if "B" in self.phases:
                self.phaseB(l)
            if "C" in self.phases:
                self.phaseCD(l, src, dst)
        S.finish()

    def phase0(self, l):
        nc, S = self.nc, self.S
        with nc.sbuf_tensor(self.u("cin"), [128, 8], F32) as cin, \
                nc.sbuf_tensor(self.u("ctmp"), [128, 8], F32) as ctmp, \
                nc.sbuf_tensor(self.u("cact"), [128, 8], F32) as cact, \
                nc.sbuf_tensor(self.u("wa0"), [128, 8, 512], F32) as wa0, \
                nc.sbuf_tensor(self.u("wa1"), [128, 8, 512], F32) as wa1, \
                nc.sbuf_tensor(self.u("modrow"), [1, 6 * D], F32) as modrow, \
                nc.sbuf_tensor(self.u("badar"), [1, 6 * D], F32) as badar:
            S.dma("sp", cin[:], self.c8.ap(), writes=["cin"])
            S.dma("sp", badar[:], self.b_ada[l:l + 1, :], writes=["badar"])
            self.act(ctmp[:], cin[:], AF.Exp, ["cin"], ["ctmp"], scale=-1.0)
            self.ts("dve", ctmp[:], ctmp[:], 1.0, None, ALU.add, None, ["ctmp"], ["ctmp"])
            S.op("dve", lambda e: e.reciprocal(ctmp[:], ctmp[:]), ["ctmp"], ["ctmp"])
            self.tt("dve", cact[:], cin[:], ctmp[:], ALU.mult, ["cin", "ctmp"], ["cact"])
            wring = Ring("wa", [wa0, wa1])
            for j in range(12):
                buf, bk = wring.next()
                S.dma("sp", buf[:], self.w_ada[l, :, j * 512:(j + 1) * 512].rearrange("(kt p) n -> p kt n", p=128),
                      writes=[bk])
                ps = self.PS[j % 2]
                pk = ("ps", j % 2)
                self.mm([(ps[0:1, :], cact[:, kt:kt + 1], buf[:, kt, :], kt == 0, kt == 7) for kt in range(8)],
                        ["cact", bk], [pk])
                self.tt("dve", modrow[0:1, j * 512:(j + 1) * 512], ps[0:1, :], badar[0:1, j * 512:(j + 1) * 512],
                        ALU.add, [pk, "badar"], ["modrow"])
            ps = self.PS[2]
            self.mm([(ps[:, j:j + 1], modrow[0:1, j * 128:(j + 1) * 128], self.ones_f[0:1, 0:1], True, True)
                     for j in range(48)], ["modrow", "ones_f"], [("ps", 2)])
            self.cp("dve", self.modv[:], ps[:, 0:48], [("ps", 2)], ["modv"])
            pp = self.ppt
            self.stt("dve", self.AB[:, 0:8], self.modv[:, 8:16], 1.0, pp[:, l, 0:8], ALU.add, ALU.mult,
                     ["modv", "ppt"], ["AB"])
            self.stt("dve", self.AB[:, 8:16], self.modv[:, 32:40], 1.0, pp[:, l, 8:16], ALU.add, ALU.mult,
                     ["modv", "ppt"], ["AB"])
            if self.dbg:
                S.dma("sp", self.modd.ap(), self.modv[:], reads=["modv"], writes=["modd"])
            S.barrier()

    def bg_run(self, n):
        for _ in range(n):
            if getattr(self, "bg", None) is None:
                return
            try:
                next(self.bg)
            except StopIteration:
                self.bg = None

    def bg2_run(self, n):
        q = getattr(self, "bg2", None)
        for _ in range(n):
            while q:
                try:
                    next(q[0])
                    break
                except StopIteration:
                    q.pop(0)
            if not q:
                return

    def bg2_drain(self):
        while getattr(self, "bg2", None):
            self.bg2_run(64)

    def bg_drain(self):
        while getattr(self, "bg", None) is not None:
            self.bg_run(64)

    def modulate_g(self, xc, xk, h, hk, sq, sqk, rstd, rk, tmpring, psidx, N, Acol, Bcol, on_act=False):
        ps = self.PS[psidx]
        pk = ("ps", psidx)
        for kt in range(8):
            if on_act:
                self.act(sq[:, kt, :N], xc[:, kt, :N], AF.Square, [xk], [(sqk, kt)])
            else:
                self.tt("pool", sq[:, kt, :N], xc[:, kt, :N], xc[:, kt, :N], ALU.mult, [xk], [(sqk, kt)])
            yield
        self.mm([(ps[:, :N], self.ones_bf[:], sq[:, kt, :N], kt == 0, kt == 7) for kt in range(8)],
                ["ones_bf"] + [(sqk, kt) for kt in range(8)], [pk])
        yield
        self.ts("dve", rstd[:, :N], ps[:, :N], 1.0 / D, EPS, ALU.mult, ALU.add, [pk], [rk])
        yield
        self.act(rstd[:, :N], rstd[:, :N], AF.Sqrt, [rk], [rk])
        yield
        self.S.op("dve", lambda e: e.reciprocal(rstd[:, :N], rstd[:, :N]), [rk], [rk])
        yield
        for kt in range(8):
            tmp, tk = tmpring.next()
            self.stt("dve", tmp[:, :N], xc[:, kt, :N], self.AB[:, Acol + kt:Acol + kt + 1], rstd[:, :N],
                     ALU.mult, ALU.mult, [xk, "AB", rk], [tk])
            yield
            if on_act:
                self.act(h[:, kt, :N], tmp[:, :N], AF.Identity, [tk, "modv"], [(hk, kt)],
                         bias=self.modv[:, Bcol + kt:Bcol + kt + 1])
            else:
                self.ts("pool", h[:, kt, :N], tmp[:, :N], self.modv[:, Bcol + kt:Bcol + kt + 1], None, ALU.add, None,
                        [tk, "modv"], [(hk, kt)])
            yield

    def phaseA(self, l, src):
        nc, S = self.nc, self.S
        with ExitStack() as es:
            T = lambda nm, sh, dt_: es.enter_context(nc.sbuf_tensor(self.u(nm), sh, dt_))
            win = T("win", [128, 8, NCOL], BF16)
            xa = [T("xa%d" % i, [128, 8, 512], F32) for i in range(2)]
            ha = [T("ha%d" % i, [128, 8, 512], BF16) for i in range(2)]
            sqa = T("sqa", [128, 8, 512], BF16)
            rstda = T("rstda", [128, 512], F32)
            tmpring = Ring("tmpa", [T("tmpa%d" % i, [128, 512], F32) for i in range(2)])
            string = Ring("sta", [T("sta%d" % i, [128, 512], F32) for i in range(4)])
            stbring = Ring("stb", [T("stb%d" % i, [128, 512], BF16) for i in range(2)])
            for kt in range(8):
                S.dma("pool", win[:, kt, :], self.w_in[l, kt * 128:(kt + 1) * 128, :], writes=[("win", kt)])
            wink = [("win", kt) for kt in range(8)]
            srcv = src.ap().rearrange("(kt p) t -> p kt t", p=128)

            def stage1(c):
                xc, xk = xa[c % 2], ("xa", c % 2)
                S.dma("sp", xc[:], srcv[:, :, c * 512:(c + 1) * 512], writes=[xk])
                yield
                yield from self.modulate_g(xc, xk, ha[c % 2], ("ha", c % 2), sqa, "sqa", rstda, "rstda",
                                           tmpring, 7, 512, 0, 0)

            for _ in stage1(0):
                pass
            psi = 0
            ev = 0
            for c in range(8):
                self.bg = stage1(c + 1) if c + 1 < 8 else None
                h = ha[c % 2]
                hks = [(("ha", c % 2), kt) for kt in range(8)]
                for ft in range(NFM):
                    ps = self.PS[psi]
                    pk = ("ps", psi)
                    psi = (psi + 1) % 6
                    self.mm([(ps[:], win[:, kt, ft * 128:(ft + 1) * 128], h[:, kt, :], kt == 0, kt == 7)
                             for kt in range(8)], hks + wink, [pk])
                    st, sk = string.next()
                    self.cp("act" if ev % 2 == 0 else "dve", st[:], ps[:], [pk], [sk])
                    ev += 1
                    S.dma("sp", self.zfm[ft * 128:(ft + 1) * 128, c * 512:(c + 1) * 512], st[:],
                          reads=[sk], writes=[("zfm", ft, c)])
                    self.bg_run(1)
                for tt in range(4):
                    for half in range(2):
                        ps = self.PS[psi]
                        pk = ("ps", psi)
                        psi = (psi + 1) % 6
                        c0 = NFM * 128 + half * 512
                        self.mm([(ps[:], h[:, kt, tt * 128:(tt + 1) * 128], win[:, kt, c0:c0 + 512], kt == 0, kt == 7)
                                 for kt in range(8)], hks + wink, [pk])
                        st, sk = stbring.next()
                        self.cp("act" if ev % 2 == 0 else "dve", st[:], ps[:], [pk], [sk])
                        ev += 1
                        r0 = c * 512 + tt * 128
                        S.dma("sp", self.ztm[r0:r0 + 128, half * 512:(half + 1) * 512], st[:],
                              reads=[sk], writes=[("ztm", c, tt, half)])
                        self.bg_run(1)
                self.bg_drain()
            S.barrier()

    def phaseB(self, l):
        S = self.S
        self.setupB(l)
        S.barrier()
        if "h" in self.mixers:
            self.gla(l, "hgrn")
            S.barrier()
        if "m" in self.mixers:
            self.gla(l, "mlstm")
            S.barrier()
        if "f" in self.mixers:
            self.attn(l, "fox")
            S.barrier()
        if "d" in self.mixers:
            self.attn(l, "diff")
            S.barrier()

    def setupB(self, l):
        nc, S = self.nc, self.S
        if not hasattr(self, "sv"):
            self.sv = self.sb("sv", [128, 16], F32)
            self.lamrow = self.sb("lamrow", [1, 256], F32)
            self.negpp = self.sb("negpp", [128, NPP], F32)
        sv, pp = self.sv, self.ppt
        self.ts("dve", self.negpp[:], pp[:, l, :], -1.0, None, ALU.mult, None, ["ppt"], ["negpp"])
        self.tt("dve", sv[:, 0:2], pp[:, l, 18:20], pp[:, l, 16:18], ALU.subtract, ["ppt"], ["sv"])
        self.act(sv[:, 0:2], sv[:, 0:2], AF.Exp, ["sv"], ["sv"], scale=-1.0)
        self.ts("dve", sv[:, 0:2], sv[:, 0:2], 1.0, None, ALU.add, None, ["sv"], ["sv"])
        S.op("dve", lambda e: e.reciprocal(sv[:, 0:2], sv[:, 0:2]), ["sv"], ["sv"])
        self.ts("dve", sv[:, 0:2], sv[:, 0:2], float(l), None, ALU.mult, None, ["sv"], ["sv"])
        self.ts("dve", sv[:, 2:4], sv[:, 0:2], -1.0, 1.0, ALU.mult, ALU.add, ["sv"], ["sv"])
        lr = self.lamrow
        S.dma("sp", lr[0:1, 0:128], self.lam[l:l + 1, :], writes=["lamrow"])
        self.tt("dve", lr[0:1, 128:160], lr[0:1, 0:32], lr[0:1, 32:64], ALU.mult, ["lamrow"], ["lamrow"])
        self.tt("dve", lr[0:1, 160:192], lr[0:1, 64:96], lr[0:1, 96:128], ALU.mult, ["lamrow"], ["lamrow"])
        S.op("dve", lambda e: e.tensor_reduce(lr[0:1, 192:193], lr[0:1, 128:160], AX.X, ALU.add), ["lamrow"], ["lamrow"])
        S.op("dve", lambda e: e.tensor_reduce(lr[0:1, 193:194], lr[0:1, 160:192], AX.X, ALU.add), ["lamrow"], ["lamrow"])
        self.act(lr[0:1, 192:194], lr[0:1, 192:194], AF.Exp, ["lamrow"], ["lamrow"])
        import math
        lam_init = 0.8 - 0.6 * math.exp(-0.3 * l)
        self.lam_init = lam_init
        self.tt("dve", lr[0:1, 194:195], lr[0:1, 193:194], lr[0:1, 192:193], ALU.subtract, ["lamrow"], ["lamrow"])
        self.ts("dve", lr[0:1, 194:195], lr[0:1, 194:195], -lam_init, None, ALU.add, None, ["lamrow"], ["lamrow"])
        ps = self.PS[0]
        self.mm([(ps[:, 0:1], self.ones_f[0:1, 0:128], lr[0:1, 194:195], True, True)], ["lamrow", "ones_f"], [("ps", 0)])
        self.cp("dve", sv[:, 4:5], ps[:, 0:1], [("ps", 0)], ["sv"])

    def sigmoid_inplace(self, t, key):
        self.act(t, t, AF.Exp, [key], [key], scale=-1.0)
        self.ts("dve", t, t, 1.0, None, ALU.add, None, [key], [key])
        self.S.op("dve", lambda e: e.reciprocal(t, t), [key], [key])

    def rstd_from_ps(self, out, ps, pk, key, mult, add):
        self.act(out, ps, AF.Ln, [pk], [key], scale=mult, bias=add)
        self.act(out, out, AF.Exp, [key], [key], scale=-0.5)

    def sigmoid_act(self, t, key):
        self.act(t, t, AF.Sigmoid, [key], [key])

    def attn(self, l, kind):
        nc, S = self.nc, self.S
        fox = kind == "fox"
        pp, sv = self.ppt, self.sv
        with ExitStack() as es:
            T = lambda nm, sh, dt_: es.enter_context(nc.sbuf_tensor(self.u(nm), sh, dt_))
            qraws = [T("qraw%d" % i, [128, SEQ], F32) for i in range(2)]
            kraws = [T("kraw%d" % i, [128, SEQ], F32) for i in range(2)]
            qps = [T("qp%d" % i, [128, SEQ], BF16) for i in range(2)]
            kps = [T("kp%d" % i, [128, SEQ], BF16) for i in range(2)]
            Vps = [T("Vp%d" % i, [128, 32, 128], BF16) for i in range(2)]
            negbs = [T("negb%d" % i, [128, 32], F32) for i in range(2)]
            ones4k = T("ones4k", [128, SEQ], F32) if fox else None
            sqring = Ring("sqt", [T("sqt%d" % i, [128, 512], BF16) for i in range(2)])
            rsring = Ring("rst", [T("rst%d" % i, [128, 512], F32) for i in range(2)])
            pring = Ring("pT", [T("pT%d" % i, [128, 512], BF16) for i in range(4)])
            rrow = T("rrow", [128, 2, 512], F32)
            ocps = [T("ocp%d" % i, [128, 2, 512], F32) for i in range(2)]
            bcs = T("bcs", [128, 2, 512], F32)
            osb = T("osb", [128, 512], F32)
            osb2 = T("osb2", [128, 512], F32)
            esq = T("esq", [128, 512], BF16)
            erst = T("erst", [128, 512], F32)
            gch = T("gch", [128, 512], F32)
            yst = T("yst", [128, 512], BF16)
            if fox:
                S.op("pool", lambda e: e.memset(ones4k[:], 1.0), writes=["ones4k"])
                if FULLK:
                    for b_ in range(2):
                        S.op("pool", lambda e, b_=b_: e.memset(qps[b_][64:128, :], 0.0), writes=[("qp", b_)])
                        S.op("pool", lambda e, b_=b_: e.memset(kps[b_][64:128, :], 0.0), writes=[("kp", b_)])
            sring = Ring("pss", [self.PS[0], self.PS[1], self.PS[2], self.PS[3]])
            if fox:
                ranges = [(0, 64)]
                nd = 64.0
                gq, gk = pp[:, l, 24:25], pp[:, l, 25:26]
                lhs_n = self.ones_bf[0:64, 0:64]
                nrows = 64
                KR = [(0, 128)] if FULLK else [(0, 65)]
            else:
                ranges = [(0, 32), (64, 96)]
                nd = 32.0
                gq, gk = pp[:, l, 21:22], pp[:, l, 22:23]
                lhs_n = self.blk32d_bf
                nrows = 128
                KR = [(0, 33), (64, 97)]
            nh = len(KR)
            M = 128
            ops_ = [self.PS[4], self.PS[5]]
            opk = [("ps", 4), ("ps", 5)]

            def prep(h):
                b = h % 2
                qraw, kraw, qp, kp, Vp, negb = qraws[b], kraws[b], qps[b], kps[b], Vps[b], negbs[b]
                kq, kk, kqp, kkp, kv, kn = ("qraw", b), ("kraw", b), ("qp", b), ("kp", b), ("Vp", b), ("negb", b)
                par = h % 2
                srow = 64 if par == 0 else 0
                voff = 0 if par == 0 else 64
                tq = (T_FQ if fox else T_DQ) + h
                tk = (T_FK if fox else T_DK) + h
                S.dma("sp", qraw[:], self.zfm[tq * 128:(tq + 1) * 128, :], writes=[kq])
                S.dma("sp", kraw[:], self.zfm[tk * 128:(tk + 1) * 128, :], writes=[kk])
                yield
                S.op("pool", lambda e: e.memset(Vp[:], 0.0), writes=[kv])
                vcol = (V_F if fox else V_D) + 64 * h
                S.dma("sp", Vp[:, :, voff:voff + 64],
                      self.ztm[:, vcol:vcol + 64].rearrange("(j p) c -> p j c", p=128), reads=[kv], writes=[kv])
                S.op("pool", lambda e, c1=srow: e.memset(Vp[:, :, c1:c1 + 64], 1.0), reads=[kv], writes=[kv])
                yield
                for c in range(8):
                    cs = slice(c * 512, (c + 1) * 512)
                    for (raw, rk_, dstt, dk_, g, isq) in ((qraw, kq, qp, kqp, gq, True), (kraw, kk, kp, kkp, gk, False)):
                        sqt, sqk = sqring.next()
                        rst, rsk = rsring.next()
                        self.tt("pool", sqt[0:nrows, :], raw[0:nrows, cs], raw[0:nrows, cs], ALU.mult, [rk_], [sqk])
                        yield
                        ps = self.PS[7]
                        self.mm([(ps[0:nrows, :], lhs_n, sqt[0:nrows, :], True, True)], [sqk, "cbf", "ones_bf"], [("ps", 7)])
                        yield
                        if isq:
                            self.act(rst[0:nrows, :], ps[0:nrows, :], AF.Ln, [("ps", 7)], [rsk], scale=1.0, bias=nd * EPS)
                        else:
                            self.act(rst[0:nrows, :], ps[0:nrows, :], AF.Ln, [("ps", 7)], [rsk], scale=1.0 / nd, bias=EPS)
                        yield
                        self.act(rst[0:nrows, :], rst[0:nrows, :], AF.Exp, [rsk], [rsk], scale=-0.5)
                        yield
                        for (a, b_) in ranges:
                            self.stt("dve", dstt[a:b_, cs], raw[a:b_, cs], g[a:b_, :], rst[a:b_, :], ALU.mult, ALU.mult,
                                     [rk_, rsk, "ppt"], [dk_])
                        yield
                if fox:
                    fr = qraw[64:65, :]
                    self.act(fr, fr, AF.Exp, [kq], [kq], scale=-1.0, bias=self.negpp[64:65, 26 + h:27 + h])
                    yield
                    self.act(fr, fr, AF.Ln, [kq], [kq], bias=1.0)
                    yield
                    S.op("dve", lambda e: e.tensor_tensor_scan(fr, ones4k[64:65, :], fr, 0.0, ALU.mult, ALU.subtract),
                         [kq, "ones4k"], [kq])
                    yield
                    self.cp("dve", qp[64:65, :], fr, [kq], [kqp])
                    S.op("pool", lambda e: e.memset(kp[64:65, :], 1.0), reads=[kkp], writes=[kkp])
                    yield
                    ps = self.PS[7]
                    S.pe_group([lambda e, j=j, ps=ps: e.transpose(ps[:, j:j + 1], qraw[64:65, j * 128:(j + 1) * 128],
                                                                  self.ident_f[64:65, 64:65]) for j in range(32)],
                               [kq, "cf"], [("ps", 7)])
                    yield
                    self.ts("dve", negb[:], ps[:, 0:32], -1.0, None, ALU.mult, None, [("ps", 7)], [kn])
                    yield
                else:
                    for r0 in (32, 96):
                        S.dma("pool", qp[r0:r0 + 1, :], self.crow[h:h + 1, :], reads=[kqp], writes=[kqp])
                        S.dma("pool", kp[r0:r0 + 1, :], self.onesrow[0:1, :], reads=[kkp], writes=[kkp])
                    S.dma("sp", negb[:], self.alibi[:, h * 32:(h + 1) * 32], writes=[kn])
                    yield

            def main(h):
                b = h % 2
                qp, kp, Vp, negb = qps[b], kps[b], Vps[b], negbs[b]
                kqp, kkp, kv, kn = ("qp", b), ("kp", b), ("Vp", b), ("negb", b)
                par = h % 2
                pb = 64 * par
                srow = 64 if par == 0 else 0

                def emit_qk(c, j, a):
                    dj = j - 4 * c
                    n0 = 128 * dj if dj > 0 else 0
                    k0, k1 = KR[a]
                    ps_s, sk_ = sring.next()
                    items = [(ps_s[:, n0:512], kp[k0:k1, j * 128:(j + 1) * 128],
                              qp[k0:k1, c * 512 + n0:(c + 1) * 512], True, dj < 0)]
                    if nh == 1 and WARM:
                        items = [items[0], items[0]]
                    if dj >= 0:
                        items.append((ps_s[:, n0:n0 + 128], self.ident_bf, self.trimask_bf, False, True))
                    self.mm(items, [kkp, kqp, "cbf"], [sk_])
                    pT, pk_ = pring.next()
                    self.act(pT[:, n0:512], ps_s[:, n0:512], AF.Exp, [sk_, kn], [pk_], bias=negb[:, j:j + 1])
                    return pT, pk_, n0

                def emit_pv(c, j, a, pT, pk_, n0):
                    nj = 4 * c + 4
                    self.mm([(ops_[a][0:M, n0:512], Vp[:, j, 0:M], pT[:, n0:512], j == 0, j == nj - 1)],
                            [kv, pk_], [opk[a]])
                    if j == nj - 1 and a == nh - 1:
                        if len(self.bg2) >= 2:
                            self.bg2_drain()
                        oi = c % 2
                        for a2 in range(nh):
                            self.cp("dve", ocps[oi][:, a2, :], ops_[a2][:, :], [opk[a2]], [("ocp", oi, a2)])
                        self.bg2.append(epilogue(c, oi))

                def epilogue(c, oi):
                    ocp = ocps[oi]
                    cs = slice(c * 512, (c + 1) * 512)
                    rows = slice(pb, pb + 64)
                    if fox:
                        tg = T_FG + h // 2
                        S.dma("sp", gch[rows, :], self.zfm[tg * 128 + pb:tg * 128 + pb + 64, cs], writes=["gch"])
                        yield
                    for a in range(nh):
                        S.op("dve", lambda e, a=a, srow=srow: e.reciprocal(rrow[srow:srow + 64, a, :], ocp[srow:srow + 64, a, :]),
                             [("ocp", oi, a)], [("rrow", a)])
                        yield
                    for _ in range(10):
                        yield
                    for a in range(nh):
                        psb = self.PS[6]
                        self.mm([(psb[:, :], self.ones_f[srow:srow + 1, 0:128], rrow[srow:srow + 1, a, :], True, True)],
                                [("rrow", a), "ones_f"], [("ps", 6)])
                        self.cp("dve", bcs[pb:pb + 64, a, :], psb[pb:pb + 64, :], [("ps", 6)], [("bcs", a)])
                        yield
                    if fox:
                        self.act(gch[rows, :], gch[rows, :], AF.Exp, ["gch"], ["gch"], scale=-1.0)
                        yield
                        self.ts("dve", gch[rows, :], gch[rows, :], 1.0, None, ALU.add, None, ["gch"], ["gch"])
                        S.op("dve", lambda e: e.reciprocal(gch[rows, :], gch[rows, :]), ["gch"], ["gch"])
                        for _ in range(4):
                            yield
                        self.tt("dve", osb[rows, :], ocp[rows, 0, :], bcs[rows, 0, :], ALU.mult, [("ocp", oi, 0), ("bcs", 0)], ["osb"])
                        yield
                        self.tt("pool", yst[rows, :], osb[rows, :], gch[rows, :], ALU.mult, ["osb", "gch"], ["yst"])
                        yield
                        S.dma("sp", self.ycat[512 + 64 * h:512 + 64 * h + 64, cs], yst[rows, :], reads=["yst"],
                              writes=[("ycat", kind, h, c)])
                        yield
                    else:
                        self.tt("dve", osb[rows, :], ocp[rows, 0, :], bcs[rows, 0, :], ALU.mult, [("ocp", oi, 0), ("bcs", 0)], ["osb"])
                        self.tt("dve", osb2[rows, :], ocp[rows, 1, :], bcs[rows, 1, :], ALU.mult, [("ocp", oi, 1), ("bcs", 1)], ["osb2"])
                        yield
                        self.stt("dve", osb[rows, :], osb2[rows, :], sv[rows, 4:5], osb[rows, :], ALU.mult, ALU.add,
                                 ["osb", "osb2", "sv"], ["osb"])
                        yield
                        self.tt("pool", esq[rows, :], osb[rows, :], osb[rows, :], ALU.mult, ["osb"], ["esq"])
                        for _ in range(6):
                            yield
                        ps = self.PS[6]
                        self.mm([(ps[:, :], self.blk64_bf[rows, :], esq[rows, :], True, True)], ["esq", "cbf"], [("ps", 6)])
                        for _ in range(3):
                            yield
                        self.act(erst[rows, :], ps[rows, :], AF.Ln, [("ps", 6)], ["erst"], scale=1.0 / 64.0, bias=EPS)
                        yield
                        self.act(erst[rows, :], erst[rows, :], AF.Exp, ["erst"], ["erst"], scale=-0.5)
                        yield
                        self.stt("dve", osb[rows, :], osb[rows, :], pp[rows, l, 23:24], erst[rows, :], ALU.mult, ALU.mult,
                                 ["osb", "erst", "ppt"], ["osb"])
                        yield
                        self.ts("pool", yst[rows, :], osb[rows, :], float(1.0 - self.lam_init), None, ALU.mult, None, ["osb"], ["yst"])
                        yield
                        S.dma("sp", self.ycat[256 + 64 * h:256 + 64 * h + 64, cs], yst[rows, :], reads=["yst"],
                              writes=[("ycat", kind, h, c)])
                        yield

                steps = [(c, j, a) for c in range(8) for j in range(4 * c + 4) for a in range(nh)]
                LAG = 3
                pend = []
                for st in steps:
                    pend.append((st, emit_qk(*st)))
                    if len(pend) > LAG:
                        st0, info = pend.pop(0)
                        emit_pv(*st0, *info)
                    self.bg_run(1)
                    self.bg2_run(2)
                while pend:
                    st0, info = pend.pop(0)
                    emit_pv(*st0, *info)
                self.bg2_drain()

            self.bg2 = []
            for _ in prep(0):
                pass
            for h in range(4):
                self.bg = prep(h + 1) if h + 1 < 4 else None
                main(h)
                self.bg_drain()

    def gla(self, l, kind):
        nc, S = self.nc, self.S
        hg = kind == "hgrn"
        pp, sv = self.ppt, self.sv
        NV = 128 if hg else 256
        CL = 40.0
        with ExitStack() as es:
            T = lambda nm, sh, dt_: es.enter_context(nc.sbuf_tensor(self.u(nm), sh, dt_))
            BR = T("BR", [128, 2, 64, 64], F32)
            B3 = BR[:, 0]
            R3 = BR[:, 1]
            qx = T("qx", [128, SEQ], F32)
            kx = T("kx", [128, SEQ], F32)
            tmp = T("tmp", [128, SEQ], F32)
            ones4k = T("ones4k", [128, SEQ], F32)
            Qs = T("Qs", [128, SEQ], BF16)
            Qb = T("Qb", [128, SEQ], BF16)
            Kb = T("Kb", [128, SEQ], BF16)
            Q32 = T("Q32", [128, SEQ], BF16)
            K32 = T("K32", [128, SEQ], BF16)
            K2 = T("K2", [128, SEQ], BF16)
            K2tm = T("K2tm", [128, 32, 128], BF16)
            Vx = T("Vx", [128, 32, 128], BF16)
            sm = T("sm", [128, 6, 64], F32)
            sm32 = T("sm32", [128, 128], F32)
            St = T("St", [128, NV], F32)
            Sall = BR[:].rearrange("p a b c -> p (a b c)").bitcast(BF16)[:, 0:64 * NV].rearrange("p (n v) -> p n v", v=NV)
            At0 = T("At0", [128, 512], BF16)
            At1 = T("At1", [128, 512], BF16)
            osb = T("osb", [128, 512], F32)
            dsb = T("dsb", [128, 512], F32) if not hg else None
            sqt = T("sqt", [128, 512], BF16) if hg else None
            rst = T("rst", [128, 512], F32) if hg else None
            gch = T("gch", [128, 512], F32)
            gch2 = T("gch2", [128, 512], F32) if hg else None
            yst = T("yst", [128, 512], BF16)
            B2 = B3[:].rearrange("p a b -> p (a b)")
            R2 = R3[:].rearrange("p a b -> p (a b)")
            B4 = B3[:].rearrange("p a (h b) -> p (a h) b", h=2)
            R4 = R3[:].rearrange("p a (h b) -> p (a h) b", h=2)
            S.op("pool", lambda e: e.memset(ones4k[:], 1.0), writes=["ones4k"])
            bprev, blast, b31, dch = sm[:, 0, :], sm[:, 1, :], sm[:, 2, :], sm[:, 3, :]
            mask4 = self.mask4_f
            atring = Ring("At", [At0, At1])

            def bc64(i):
                return sm[:, i, :].unsqueeze(2).to_broadcast([128, 64, 64])

            for tp in range(2):
                if hg:
                    tq, tf = T_HQ + tp, T_HF + tp
                    S.dma("sp", tmp[:], self.zfm[tf * 128:(tf + 1) * 128, :], writes=["tmp"])
                    S.dma("sp", qx[:], self.zfm[tq * 128:(tq + 1) * 128, :], writes=["qx"])
                    self.sigmoid_act(tmp[:], "tmp")
                    self.ts("dve", kx[:], tmp[:], sv[:, 2 + tp:3 + tp], sv[:, tp:tp + 1], ALU.mult, ALU.add,
                            ["tmp", "sv"], ["kx"])
                    self.act(B2, kx[:], AF.Ln, ["kx"], ["B"])
                    self.ts("dve", kx[:], kx[:], -1.0, 1.0, ALU.mult, ALU.add, ["kx"], ["kx"])
                    S.op("dve", lambda e: e.tensor_tensor_scan(B2, ones4k[:], B2, 0.0, ALU.mult, ALU.add),
                         ["B", "ones4k"], ["B"])
                    qscale = 1.0
                else:
                    for (tsrc, dst_, dk_, cb) in ((T_MQ + tp, qx, "qx", 34 + tp * 4), (T_MK + tp, kx, "kx", 42 + tp * 4)):
                        S.dma("sp", R2, self.zfm[tsrc * 128:(tsrc + 1) * 128, :], writes=["R"])
                        self.ts("dve", tmp[:], R2, pp[:, l, cb + 3:cb + 4], None, ALU.mult, None, ["R", "ppt"], ["tmp"])
                        for j in (2, 1, 0):
                            sh = 3 - j
                            self.stt("dve", tmp[:, sh:], R2[:, 0:SEQ - sh], pp[:, l, cb + j:cb + j + 1], tmp[:, sh:],
                                     ALU.mult, ALU.add, ["R", "tmp", "ppt"], ["tmp"])
                        self.act(dst_[:], tmp[:], AF.Silu, ["tmp"], [dk_])
                    tf, ti = T_MF + tp, T_MI + tp
                    S.dma("sp", B2, self.zfm[tf * 128:(tf + 1) * 128, :], reads=["B"], writes=["B"])
                    S.dma("sp", tmp[:], self.zfm[ti * 128:(ti + 1) * 128, :], reads=["tmp"], writes=["tmp"])
                    self.act(B2, B2, AF.Exp, ["B", "negpp"], ["B"], scale=-1.0, bias=self.negpp[:, 32 + tp:33 + tp])
                    self.act(B2, B2, AF.Ln, ["B"], ["B"], bias=1.0)
                    S.op("dve", lambda e: e.tensor_tensor_scan(B2, ones4k[:], B2, 0.0, ALU.mult, ALU.subtract),
                         ["B", "ones4k"], ["B"])
                    self.ts("dve", tmp[:], tmp[:], pp[:, l, 30 + tp:31 + tp], None, ALU.add, None, ["tmp", "ppt"], ["tmp"])
                    qscale = 0.125
                self.cp("dve", blast, B3[:, :, 63], ["B"], ["sm"])
                self.cp("dve", b31, B3[:, :, 31], ["B"], ["sm"])
                S.op("dve", lambda e: e.memset(sm[:, 0, 0:1], 0.0), ["sm"], ["sm"])
                self.cp("dve", bprev[:, 1:64], blast[:, 0:63], ["sm"], ["sm"])
                self.cp("dve", sm32[:], B4[:, :, 15], ["B"], ["sm32"])
                self.tt("dve", dch, blast, bprev, ALU.subtract, ["sm"], ["sm"])
                self.act(dch, dch, AF.Exp, ["sm"], ["sm"])
                bc32 = sm32[:].unsqueeze(2).to_broadcast([128, 128, 32])

                def emit(dst_, dk_, src, sk_, with_i, scale):
                    if with_i:
                        self.tt("dve", R2, R2, tmp[:], ALU.add, ["R", "tmp"], ["R"])
                    self.act(R2, R2, AF.Exp, ["R"], ["R"])
                    if scale == 1.0:
                        self.tt("dve", dst_[:], src[:], R2, ALU.mult, [sk_, "R"], [dk_])
                    else:
                        self.stt("dve", dst_[:], src[:], scale, R2, ALU.mult, ALU.mult, [sk_, "R"], [dk_])

                wi = not hg
                self.tt("dve", R3[:], B3[:], bc64(0), ALU.subtract, ["B", "sm"], ["R"])
                emit(Qs, "Qs", qx, "qx", False, qscale)
                self.tt("dve", R3[:], B3[:], bc64(1), ALU.subtract, ["B", "sm"], ["R"])
                self.ts("dve", R2, R2, -1.0, None, ALU.mult, None, ["R"], ["R"])
                emit(K2, "K2", kx, "kx", wi, 1.0)
                self.tt("dve", R3[:], B3[:], bc64(2), ALU.subtract, ["B", "sm"], ["R"])
                self.ts("dve", R2, R2, 0.0, None, ALU.min, None, ["R"], ["R"])
                emit(Qb, "Qb", qx, "qx", False, qscale)
                self.tt("dve", R3[:], B3[:], bc64(2), ALU.subtract, ["B", "sm"], ["R"])
                self.ts("dve", R2, R2, -1.0, 0.0, ALU.mult, ALU.min, ["R"], ["R"])
                emit(Kb, "Kb", kx, "kx", wi, 1.0)
                self.tt("dve", R4, B4, bc32, ALU.subtract, ["B", "sm32"], ["R"])
                self.ts("dve", R2, R2, CL, -CL, ALU.min, ALU.max, ["R"], ["R"])
                emit(Q32, "Q32", qx, "qx", False, qscale)
                self.tt("dve", R4, B4, bc32, ALU.subtract, ["B", "sm32"], ["R"])
                self.ts("dve", R2, R2, -1.0, CL, ALU.mult, ALU.min, ["R"], ["R"])
                self.ts("dve", R2, R2, -CL, None, ALU.max, None, ["R"], ["R"])
                emit(K32, "K32", kx, "kx", wi, 1.0)
                psT = self.PS[7][:, 0:64].bitcast(BF16)
                for m in range(32):
                    S.pe_group([lambda e, m=m: e.transpose(psT, K2[:, m * 128:(m + 1) * 128], self.ident_bf)],
                               ["K2", "cbf"], [("ps", 7)])
                    self.cp("act" if m % 2 == 0 else "dve", K2tm[:, m, :], psT, [("ps", 7)], [("K2tm", m)])
                voff = (V_H if hg else V_M) + tp * 128
                S.dma("sp", Vx[:, :, 0:128], self.ztm[:, voff:voff + 128].rearrange("(j p) c -> p j c", p=128),
                      writes=["Vx"])
                S.barrier()
                S.op("pool", lambda e: e.memset(St[:], 0.0), writes=["St"])
                S.op("pool", lambda e: e.memset(Sall[:, 0, :], 0.0), writes=[("Sall", 0)])
                pdr = [(self.PS[5], ("ps", 5)), (self.PS[7], ("ps", 7))]
                for n in range(64):
                    m, r = n // 2, n % 2
                    psd, pdk = pdr[n % 2]
                    items = [(psd[:, 0:128], K2tm[64 * r:64 * r + 64, m, :], Vx[64 * r:64 * r + 64, m, :], True, True)]
                    if not hg:
                        items.append((psd[:, 128:256], K2tm[64 * r:64 * r + 64, m, :], self.ones_bf[64 * r:64 * r + 64, :], True, True))
                    self.mm(items, [("K2tm", m), "Vx", "ones_bf"], [pdk])
                    self.stt("dve", St[:], St[:], dch[:, n:n + 1], psd[:, 0:NV], ALU.mult, ALU.add,
                             ["St", "sm", pdk], ["St"])
                    if n < 63:
                        self.cp("pool" if n % 2 == 0 else "act", Sall[:, n + 1, :], St[:], ["St"], [("Sall", n + 1)])
                NE, NO, DE, DO = self.PS[0], self.PS[1], self.PS[2], self.PS[3]
                allps = [("ps", 0), ("ps", 1), ("ps", 2), ("ps", 3)]
                for m in range(32):
                    cm = (m % 4) * 128
                    ms = slice(m * 128, (m + 1) * 128)
                    psa, psa2 = self.PS[4], self.PS[6]
                    self.mm([(psa[:, 0:128], K32[0:64, ms], Q32[0:64, ms], True, True)], ["K32", "Q32"], [("ps", 4)])
                    self.mm([(psa2[:, 0:128], K32[64:128, ms], Q32[64:128, ms], True, True)], ["K32", "Q32"], [("ps", 6)])
                    self.mm([(psa[:, 128:256], Kb[0:64, ms], Qb[0:64, ms], True, True)], ["Kb", "Qb"], [("ps", 4)])
                    self.mm([(psa2[:, 128:256], Kb[64:128, ms], Qb[64:128, ms], True, True)], ["Kb", "Qb"], [("ps", 6)])
                    At, ak = atring.next()
                    self.tt("dve", At[:, 0:256], psa[:, 0:256], mask4[:, 0:256], ALU.mult, [("ps", 4), "cf2"], [ak])
                    self.tt("dve", At[:, 256:512], psa2[:, 0:256], mask4[:, 0:256], ALU.mult, [("ps", 6), "cf2", ak], [ak])
                    items = [(NE[:, cm:cm + 128], Vx[:, m, 0:128], At[:, 0:128], True, False),
                             (NE[:, cm:cm + 128], Vx[:, m, 0:128], At[:, 128:256], False, False),
                             (NO[:, cm:cm + 128], Vx[:, m, 0:128], At[:, 256:384], True, False),
                             (NO[:, cm:cm + 128], Vx[:, m, 0:128], At[:, 384:512], False, False)]
                    if not hg:
                        items += [(DE[:, cm:cm + 128], self.ones_bf[:, :], At[:, 0:128], True, False),
                                  (DE[:, cm:cm + 128], self.ones_bf[:, :], At[:, 128:256], False, False),
                                  (DO[:, cm:cm + 128], self.ones_bf[:, :], At[:, 256:384], True, False),
                                  (DO[:, cm:cm + 128], self.ones_bf[:, :], At[:, 384:512], False, False)]
                    self.mm(items, ["Vx", ak], allps)
                    for r in range(2):
                        n = 2 * m + r
                        ns = slice(n * 64, (n + 1) * 64)
                        cn = cm + 64 * r
                        sbt = Sall[:, n, :]
                        items = [(NE[:, cn:cn + 64], sbt[0:64, 0:128], Qs[0:64, ns], False, r == 1),
                                 (NO[:, cn:cn + 64], sbt[64:128, 0:128], Qs[64:128, ns], False, r == 1)]
                        if not hg:
                            items += [(DE[:, cn:cn + 64], sbt[0:64, 128:256], Qs[0:64, ns], False, r == 1),
                                      (DO[:, cn:cn + 64], sbt[64:128, 128:256], Qs[64:128, ns], False, r == 1)]
                        self.mm(items, [("Sall", n), "Qs"], allps)
                    if m % 4 == 3:
                        c = m // 4
                        cs = slice(c * 512, (c + 1) * 512)
                        tg = (T_HG if hg else T_MO) + tp
                        S.dma("sp", gch[:], self.zfm[tg * 128:(tg + 1) * 128, cs], writes=["gch"])
                        if hg:
                            self.cp("act", osb[0:64, :], NE[0:64, :], [("ps", 0)], ["osb"])
                            self.cp("act", osb[64:128, :], NO[64:128, :], [("ps", 1), "osb"], ["osb"])
                            self.tt("pool", sqt[:], osb[:], osb[:], ALU.mult, ["osb"], ["sqt"])
                            ps = self.PS[7]
                            self.mm([(ps[:], self.blk64_bf, sqt[:], True, True)], ["sqt", "cbf"], [("ps", 7)])
                            self.rstd_from_ps(rst[:], ps[:], ("ps", 7), "rst", 1.0 / 64.0, EPS)
                            self.stt("dve", osb[:], osb[:], pp[:, l, 20:21], rst[:], ALU.mult, ALU.mult,
                                     ["osb", "rst", "ppt"], ["osb"])
                            self.act(gch[:], gch[:], AF.Silu, ["gch"], ["gch"])
                            self.tt("dve", yst[:], osb[:], gch[:], ALU.mult, ["osb", "gch"], ["yst"])
                            S.dma("sp", self.ycat[tp * 128:(tp + 1) * 128, cs], yst[:], reads=["yst"],
                                  writes=[("ycat", kind, tp, c)])
                        else:
                            self.cp("dve", dsb[0:64, :], DE[0:64, :], [("ps", 2)], ["dsb"])
                            self.cp("dve", dsb[64:128, :], DO[64:128, :], [("ps", 3), "dsb"], ["dsb"])
                            self.stt("dve", dsb[:], dsb[:], -1.0, dsb[:], ALU.mult, ALU.max, ["dsb"], ["dsb"])
                            self.ts("dve", dsb[:], dsb[:], 1.0, None, ALU.max, None, ["dsb"], ["dsb"])
                            S.op("dve", lambda e: e.reciprocal(dsb[:], dsb[:]), ["dsb"], ["dsb"])
                            self.tt("dve", osb[0:64, :], NE[0:64, :], dsb[0:64, :], ALU.mult, [("ps", 0), "dsb"], ["osb"])
                            self.tt("dve", osb[64:128, :], NO[64:128, :], dsb[64:128, :], ALU.mult, [("ps", 1), "dsb", "osb"], ["osb"])
                            self.sigmoid_act(gch[:], "gch")
                            self.tt("pool", yst[:], osb[:], gch[:], ALU.mult, ["osb", "gch"], ["yst"])
                            S.dma("sp", self.ycat[768 + tp * 128:768 + (tp + 1) * 128, cs], yst[:], reads=["yst"],
                                  writes=[("ycat", kind, tp, c)])
                S.barrier()

    def phaseCD(self, l, src, dst):
        nc, S = self.nc, self.S
        N = 512
        NCH = SEQ // N
        dstv = dst.ap().rearrange("(kt p) t -> p kt t", p=128)
        h2v = self.h2d.ap().rearrange("(kt p) t -> p kt t", p=128)
        srcv = src.ap().rearrange("(kt p) t -> p kt t", p=128)
        if self.dbg and "B" not in self.phases:
            ysrc = self.ycat_in.ap().rearrange("(kt p) t -> p kt t", p=128)
        else:
            ysrc = self.ycat.ap().rearrange("(kt p) t -> p kt t", p=128)
        self._psi = 0

        def nextps():
            i = self._psi
            self._psi = (i + 1) % 6
            return self.PS[i], ("ps", i)

        with ExitStack() as es0:
            T0 = lambda nm, sh, dt_: es0.enter_context(nc.sbuf_tensor(self.u(nm), sh, dt_))
            wff1 = T0("wff1", [128, 8, 4 * D], BF16)
            wff2 = T0("wff2", [128, 32, D], BF16)
            with ExitStack() as es:
                T = lambda nm, sh, dt_: es.enter_context(nc.sbuf_tensor(self.u(nm), sh, dt_))
                wout = T("wout", [128, 8, D], BF16)
                xcs = [T("xc%d" % i, [128, 8, N], F32) for i in range(2)]
                ycs = [T("yc0", [128, 8, N], BF16)] * 2
                hcs = [T("hc0", [128, 8, N], BF16)] * 2
                rstdc = T("rstdc", [128, N], F32)
                tmpring = Ring("tmpc", [T("tmpc%d" % i, [128, N], F32) for i in range(2)])
                for kt in range(8):
                    S.dma("pool", wout[:, kt, :], self.w_out[l, kt * 128:(kt + 1) * 128, :], writes=[("wout", kt)])
                woutk = [("wout", kt) for kt in range(8)]

                def loads(c):
                    S.dma("sp", xcs[c % 2][:], srcv[:, :, c * N:(c + 1) * N], writes=[("xc", c % 2)])

                def loady(c):
                    S.dma("pool" if self.dbg and "B" not in self.phases else "sp", ycs[0][:], ysrc[:, :, c * N:(c + 1) * N], writes=[("yc", 0)])

                loads(0)
                loady(0)
                for kt in range(8):
                    S.dma("pool", wff1[:, kt, :], self.w_ff1[l, kt * 128:(kt + 1) * 128, :], writes=[("wff1", kt)])
                for ft in range(32):
                    S.dma("pool", wff2[:, ft, :], self.w_ff2[l, ft * 128:(ft + 1) * 128, :], writes=[("wff2", ft)])
                for c in range(NCH):
                    if c + 1 < NCH:
                        loads(c + 1)
                    xc, xk = xcs[c % 2], ("xc", c % 2)
                    yc, yk = ycs[0], ("yc", 0)
                    hc, hk = hcs[0], ("hc", 0)
                    for ot in range(8):
                        ps, pk = nextps()
                        self.mm([(ps[:, :N], wout[:, kt, ot * 128:(ot + 1) * 128], yc[:, kt, :], kt == 0, kt == 7)
                                 for kt in range(8)], [yk] + woutk, [pk])
                        self.stt("dve", xc[:, ot, :], ps[:, :N], self.modv[:, 16 + ot:17 + ot], xc[:, ot, :],
                                 ALU.mult, ALU.add, [pk, xk, "modv"], [xk])
                    if c + 1 < NCH:
                        loady(c + 1)
                    S.dma("sp", dstv[:, :, c * N:(c + 1) * N], xc[:], reads=[xk], writes=[("dst", c)])
                    for _ in self.modulate_g(xc, xk, hc, hk, hc, hk, rstdc, "rstdc", tmpring, 7, N, 8, 24, on_act=True):
                        pass
                    S.dma("sp", h2v[:, :, c * N:(c + 1) * N], hc[:], reads=[(hk, kt) for kt in range(8)],
                          writes=[("h2d", c)])
                S.barrier()
            with ExitStack() as es:
                T = lambda nm, sh, dt_: es.enter_context(nc.sbuf_tensor(self.u(nm), sh, dt_))
                xds = [T("xd%d" % i, [128, 8, N], F32) for i in range(2)]
                hds = [T("hd%d" % i, [128, 8, N], BF16) for i in range(2)]
                hid = T("hid", [128, 16, N], BF16)
                rlring = Ring("rl", [T("rl%d" % i, [128, N], F32) for i in range(2)])
                wff1k = [("wff1", kt) for kt in range(8)]
                wff2k = [("wff2", ft) for ft in range(32)]

                def loads2(c):
                    S.dma("sp", xds[c % 2][:], dstv[:, :, c * N:(c + 1) * N], writes=[("xd", c % 2)])
                    S.dma("sp", hds[c % 2][:], h2v[:, :, c * N:(c + 1) * N], writes=[("hd", c % 2)])

                loads2(0)
                ev = 0
                for c in range(NCH):
                    if c + 1 < NCH:
                        loads2(c + 1)
                    xd, xk = xds[c % 2], ("xd", c % 2)
                    hd, hk = hds[c % 2], ("hd", c % 2)
                    for half in range(2):
                        for f in range(16):
                            ft = half * 16 + f
                            ps, pk = nextps()
                            self.mm([(ps[:, :N], wff1[:, kt, ft * 128:(ft + 1) * 128], hd[:, kt, :], kt == 0, kt == 7)
                                     for kt in range(8)], [hk] + wff1k, [pk])
                            rl, rk = rlring.next()
                            self.act(rl[:], ps[:, :N], AF.Relu, [pk], [rk])
                            self.tt("pool" if ev % 2 == 0 else "dve", hid[:, f, :], rl[:], rl[:], ALU.mult, [rk], [("hid", f)])
                            ev += 1
                        hidk = [("hid", f) for f in range(16)]
                        for ot in range(8):
                            ps, pk = nextps()
                            self.mm([(ps[:, :N], wff2[:, half * 16 + f, ot * 128:(ot + 1) * 128], hid[:, f, :], f == 0, f == 15)
                                     for f in range(16)], hidk + wff2k, [pk])
                            self.stt("dve", xd[:, ot, :], ps[:, :N], self.modv[:, 40 + ot:41 + ot], xd[:, ot, :],
                                     ALU.mult, ALU.add, [pk, xk, "modv"], [xk])
                    S.dma("sp", dstv[:, :, c * N:(c + 1) * N], xd[:], reads=[xk], writes=[("dst2", c)])
                S.barrier()


def _prep_inputs(inp):
    cols = _col_index()
    w_in = np.asarray(inp["w_in"], np.float32)
    w_in_p = np.zeros((L, D, NCOL), np.float32)
    valid = cols >= 0
    w_in_p[:, :, valid] = w_in[:, :, cols[valid]]
    cmat, alibi, crow = _host_consts()
    pp = np.stack([_pp_layer(inp, l) for l in range(L)], 0)
    lam = np.asarray(inp["diff_lambda"], np.float32).reshape(L, 128)
    shared = dict(
        w_ada=np.ascontiguousarray(inp["w_ada"], np.float32), b_ada=np.ascontiguousarray(inp["b_ada"], np.float32),
        w_in=w_in_p, w_out=np.ascontiguousarray(inp["w_out"], np.float32),
        w_ff1=np.ascontiguousarray(inp["w_ff1"], np.float32), w_ff2=np.ascontiguousarray(inp["w_ff2"], np.float32),
        pp=pp, lam=lam, cmat=cmat, alibi=alibi, crow=crow,
        onesrow=np.ones((1, SEQ), np.float32), mask4=_mask4())
    maps = []
    x = np.asarray(inp["x"], np.float32)
    c = np.asarray(inp["c"], np.float32)
    for b in range(x.shape[0]):
        m = dict(shared)
        m["xT"] = np.ascontiguousarray(x[b].T)
        m["c8"] = np.ascontiguousarray(c[b].reshape(8, 128).T)
        maps.append(m)
    return maps


def kernel(**inputs):
    inp = {k: np.asarray(v) for k, v in inputs.items()}
    maps = _prep_inputs(inp)
    mk = MK()
    res = run_bass_kernel_spmd(mk.nc, maps, core_ids=list(range(len(maps))))
    out = np.stack([np.ascontiguousarray(r["outT"].T) for r in res.results], 0)
    return out.astype(np.float32)
```
